# Optimizing a Trainium2 kernel written in Bass

```python
import math
import jax
import jax.numpy as jnp
from jax import lax
import numpy as np

D_MODEL = 1024
BATCH = 16
SEQ = 2048
DEPTH = 4

GRID_W = 64
CTX_LEN = 256

HEAD_DIM = 64
N_Q_HEADS = 8
N_KV_HEADS = 2
Q_GROUP = N_Q_HEADS // N_KV_HEADS
ATT_Q_W = N_Q_HEADS * HEAD_DIM
ATT_KV_W = N_KV_HEADS * HEAD_DIM
Q_BLOCK = 128
ROPE_THETA = 10000.0
ROPE_AXIS_DIM = HEAD_DIM // 2

HY_W = D_MODEL // 4
HY_ORDER = 2
HY_SHORT = 3
HY_BANDS = 16
HY_EMB = 2 * HY_BANDS + 1
HY_FFN = 64
HY_DECAY_TARGET = 1e-2
HY_DECAY_SHORT_PCT = 0.3
HY_DECAY_LONG_PCT = 1.5

FN_GROUPS = 4
FN_GROUP_W = D_MODEL // 16
FN_W = FN_GROUPS * FN_GROUP_W

MIX_W = ATT_Q_W + HY_W + FN_W
N_BRANCH = 3

OFF_Q = 0
OFF_K = OFF_Q + ATT_Q_W
OFF_V = OFF_K + ATT_KV_W
OFF_HY = OFF_V + ATT_KV_W
OFF_FN = OFF_HY + (HY_ORDER + 1) * HY_W
OFF_GATE = OFF_FN + FN_W
IN_W = OFF_GATE + N_BRANCH * D_MODEL

N_EXPERTS = 16
EXPERT_FF = 2 * D_MODEL
CAPACITY_FACTOR = 2

EPS = 1e-6

kernel_name = "hybrid_hyena_fnet_gqa_ecmoe_dit"


def rms_norm(x, g):
    xf = x.astype(jnp.float32)
    y = xf * lax.rsqrt(jnp.mean(xf * xf, axis=-1, keepdims=True) + EPS)
    return (y * g.astype(jnp.float32)).astype(x.dtype)


def modulate(h, shift, scale):
    return h * (1.0 + scale) + shift


def axial_rope_tables(n_tokens):
    rows = n_tokens // GRID_W
    row = jnp.repeat(jnp.arange(rows, dtype=jnp.float32), GRID_W)
    col = jnp.tile(jnp.arange(GRID_W, dtype=jnp.float32), rows)
    inv = ROPE_THETA ** (-jnp.arange(0, ROPE_AXIS_DIM, 2, dtype=jnp.float32) / ROPE_AXIS_DIM)
    ang = jnp.concatenate([row[:, None] * inv, col[:, None] * inv], axis=-1)
    return jnp.cos(ang), jnp.sin(ang)


def apply_rope(u, cos, sin):
    uf = u.astype(jnp.float32).reshape(*u.shape[:-1], HEAD_DIM // 2, 2)
    u0, u1 = uf[..., 0], uf[..., 1]
    c = cos[None, :, None, :]
    s = sin[None, :, None, :]
    out = jnp.stack([u0 * c - u1 * s, u0 * s + u1 * c], axis=-1)
    return out.reshape(u.shape).astype(u.dtype)


def qk_heads(u, n_heads, gain, rope):
    b, n, _ = u.shape
    u = rms_norm(u.reshape(b, n, n_heads, HEAD_DIM), gain)
    if rope is not None:
        u = apply_rope(u, rope[0], rope[1])
    return u


def block_attention(q, k, v):
    b, n = q.shape[0], q.shape[1]
    nb = n // Q_BLOCK
    qb = q.reshape(b, nb, Q_BLOCK, N_KV_HEADS, Q_GROUP, HEAD_DIM).transpose(1, 0, 2, 3, 4, 5)
    scale = HEAD_DIM ** -0.5

    def attend(qblk):
        s = jnp.einsum('bqkgd,bskd->bkgqs', qblk, k, preferred_element_type=jnp.float32) * scale
        p = jax.nn.softmax(s, axis=-1).astype(v.dtype)
        return jnp.einsum('bkgqs,bskd->bqkgd', p, v)

    o = lax.map(attend, qb)
    return o.transpose(1, 0, 2, 3, 4, 5).reshape(b, n, ATT_Q_W)


def short_conv(u, w, b):
    n = u.shape[1]
    pad = HY_SHORT // 2
    up = jnp.pad(u, ((0, 0), (pad, pad), (0, 0)))
    out = b
    for j in range(HY_SHORT):
        out = out + up[:, j:j + n] * w[j]
    return out


def hyena_spectrum(n, w1, b1, w2, b2, w3, freq):
    f32 = jnp.float32
    w1, b1, w2, b2, w3, freq = (a.astype(f32) for a in (w1, b1, w2, b2, w3, freq))
    pos = jnp.arange(n, dtype=f32)
    t = pos / (n - 1)
    bands = jnp.linspace(1e-4, HY_BANDS - 1, HY_BANDS, dtype=f32)
    ang = (2.0 * math.pi / n) * pos[:, None] * bands[None, :]
    feats = jnp.concatenate([t[:, None], jnp.cos(ang), -jnp.sin(ang)], axis=-1)
    h = jnp.sin(freq * (feats @ w1 + b1))
    h = jnp.sin(freq * (h @ w2 + b2))
    h = (h @ w3).reshape(n, 2, HY_ORDER, HY_W)
    deltas = jnp.abs(jnp.linspace(math.log(HY_DECAY_TARGET) / HY_DECAY_LONG_PCT,
                                  math.log(HY_DECAY_TARGET) / HY_DECAY_SHORT_PCT, HY_W, dtype=f32))
    h = h * jnp.exp(-t[:, None] * deltas[None, :])[:, None, None, :]
    filt = jnp.concatenate([h[:, 0], jnp.zeros((1, HY_ORDER, HY_W), f32), h[1:, 1][::-1]], axis=0)
    filt = filt / jnp.sum(jnp.abs(filt), axis=0, keepdims=True)
    return jnp.fft.rfft(filt, axis=0)


def long_conv(z, spec, d):
    n = z.shape[1]
    zf = z.astype(jnp.float32)
    y = jnp.fft.irfft(jnp.fft.rfft(zf, n=2 * n, axis=1) * spec[None], n=2 * n, axis=1)[:, :n]
    return (y + zf * d.astype(jnp.float32)).astype(z.dtype)


def hyena_mixer(u, sw, sb, spec, bias):
    u = short_conv(u, sw, sb)
    v, g1, g2 = jnp.split(u, HY_ORDER + 1, axis=-1)
    z = g1 * long_conv(v, spec[:, 0], bias[0])
    return g2 * long_conv(z, spec[:, 1], bias[1])


def fourier_mix(u):
    b, n, _ = u.shape
    ug = u.astype(jnp.float32).reshape(b, n, FN_GROUPS, FN_GROUP_W)
    y = jnp.fft.fft2(ug, axes=(1, 3), norm='ortho').real
    return y.reshape(b, n, FN_W).astype(u.dtype)


def merge_branches(att, hy, fn, gates, w_branch, w_out):
    ga, gh, gf = jnp.split(gates, N_BRANCH, axis=-1)
    ya = att @ w_branch[:ATT_Q_W]
    yh = hy @ w_branch[ATT_Q_W:ATT_Q_W + HY_W]
    yf = fn @ w_branch[ATT_Q_W + HY_W:]
    m = jax.nn.sigmoid(ga) * ya + jax.nn.sigmoid(gh) * yh + jax.nn.sigmoid(gf) * yf
    return m @ w_out


def mixer_sublayer(h, k_ext, v_ext, rope, w_in, q_gain, k_gain, sw, sb, spec, hy_bias, w_branch, w_out):
    b, n, _ = h.shape
    p = h @ w_in
    q = qk_heads(p[..., OFF_Q:OFF_K], N_Q_HEADS, q_gain, rope)
    k = qk_heads(p[..., OFF_K:OFF_V], N_KV_HEADS, k_gain, rope)
    v = p[..., OFF_V:OFF_HY].reshape(b, n, N_KV_HEADS, HEAD_DIM)
    if k_ext is None:
        att = block_attention(q, k, v)
    else:
        att = block_attention(q, jnp.concatenate([k, k_ext], axis=1), jnp.concatenate([v, v_ext], axis=1))
    hy = hyena_mixer(p[..., OFF_HY:OFF_FN], sw, sb, spec, hy_bias)
    fn = fourier_mix(p[..., OFF_FN:OFF_GATE])
    return merge_branches(att, hy, fn, p[..., OFF_GATE:], w_branch, w_out), k, v


def expert_choice_moe(h, w_router, w_gate, w_up, w_down):
    b, n, d = h.shape
    cap = CAPACITY_FACTOR * n // N_EXPERTS
    logits = jnp.einsum('bld,de->ble', h, w_router, preferred_element_type=jnp.float32)
    aff = jax.nn.softmax(logits, axis=-1)
    top_w, top_i = lax.top_k(aff.transpose(0, 2, 1), cap)
    xg = jax.vmap(lambda hb, ib: hb[ib])(h, top_i)
    a = jnp.einsum('becd,edf->becf', xg, w_gate)
    u = jnp.einsum('becd,edf->becf', xg, w_up)
    y = jnp.einsum('becf,efd->becd', jax.nn.silu(a) * u, w_down)
    y = y * top_w[..., None].astype(y.dtype)
    return jax.vmap(lambda yb, ib: jnp.zeros((n, d), y.dtype).at[ib.reshape(-1)].add(yb.reshape(-1, d)))(y, top_i)


def setup_inputs(seed: int = 0) -> dict:
    key = jax.random.key(seed)
    ks = jax.random.split(key, 27)
    f32 = jnp.float32
    D = D_MODEL

    def nrm(k, shape, scale):
        return jax.random.normal(k, shape, f32) * scale

    return {
        "x": nrm(ks[0], (BATCH, SEQ, D), 1.0),
        "c": nrm(ks[1], (BATCH, D), 1.0),
        "ctx": nrm(ks[2], (BATCH, CTX_LEN, D), 1.0),
        "c_ctx": nrm(ks[3], (D,), 1.0),
        "w_mod": nrm(ks[4], (DEPTH, D, 6 * D), 0.5 * D ** -0.5),
        "b_mod": nrm(ks[5], (DEPTH, 6 * D), 0.02),
        "norm1_g": 1.0 + nrm(ks[6], (DEPTH, D), 0.02),
        "norm2_g": 1.0 + nrm(ks[7], (DEPTH, D), 0.02),
        "w_in": nrm(ks[8], (DEPTH, D, IN_W), D ** -0.5),
        "q_gain": 1.0 + nrm(ks[9], (DEPTH, HEAD_DIM), 0.02),
        "k_gain": 1.0 + nrm(ks[10], (DEPTH, HEAD_DIM), 0.02),
        "hy_short_w": nrm(ks[11], (DEPTH, HY_SHORT, (HY_ORDER + 1) * HY_W), HY_SHORT ** -0.5),
        "hy_short_b": nrm(ks[12], (DEPTH, (HY_ORDER + 1) * HY_W), 0.02),
        "hy_f_w1": nrm(ks[13], (DEPTH, HY_EMB, HY_FFN), HY_EMB ** -0.5),
        "hy_f_b1": nrm(ks[14], (DEPTH, HY_FFN), 0.1),
        "hy_f_w2": nrm(ks[15], (DEPTH, HY_FFN, HY_FFN), HY_FFN ** -0.5),
        "hy_f_b2": nrm(ks[16], (DEPTH, HY_FFN), 0.1),
        "hy_f_w3": nrm(ks[17], (DEPTH, HY_FFN, 2 * HY_ORDER * HY_W), HY_FFN ** -0.5),
        "hy_f_freq": 1.0 + nrm(ks[18], (DEPTH, HY_FFN), 0.02),
        "hy_bias": nrm(ks[19], (DEPTH, HY_ORDER, HY_W), 1.0),
        "w_branch": nrm(ks[20], (DEPTH, MIX_W, D), (MIX_W // 4) ** -0.5),
        "w_out": nrm(ks[21], (DEPTH, D, D), D ** -0.5),
        "w_router": nrm(ks[22], (DEPTH, D, N_EXPERTS), D ** -0.5),
        "w_gate": nrm(ks[23], (DEPTH, N_EXPERTS, D, EXPERT_FF), D ** -0.5),
        "w_up": nrm(ks[24], (DEPTH, N_EXPERTS, D, EXPERT_FF), D ** -0.5),
        "w_down": nrm(ks[25], (DEPTH, N_EXPERTS, EXPERT_FF, D), EXPERT_FF ** -0.5),
        "final_g": 1.0 + nrm(ks[26], (D,), 0.02),
    }


def reference(x, c, ctx, c_ctx, w_mod, b_mod, norm1_g, norm2_g, w_in, q_gain, k_gain,
              hy_short_w, hy_short_b, hy_f_w1, hy_f_b1, hy_f_w2, hy_f_b2, hy_f_w3, hy_f_freq,
              hy_bias, w_branch, w_out, w_router, w_gate, w_up, w_down, final_g):
    n_lat = x.shape[1]
    n_ctx = ctx.shape[1]
    rope = axial_rope_tables(n_lat)
    sc = jax.nn.silu(c)
    scc = jax.nn.silu(c_ctx)
    for l in range(DEPTH):
        last = l == DEPTH - 1
        mx = (sc @ w_mod[l] + b_mod[l])[:, None, :]
        mc = scc @ w_mod[l] + b_mod[l]
        sh1, sc1, g1, sh2, sc2, g2 = jnp.split(mx, 6, axis=-1)
        csh1, csc1, cg1, csh2, csc2, cg2 = jnp.split(mc, 6, axis=-1)

        hc = modulate(rms_norm(ctx, norm1_g[l]), csh1, csc1)
        if last:
            pkv = hc @ w_in[l][:, OFF_K:OFF_HY]
            kc = qk_heads(pkv[..., :ATT_KV_W], N_KV_HEADS, k_gain[l], None)
            vc = pkv[..., ATT_KV_W:].reshape(ctx.shape[0], n_ctx, N_KV_HEADS, HEAD_DIM)
        else:
            spec_c = hyena_spectrum(n_ctx, hy_f_w1[l], hy_f_b1[l], hy_f_w2[l], hy_f_b2[l], hy_f_w3[l], hy_f_freq[l])
            yc, kc, vc = mixer_sublayer(hc, None, None, None, w_in[l], q_gain[l], k_gain[l],
                                        hy_short_w[l], hy_short_b[l], spec_c, hy_bias[l], w_branch[l], w_out[l])

        spec_x = hyena_spectrum(n_lat, hy_f_w1[l], hy_f_b1[l], hy_f_w2[l], hy_f_b2[l], hy_f_w3[l], hy_f_freq[l])
        hx = modulate(rms_norm(x, norm1_g[l]), sh1, sc1)
        yx, _, _ = mixer_sublayer(hx, kc, vc, rope, w_in[l], q_gain[l], k_gain[l],
                                  hy_short_w[l], hy_short_b[l], spec_x, hy_bias[l], w_branch[l], w_out[l])
        x = x + g1 * yx
        h2 = modulate(rms_norm(x, norm2_g[l]), sh2, sc2)
        x = x + g2 * expert_choice_moe(h2, w_router[l], w_gate[l], w_up[l], w_down[l])

        if not last:
            ctx = ctx + cg1 * yc
            hc2 = modulate(rms_norm(ctx, norm2_g[l]), csh2, csc2)
            ctx = ctx + cg2 * expert_choice_moe(hc2, w_router[l], w_gate[l], w_up[l], w_down[l])
    return rms_norm(x, final_g)
```

```python
import contextlib
import numpy as np
import concourse.bass as bass
import concourse.mybir as mybir

F32 = mybir.dt.float32
BF16 = mybir.dt.bfloat16
U32 = mybir.dt.uint32
I32 = mybir.dt.int32
ALU = mybir.AluOpType
AF = mybir.ActivationFunctionType
AX = mybir.AxisListType


class Res:
    __slots__ = ("w", "r")

    def __init__(self):
        self.w = {}
        self.r = {}


class Eng:
    def __init__(self, name, h, sem):
        self.name = name
        self.h = h
        self.sem = sem
        self.cnt = 0
        self.seen = {}


class DmaQ:
    def __init__(self, name, h, sems):
        self.name = name
        self.h = h
        self.sems = sems
        self.j = 0
        self.seen = {}


class FW:
    def __init__(self, nc, n_dma_sems=8):
        self.nc = nc
        self.es = contextlib.ExitStack()
        mk = lambda n: self.es.enter_context(nc.semaphore(n))
        self.pe = Eng("pe", nc.tensor, mk("s_pe"))
        self.act = Eng("act", nc.scalar, mk("s_act"))
        self.dve = Eng("dve", nc.vector, mk("s_dve"))
        self.pool = Eng("pool", nc.gpsimd, mk("s_pool"))
        self.engs = [self.pe, self.act, self.dve, self.pool]
        self.sp = DmaQ("sp", nc.sync, [mk("s_sp%d" % i) for i in range(n_dma_sems)])
        self.qs = [self.sp]
        self.semid = {}
        self.n_wait = 0
        self.n_ins = 0
        self._psum = []
        self._psum_i = 0
        self._ring = list(range(8))

    def sbuf(self, name, shape, dtype):
        return self.es.enter_context(self.nc.sbuf_tensor(name, list(shape), dtype))

    def psum_banks(self):
        for i in range(8):
            t = self.es.enter_context(self.nc.psum_tensor("ps%d" % i, [128, 512], F32))
            self._psum.append((t, Res()))

    def ps(self):
        ring = self._ring
        t, r = self._psum[ring[self._psum_i % len(ring)]]
        self._psum_i += 1
        return t, r

    def ring(self, banks):
        self._ring = list(banks)

    def bank(self, i):
        return self._psum[i]

    def tr(self, reads, writes, out, in_, ident):
        e = self.pe
        self._wait(e, self._deps(reads, writes))
        ins = e.h.transpose(out, in_, ident)
        e.cnt += 1
        ins.then_inc(e.sem, 1)
        self._mark(reads, writes, self._key(e.sem), e.cnt)
        self.n_ins += 1

    def close(self):
        self.es.close()

    def _key(self, sem):
        k = id(sem)
        self.semid[k] = sem
        return k

    def _deps(self, reads, writes):
        d = {}
        for r in reads:
            for k, v in r.w.items():
                if d.get(k, 0) < v:
                    d[k] = v
        for w in writes:
            for k, v in w.w.items():
                if d.get(k, 0) < v:
                    d[k] = v
            for k, v in w.r.items():
                if d.get(k, 0) < v:
                    d[k] = v
        return d

    def _wait(self, e, deps):
        for k, v in deps.items():
            if e is self.pe and self.semid[k] is e.sem:
                continue
            if e.seen.get(k, 0) < v:
                e.h.wait_ge(self.semid[k], v)
                e.seen[k] = v
                self.n_wait += 1

    def _mark(self, reads, writes, k, v):
        for r in reads:
            if r.r.get(k, 0) < v:
                r.r[k] = v
        for w in writes:
            w.w = {k: v}
            w.r = {}

    def op(self, e, reads, writes, name, *a, **kw):
        self._wait(e, self._deps(reads, writes))
        ins = getattr(e.h, name)(*a, **kw)
        e.cnt += 1
        ins.then_inc(e.sem, 1)
        k = self._key(e.sem)
        self._mark(reads, writes, k, e.cnt)
        self.n_ins += 1
        return ins

    def mm(self, reads, writes, out, pairs, start=True, stop=True):
        e = self.pe
        self._wait(e, self._deps(reads, writes))
        ins = None
        n = len(pairs)
        for i, (a, b) in enumerate(pairs):
            ins = e.h.matmul(out, a, b, start=(start and i == 0), stop=(stop and i == n - 1))
            self.n_ins += 1
        e.cnt += 1
        ins.then_inc(e.sem, 1)
        k = self._key(e.sem)
        self._mark(reads, writes, k, e.cnt)

    def dma(self, reads, writes, out, in_, q=None, **kw):
        q = q or self.sp
        deps = self._deps(reads, writes)
        n = len(q.sems)
        slot = q.j % n
        sem = q.sems[slot]
        k = self._key(sem)
        prev = 16 * (q.j // n)
        if prev > 0:
            deps[k] = max(deps.get(k, 0), prev)
        self._wait(q, deps)
        q.h.dma_start(out=out, in_=in_, **kw).then_inc(sem, 16)
        q.j += 1
        self._mark(reads, writes, k, prev + 16)
        self.n_ins += 1

    def barrier(self):
        d = {}
        for e in self.engs:
            if e.cnt:
                d[self._key(e.sem)] = e.cnt
        for q in self.qs:
            n = len(q.sems)
            for s in range(n):
                uses = (q.j - s + n - 1) // n
                if uses > 0:
                    d[self._key(q.sems[s])] = 16 * uses
        for e in self.engs + self.qs:
            self._wait(e, d)

import math
import numpy as np
import ml_dtypes

BF = ml_dtypes.bfloat16
D = 1024
DEPTH = 4
NL = 2048
NCX = 256
HD = 64


def col_order():
    cols = []
    types = []
    sw = lambda base, n: [base + (i ^ 1) for i in range(n)]
    for hp in range(4):
        b = hp * 128
        cols += list(range(b, b + 128)); types.append(("q", hp))
        cols += sw(b, 128); types.append(("qs", hp))
    cols += list(range(512, 640)); types.append(("k", 0))
    cols += sw(512, 128); types.append(("ks", 0))
    cols += list(range(640, 768)); types.append(("v", 0))
    for i in range(2):
        cols += list(range(1536 + i * 128, 1536 + (i + 1) * 128)); types.append(("fn", i))
    for i in range(6):
        cols += list(range(768 + i * 128, 768 + (i + 1) * 128)); types.append(("hy", i))
    for i in range(24):
        cols += list(range(1792 + i * 128, 1792 + (i + 1) * 128)); types.append(("g", i))
    return np.array(cols), types


def pvec(v, nch):
    return np.ascontiguousarray(v.reshape(nch, 128).T)


def tile_stationary(M):
    R, C = M.shape
    return np.ascontiguousarray(M.reshape(R // 128, 128, C // 128, 128).transpose(2, 1, 0, 3))


def tile_moving(M, w):
    R, C = M.shape
    return np.ascontiguousarray(M.reshape(R // 128, 128, C // w, w).transpose(2, 1, 0, 3))


def dft_consts(n, pre):
    a = np.arange(n, dtype=np.float64)
    ang = np.pi * np.outer(a, a) / n
    Cm = np.cos(ang)
    Fs = -np.sin(ang)
    Fs[:, 0] = (-1.0) ** a
    Gs = Fs.T.copy()
    ang2 = 2.0 * ang
    Ct = np.cos(ang2)
    Stn = -np.sin(ang2)
    w = min(512, n)
    c = {}
    c[pre + "C_st"] = tile_stationary(Cm).astype(BF)
    c[pre + "C_mv"] = tile_moving(Cm, w).astype(BF)
    c[pre + "Fs_st"] = tile_stationary(Fs).astype(BF)
    c[pre + "Gs_st"] = tile_stationary(Gs).astype(BF)
    c[pre + "Gs_mv"] = tile_moving(Gs, w).astype(BF)
    c[pre + "Ct_mv"] = tile_moving(Ct, w).astype(BF)
    c[pre + "St_mv"] = tile_moving(Stn, w).astype(BF)
    t = a / (n - 1)
    bands = np.linspace(1e-4, 15.0, 16)
    an = (2.0 * math.pi / n) * np.outer(a, bands)
    feats = np.concatenate([t[:, None], np.cos(an), -np.sin(an)], axis=-1)
    c[pre + "featsT"] = np.ascontiguousarray(feats.T).astype(np.float32)
    deltas = np.abs(np.linspace(math.log(1e-2) / 1.5, math.log(1e-2) / 0.3, 256))
    dec = np.exp(-t[:, None] * deltas[None, :])
    dec2 = np.concatenate([dec, dec], axis=1)
    c[pre + "dec2"] = np.ascontiguousarray(dec2.reshape(n // 128, 128, 512).transpose(1, 0, 2)).astype(np.float32)
    rs = np.zeros((n, 2))
    rs[:, 0] = 1.0 / n
    rs[0, 0] = 0.5 / n
    rs[:, 1] = 1.0 / n
    rs[0, 1] = 0.0
    c[pre + "rs"] = np.ascontiguousarray(rs.reshape(n // 128, 128, 2).transpose(1, 0, 2)).astype(np.float32)
    return c


def shared_consts():
    c = {}
    c.update(dft_consts(NL, "L_"))
    c.update(dft_consts(NCX, "X_"))
    tt = np.arange(NL)
    row = (tt // 64).astype(np.float64)
    col = (tt % 64).astype(np.float64)
    inv = 10000.0 ** (-np.arange(0, 32, 2, dtype=np.float64) / 32)
    ang = np.concatenate([row[:, None] * inv, col[:, None] * inv], axis=-1)
    p = np.arange(128)
    dim = p % 64
    i = dim // 2
    sgn = np.where(dim % 2 == 0, -1.0, 1.0)
    c["ropeC"] = np.cos(ang)[:, i].T.astype(np.float32).copy()
    c["ropeS"] = (np.sin(ang)[:, i] * sgn[None, :]).T.astype(np.float32).copy()
    cc = np.arange(64)
    th = 2 * np.pi * np.outer(cc, cc) / 64
    for pre, n in (("L_", NL), ("X_", NCX)):
        sc = 1.0 / math.sqrt(n * 64)
        Dc = np.zeros((128, 128)); Ds = np.zeros((128, 128))
        for g in range(2):
            Dc[g * 64:(g + 1) * 64, g * 64:(g + 1) * 64] = np.cos(th) * sc
            Ds[g * 64:(g + 1) * 64, g * 64:(g + 1) * 64] = np.sin(th) * sc
        c[pre + "DcDs"] = np.concatenate([Dc, Ds], axis=1).astype(BF)
    bo = np.zeros((128, 128)); bo[:64, :64] = 1; bo[64:, 64:] = 1
    c["blockones"] = bo.astype(BF)
    c["ones_bf"] = np.ones((128, 128)).astype(BF)
    alt = ((-1.0) ** np.arange(128))[:, None]
    c["altcol"] = alt.astype(BF)
    es = np.zeros((128, 32, 128))
    for q in range(32):
        es[q, q, :] = 1
    c["esel"] = es.reshape(128, 32 * 128).astype(BF)
    return c


def prep_weights(inp):
    w = {}
    cols, _ = col_order()
    w["w_mod"] = np.ascontiguousarray(inp["w_mod"])
    w["b_mod"] = np.stack([pvec(inp["b_mod"][l], 48) for l in range(DEPTH)])
    w["n1g"] = np.stack([pvec(inp["norm1_g"][l], 8) for l in range(DEPTH)])
    w["n2g"] = np.stack([pvec(inp["norm2_g"][l], 8) for l in range(DEPTH)])
    w["fing"] = pvec(inp["final_g"], 8)
    w["w_in"] = np.ascontiguousarray(inp["w_in"][:, :, cols])
    p = np.arange(128) % 64
    qg = inp["q_gain"]; kg = inp["k_gain"]
    w["qkg"] = np.ascontiguousarray(np.stack([qg[:, p], qg[:, p ^ 1], kg[:, p], kg[:, p ^ 1]], axis=-1))
    w["hy_sw"] = np.ascontiguousarray(inp["hy_short_w"].reshape(DEPTH, 3, 6, 128).transpose(0, 3, 2, 1))
    w["hy_sb"] = np.stack([pvec(inp["hy_short_b"][l], 6) for l in range(DEPTH)])
    w["hy_w1"] = np.ascontiguousarray(inp["hy_f_w1"])
    w["hy_w2"] = np.ascontiguousarray(inp["hy_f_w2"])
    w["hy_w3"] = np.ascontiguousarray(inp["hy_f_w3"])
    w["hy_b1"] = np.ascontiguousarray(inp["hy_f_b1"][:, :, None])
    w["hy_b2"] = np.ascontiguousarray(inp["hy_f_b2"][:, :, None])
    w["hy_fr"] = np.ascontiguousarray(inp["hy_f_freq"][:, :, None])
    w["hy_bias"] = np.ascontiguousarray(inp["hy_bias"].reshape(DEPTH, 1, 512))
    w["w_branch"] = np.ascontiguousarray(inp["w_branch"])
    w["w_out"] = np.ascontiguousarray(inp["w_out"])
    w["w_router"] = np.ascontiguousarray(inp["w_router"])
    w["w_gate"] = np.ascontiguousarray(inp["w_gate"])
    w["w_up"] = np.ascontiguousarray(inp["w_up"])
    w["w_down"] = np.ascontiguousarray(inp["w_down"])
    return w


def prep_core(inp, core):
    b0 = 2 * core
    m = {}
    m["x"] = np.ascontiguousarray(inp["x"][b0:b0 + 2])
    m["ctx"] = np.ascontiguousarray(inp["ctx"][b0:b0 + 2])
    c3 = np.stack([inp["c"][b0], inp["c"][b0 + 1], inp["c_ctx"]], axis=-1)
    m["cT"] = np.ascontiguousarray(c3.reshape(8, 128, 3).transpose(1, 0, 2))
    return m

import contextlib
import math
import numpy as np

EPS = 1e-6
PI = math.pi


class Buf:
    def __init__(self, ph, name, shape, dt, n=2):
        self.t = []
        self.r = []
        for i in range(n):
            t, r = ph.sb("%s_%d" % (name, i), shape, dt)
            self.t.append(t)
            self.r.append(r)
        self.i = 0

    def get(self):
        i = self.i % len(self.t)
        self.i += 1
        return self.t[i], self.r[i]


class Phase:
    cnt = 0

    def __init__(self, fw):
        self.fw = fw
        self.es = contextlib.ExitStack()

    def __enter__(self):
        return self

    def __exit__(self, *a):
        self.fw.barrier()
        self.es.close()
        return False

    def sb(self, name, shape, dt):
        Phase.cnt += 1
        t = self.es.enter_context(self.fw.nc.sbuf_tensor("%s_%d" % (name, Phase.cnt), list(shape), dt))
        return t, Res()

    def buf(self, name, shape, dt, n=2):
        return Buf(self, name, shape, dt, n)


class Stream:
    def __init__(self, nc, name, n, dumps=()):
        self.name = name
        self.n = n
        self.NT = 2 * n
        self.NC = n // 128
        self.W = min(512, n)
        self.cap = 2 * n // 16
        self.SC = (self.cap + 127) // 128
        self.SP = min(self.cap, 128)
        def d(nm, sh, dt):
            full = "%s_%s" % (name, nm)
            if full in dumps:
                return nc.dram_tensor(full, list(sh), dt, kind="ExternalOutput").ap()
            return nc.dram_tensor(full, list(sh), dt).ap()
        NT = self.NT
        self.xT = d("xT", [1024, NT], F32)
        self.qT = d("qT", [512, NT], BF16)
        self.kT = d("kT", [128, NT], BF16)
        self.v = d("v", [NT, 128], BF16)
        self.hyu = d("hyu", [768, NT], F32)
        self.ucs = d("ucs", [NT, 2, 256], BF16)
        self.sg = d("sg", [3072, NT], BF16)
        self.mixT = d("mixT", [1024, NT], BF16)
        self.specA = d("specA", [n, 512], F32)
        self.specB = d("specB", [n, 512], F32)
        self.specA2 = d("specA2", [128, 512], F32)
        self.h2tok = d("h2tok", [NT, 1024], BF16)
        self.selT = d("selT", [2, 16, self.SC, self.SP, n], BF16)
        self.yg = d("yg", [2, 16, self.SC, self.SP, 1024], BF16)
        if name == "L":
            self.tiles = [(i * 512, i // 4, (i % 4) * 512) for i in range(8)]
        else:
            self.tiles = [(0, None, 0)]


def build(cfg):
    nlayers = cfg.get("layers", 4)
    dumps = cfg.get("dump", [])
    nc = bass.Bass("TRN2", target_bir_lowering=False)
    fw = FW(nc, n_dma_sems=8)
    fw.psum_banks()
    pe, act, dve, pool = fw.pe, fw.act, fw.dve, fw.pool
    fw.gq = DmaQ("gq", nc.gpsimd, [fw.es.enter_context(nc.semaphore("s_gq%d" % i)) for i in range(3)])
    fw.qs.append(fw.gq)

    def ein(name, shape, dt):
        return nc.dram_tensor(name, list(shape), dt, kind="ExternalInput").ap()

    x_in = ein("x", [2, NL, D], F32)
    ctx_in = ein("ctx", [2, NCX, D], F32)
    cT_in = ein("cT", [128, 8, 3], F32)
    W = {}
    wshapes = dict(
        w_mod=([4, 1024, 6144], F32), b_mod=([4, 128, 48], F32), n1g=([4, 128, 8], F32), n2g=([4, 128, 8], F32),
        fing=([128, 8], F32), w_in=([4, 1024, 5504], F32), qkg=([4, 128, 4], F32), hy_sw=([4, 128, 6, 3], F32),
        hy_sb=([4, 128, 6], F32), hy_w1=([4, 33, 64], F32), hy_w2=([4, 64, 64], F32), hy_w3=([4, 64, 1024], F32),
        hy_b1=([4, 64, 1], F32), hy_b2=([4, 64, 1], F32), hy_fr=([4, 64, 1], F32), hy_bias=([4, 1, 512], F32),
        w_branch=([4, 1024, 1024], F32), w_out=([4, 1024, 1024], F32), w_router=([4, 1024, 16], F32),
        w_gate=([4, 16, 1024, 2048], F32), w_up=([4, 16, 1024, 2048], F32), w_down=([4, 16, 2048, 1024], F32))
    for k, (sh, dt) in wshapes.items():
        W[k] = ein(k, sh, dt)
    C = {}
    for pre, n in (("L_", NL), ("X_", NCX)):
        ncn = n // 128
        w = min(512, n)
        for nm in ("C_st", "Fs_st", "Gs_st"):
            C[pre + nm] = ein(pre + nm, [ncn, 128, ncn, 128], BF16)
        for nm in ("C_mv", "Gs_mv", "Ct_mv", "St_mv"):
            C[pre + nm] = ein(pre + nm, [n // w, 128, ncn, w], BF16)
        C[pre + "featsT"] = ein(pre + "featsT", [33, n], F32)
        C[pre + "dec2"] = ein(pre + "dec2", [128, ncn, 512], F32)
        C[pre + "rs"] = ein(pre + "rs", [128, ncn, 2], F32)
        C[pre + "DcDs"] = ein(pre + "DcDs", [128, 256], BF16)
    C["ropeC"] = ein("ropeC", [128, NL], F32)
    C["ropeS"] = ein("ropeS", [128, NL], F32)
    C["blockones"] = ein("blockones", [128, 128], BF16)
    C["ones_bf"] = ein("ones_bf", [128, 128], BF16)
    C["altcol"] = ein("altcol", [128, 1], BF16)
    C["esel"] = ein("esel", [128, 32 * 128], BF16)
    out = nc.dram_tensor("out", [2, NL, D], F32, kind="ExternalOutput").ap()

    SL = Stream(nc, "L", NL, dumps)
    SX = Stream(nc, "X", NCX, dumps)
    dump_aps = {}

    G = Phase(fw)
    ident_f, r_c = G.sb("ident_f", [128, 128], F32)
    ident_b, _ = G.sb("ident_b", [128, 128], BF16)
    ones_bf, _ = G.sb("ones_bf", [128, 128], BF16)
    blockones, _ = G.sb("blockones", [128, 128], BF16)
    altcol, _ = G.sb("altcol", [128, 1], BF16)
    esel, _ = G.sb("esel", [128, 32 * 128], BF16)
    iota_f, _ = G.sb("iota_f", [128, 256], F32)
    iota_p, _ = G.sb("iota_p", [128, 2], F32)
    mask0, _ = G.sb("mask0", [128, 1], F32)
    zrow, _ = G.sb("zrow", [32, NL], F32)
    bmodT, _ = G.sb("bmodT", [128, 4, 48], F32)
    n1g, _ = G.sb("n1g", [128, 4, 8], F32)
    n2g, _ = G.sb("n2g", [128, 4, 8], F32)
    fing, _ = G.sb("fing", [128, 8], F32)
    qkg, _ = G.sb("qkg", [128, 4, 4], F32)
    hysw, _ = G.sb("hysw", [128, 4, 6, 3], F32)
    hysb, _ = G.sb("hysb", [128, 4, 6], F32)
    scT, r_scT = G.sb("scT", [128, 8, 3], F32)
    mod, r_mod = G.sb("mod", [128, 48, 3], F32)
    A1, _ = G.sb("A1", [128, 8, 3], F32)
    A2, _ = G.sb("A2", [128, 8, 3], F32)
    one3, _ = G.sb("one3", [128, 8, 3], F32)

    def ld(t_ap, res, src):
        fw.dma([], [res], t_ap, src)

    ld(ones_bf[:], r_c, C["ones_bf"][:, :])
    ld(blockones[:], r_c, C["blockones"][:, :])
    ld(altcol[:], r_c, C["altcol"][:, :])
    ld(esel[:], r_c, C["esel"][:, :])
    ld(bmodT[:], r_c, W["b_mod"].rearrange("l p c -> p l c"))
    ld(n1g[:], r_c, W["n1g"].rearrange("l p c -> p l c"))
    ld(n2g[:], r_c, W["n2g"].rearrange("l p c -> p l c"))
    ld(fing[:], r_c, W["fing"][:, :])
    ld(qkg[:], r_c, W["qkg"].rearrange("l p c -> p l c"))
    ld(hysw[:].rearrange("p l c j -> p l (c j)"), r_c, W["hy_sw"].rearrange("l p c j -> p l (c j)"))
    ld(hysb[:], r_c, W["hy_sb"].rearrange("l p c -> p l c"))
    ld(scT[:], r_scT, cT_in[:, :, :])
    fw.op(pool, [], [r_c], "iota", iota_f[:], [[1, 256]], base=0, channel_multiplier=0, allow_small_or_imprecise_dtypes=True)
    fw.op(pool, [], [r_c], "iota", iota_p[:], [[128, 2]], base=0, channel_multiplier=1, allow_small_or_imprecise_dtypes=True)
    fw.op(pool, [], [r_c], "memset", zrow[:], 0.0)
    fw.op(pool, [], [r_c], "memset", one3[:], 1.0)
    fw.op(dve, [r_c], [r_c], "tensor_scalar", ident_f[:], iota_f[:, 0:128], iota_p[:, 0:1], None, ALU.is_equal)
    fw.op(dve, [r_c], [r_c], "tensor_copy", ident_b[:], ident_f[:])
    fw.op(dve, [r_c], [r_c], "tensor_single_scalar", mask0[:], iota_p[:, 0:1], 0.5, ALU.is_gt)
    fw.op(act, [r_scT], [r_scT], "activation", scT[:], scT[:], AF.Silu)
    fw.barrier()

    def evac(i, reads, writes, out_ap, in_ap):
        if i % 2 == 0:
            fw.op(act, reads, writes, "activation", out_ap, in_ap, AF.Copy)
        else:
            fw.op(dve, reads, writes, "tensor_copy", out_ap, in_ap)

    with Phase(fw) as ph:
        xin = ph.buf("xin", [128, 4, 1024], F32, 2)
        xo = ph.buf("xo", [128, 8, 512], F32, 2)
        for S, src in ((SL, x_in), (SX, ctx_in)):
            flat = src.rearrange("b t d -> (b t) d")
            for ti in range(S.NT // 512):
                t, r = xin.get()
                fw.dma([], [r], t[:], flat[ti * 512:(ti + 1) * 512, :].rearrange("(c p) d -> p c d", p=128))
                o, orr = xo.get()
                for dc in range(8):
                    pt, pr = fw.ps()
                    for tcq in range(4):
                        fw.tr([r, r_c], [pr], pt[:, tcq * 128:(tcq + 1) * 128], t[:, tcq, dc * 128:(dc + 1) * 128], ident_f[:])
                    evac(dc, [pr], [orr], o[:, dc, :], pt[:, 0:512])
                fw.dma([orr], [], S.xT.rearrange("(c p) t -> p c t", p=128)[:, :, ti * 512:(ti + 1) * 512], o[:])

    def norm_mod(ph, xt, xr, Asc, Bsc, outs, out_res, width, pools):
        sqp, rsp, tmpp = pools
        sq, sqr = sqp.get()
        fw.op(act, [xr], [sqr], "activation", sq[:, :, 0:width], xt[:, :, 0:width], AF.Square)
        pt, pr = fw.ps()
        fw.mm([sqr, r_c], [pr], pt[:, 0:width], [(ones_bf[:], sq[:, c, 0:width]) for c in range(8)])
        rs, rsr = rsp.get()
        fw.op(act, [pr], [rsr], "activation", rs[:, 0:width], pt[:, 0:width], AF.Sqrt, scale=1.0 / 1024, bias=EPS)
        fw.op(dve, [rsr], [rsr], "reciprocal", rs[:, 0:width], rs[:, 0:width])
        for c in range(8):
            tmp, tr_ = tmpp.get()
            fw.op(dve, [xr, rsr], [tr_], "tensor_tensor", tmp[:, 0:width], xt[:, c, 0:width], rs[:, 0:width], ALU.mult)
            if Bsc is None:
                fw.op(act, [tr_, r_c], [out_res], "activation", outs(c), tmp[:, 0:width], AF.Identity, scale=Asc(c))
            else:
                fw.op(act, [tr_, r_c, r_mod], [out_res], "activation", outs(c), tmp[:, 0:width], AF.Identity,
                      scale=Asc(c), bias=Bsc(c))

    def load_cast(stage, dst_ap, dst_res, src_ap, shape, idx):
        fw.dma([], [dst_res], dst_ap, src_ap, q=fw.gq)

    _, ctypes = col_order()

    def layer(l):
        last = (l == nlayers - 1) and (l == DEPTH - 1)
        streams = [SL] if last else [SX, SL]

        with Phase(fw) as ph:
            wst = ph.buf("wm", [128, 8, 512], F32, 2)
            mrow = ph.buf("mrow", [128, 512], F32, 2)
            scTp, r_sp = ph.sb("scTp", [128, 8, 128], F32)
            fw.op(pool, [], [r_sp], "memset", scTp[:], 0.0)
            fw.op(dve, [r_scT, r_sp], [r_sp], "tensor_copy", scTp[:, :, 0:3], scT[:])
            wm = W["w_mod"][l].rearrange("(c p) f -> p c f", p=128)
            for blk in range(12):
                t, r = wst.get()
                fw.dma([], [r], t[:], wm[:, :, blk * 512:(blk + 1) * 512])
                pt, pr = fw.ps()
                fw.mm([r, r_sp], [pr], pt[:, 0:512], [(scTp[:, dc, :], t[:, dc, :]) for dc in range(8)])
                mr_, mrr = mrow.get()
                evac(blk, [pr], [mrr], mr_[:], pt[:, 0:512])
                p2, pr2 = fw.ps()
                for j in range(4):
                    fw.tr([mrr, r_c], [pr2], p2[:, j * 128:(j + 1) * 128], mr_[:, j * 128:(j + 1) * 128], ident_f[:])
                for col in range(3):
                    fw.op(dve, [pr2, r_c], [r_mod], "tensor_tensor", mod[:, blk * 4:(blk + 1) * 4, col],
                          p2[:, 0:512].rearrange("p (a b) -> p a b", a=4)[:, :, col], bmodT[:, l, blk * 4:(blk + 1) * 4], ALU.add)
            for (Ax, ng, base) in ((A1, n1g, 8), (A2, n2g, 32)):
                fw.op(dve, [r_mod], [r_mod], "tensor_tensor", Ax[:], mod[:, base:base + 8, :], one3[:], ALU.add)
                for col in range(3):
                    fw.op(dve, [r_mod, r_c], [r_mod], "tensor_tensor", Ax[:, :, col], Ax[:, :, col], ng[:, l, :], ALU.mult)

        def mcol(tile):
            return 2 if tile[1] is None else tile[1]

        with Phase(fw) as ph:
            hT, r_hT = ph.sb("hT", [128, 8, 4608], BF16)
            ropeC, _ = ph.sb("ropeC", [128, NL], F32)
            ropeS, _ = ph.sb("ropeS", [128, NL], F32)
            dcds = {}
            for S in (SL, SX):
                dcds[S.name], _ = ph.sb("dcds" + S.name, [128, 256], BF16)
                ld(dcds[S.name][:], r_c, C[S.name + "_DcDs"][:, :])
            ld(ropeC[:], r_c, C["ropeC"][:, :])
            ld(ropeS[:], r_c, C["ropeS"][:, :])
            alltiles = [(SL, tl) for tl in SL.tiles] + [(SX, tl) for tl in SX.tiles]
            hoff = {"L": 0, "X": 4096}
            with Phase(fw) as p1:
                xin = p1.buf("xt", [128, 8, 512], F32, 2)
                pools = (p1.buf("sq", [128, 8, 512], BF16, 1), p1.buf("rs", [128, 512], F32, 2), p1.buf("tmp", [128, 512], F32, 8))
                for S, tl in alltiles:
                    t, r = xin.get()
                    fw.dma([], [r], t[:], S.xT.rearrange("(c p) t -> p c t", p=128)[:, :, tl[0]:tl[0] + 512])
                    col = mcol(tl)
                    c0 = hoff[S.name] + tl[0]
                    norm_mod(p1, t, r, lambda c: A1[:, c, col:col + 1], lambda c: mod[:, c, col:col + 1],
                             lambda c: hT[:, c, c0:c0 + 512], r_hT, 512, pools)
            with Phase(fw) as p2:
                stage = None
                wb = p2.buf("wbf", [128, 8, 512], BF16, 3)
                sqb = p2.buf("sqb", [128, 512], BF16, 2)
                rb = p2.buf("rb", [128, 512], F32, 2)
                t1b = p2.buf("t1b", [128, 512], F32, 2)
                t2b = p2.buf("t2b", [128, 512], F32, 2)
                ob = p2.buf("ob", [128, 512], BF16, 3)
                of = p2.buf("of", [128, 512], F32, 2)
                vb = p2.buf("vb", [128, 4, 128], BF16, 2)
                ub = p2.buf("ub", [128, 4, 256], BF16, 2)
                win = W["w_in"][l].rearrange("(c p) f -> p c f", p=128)
                nchunks = len(ctypes)
                for blk in range((nchunks + 3) // 4):
                    nb = min(4, nchunks - blk * 4)
                    wt, wr = wb.get()
                    load_cast(stage, wt[:, :, 0:nb * 128], wr, win[:, :, blk * 512:blk * 512 + nb * 128], [128, 8, nb * 128], blk)
                    for S, tl in alltiles:
                        c0 = hoff[S.name] + tl[0]
                        isx = S is SX
                        pos0 = tl[2]
                        held = None
                        for j in range(nb):
                            ty, idx = ctypes[blk * 4 + j]
                            if isx and ty in ("qs", "ks"):
                                continue
                            if isx and last and ty not in ("k", "v"):
                                continue
                            wj = lambda dc: wt[:, dc, j * 128:(j + 1) * 128]
                            if ty == "v":
                                vt, vr = vb.get()
                                pt, pr = fw.ps()
                                for tcq in range(4):
                                    fw.mm([wr, r_hT], [pr], pt[:, tcq * 128:(tcq + 1) * 128],
                                          [(hT[:, dc, c0 + tcq * 128:c0 + (tcq + 1) * 128], wj(dc)) for dc in range(8)])
                                evac(0, [pr], [vr], vt[:], pt[:, 0:512].rearrange("p (a b) -> p a b", a=4))
                                fw.dma([vr], [], S.v[tl[0]:tl[0] + 512, :].rearrange("(c p) d -> p c d", p=128), vt[:])
                                continue
                            pt, pr = fw.ps()
                            fw.mm([wr, r_hT], [pr], pt[:, 0:512], [(wj(dc), hT[:, dc, c0:c0 + 512]) for dc in range(8)])
                            if ty in ("q", "k"):
                                gi = 0 if ty == "q" else 2
                                dst = S.qT[idx * 128:(idx + 1) * 128, tl[0]:tl[0] + 512] if ty == "q" else S.kT[:, tl[0]:tl[0] + 512]
                                sq, sqr = sqb.get()
                                fw.op(act, [pr], [sqr], "activation", sq[:], pt[:, 0:512], AF.Square)
                                p2, pr2 = fw.ps()
                                fw.mm([sqr, r_c], [pr2], p2[:, 0:512], [(blockones[:], sq[:])])
                                rt, rr = rb.get()
                                fw.op(act, [pr2], [rr], "activation", rt[:], p2[:, 0:512], AF.Sqrt, scale=1.0 / 64, bias=EPS)
                                fw.op(dve, [rr], [rr], "reciprocal", rt[:], rt[:])
                                if isx:
                                    o, orr = ob.get()
                                    fw.op(dve, [pr, rr, r_c], [orr], "scalar_tensor_tensor", o[:], pt[:, 0:512], qkg[:, l, gi:gi + 1], rt[:], ALU.mult, ALU.mult)
                                    fw.dma([orr], [], dst, o[:])
                                else:
                                    t1, t1r = t1b.get()
                                    fw.op(dve, [pr, r_c], [t1r], "scalar_tensor_tensor", t1[:], pt[:, 0:512], qkg[:, l, gi:gi + 1],
                                          ropeC[:, pos0:pos0 + 512], ALU.mult, ALU.mult)
                                    held = (t1, t1r, rt, rr, dst)
                            elif ty in ("qs", "ks"):
                                gi = 1 if ty == "qs" else 3
                                t1, t1r, rt, rr, dst = held
                                t2, t2r = t2b.get()
                                fw.op(dve, [pr, r_c], [t2r], "scalar_tensor_tensor", t2[:], pt[:, 0:512], qkg[:, l, gi:gi + 1],
                                      ropeS[:, pos0:pos0 + 512], ALU.mult, ALU.mult)
                                fw.op(pool, [t1r, t2r], [t2r], "tensor_tensor", t2[:], t1[:], t2[:], ALU.add)
                                o, orr = ob.get()
                                fw.op(pool, [t2r, rr], [orr], "tensor_tensor", o[:], t2[:], rt[:], ALU.mult)
                                fw.dma([orr], [], dst, o[:])
                            elif ty == "hy":
                                o, orr = of.get()
                                evac(j, [pr], [orr], o[:], pt[:, 0:512])
                                fw.dma([orr], [], S.hyu[idx * 128:(idx + 1) * 128, tl[0]:tl[0] + 512], o[:])
                            elif ty == "g":
                                o, orr = ob.get()
                                fw.op(act, [pr], [orr], "activation", o[:], pt[:, 0:512], AF.Sigmoid)
                                fw.dma([orr], [], S.sg[idx * 128:(idx + 1) * 128, tl[0]:tl[0] + 512], o[:])
                            elif ty == "fn":
                                o, orr = ob.get()
                                evac(j, [pr], [orr], o[:], pt[:, 0:512])
                                ut, ur = ub.get()
                                for half in range(2):
                                    p2, pr2 = fw.ps()
                                    for q2 in range(2):
                                        tcq = half * 2 + q2
                                        fw.mm([orr, r_c], [pr2], p2[:, q2 * 256:(q2 + 1) * 256],
                                              [(o[:, tcq * 128:(tcq + 1) * 128], dcds[S.name][:])])
                                    evac(half, [pr2], [ur], ut[:, half * 2:half * 2 + 2, :], p2[:, 0:512].rearrange("p (a b) -> p a b", a=2))
                                fw.dma([ur], [], S.ucs[tl[0]:tl[0] + 512, idx, :].rearrange("(c p) d -> p c d", p=128), ut[:])

        def attention(S):
            n = S.n
            NK = n + (NCX if S is SL else 0)
            NKC = NK // 128
            QW = S.W
            with Phase(fw) as ph:
                kTs = ph.buf("kTs", [64, 2, NK], BF16, 2)
                vaug = ph.buf("vaug", [128, NKC, 2, 128], BF16, 2)
                for t_ in vaug.t:
                    fw.op(pool, [], [vaug.r[vaug.t.index(t_)]], "memset", t_[:], 1.0)
                qh = ph.buf("qh", [64, n], BF16, 3)
                eb = ph.buf("eb", [128, 512], BF16, 6)
                rcp = ph.buf("rcp", [64, 512], F32, 2)
                ao = ph.buf("ao", [64, 512], BF16, 2)
                fw.ring([0, 1, 2, 3, 6, 7])
                obank = 0
                for b in range(2):
                    kt, kr = kTs.get()
                    vt, vr = vaug.get()
                    fw.dma([], [kr], kt[:, :, 0:n], S.kT[:, b * n:(b + 1) * n].rearrange("(k d) t -> d k t", k=2))
                    for kk in range(2):
                        fw.dma([], [vr], vt[:, 0:n // 128, kk, 0:64],
                               S.v[b * n:(b + 1) * n, kk * 64:(kk + 1) * 64].rearrange("(c p) d -> p c d", p=128))
                    if S is SL:
                        fw.dma([], [kr], kt[:, :, n:NK], SX.kT[:, b * NCX:(b + 1) * NCX].rearrange("(k d) t -> d k t", k=2))
                        for kk in range(2):
                            fw.dma([], [vr], vt[:, n // 128:NKC, kk, 0:64],
                                   SX.v[b * NCX:(b + 1) * NCX, kk * 64:(kk + 1) * 64].rearrange("(c p) d -> p c d", p=128))
                    steps = []
                    for h in range(8):
                        for qi in range(n // QW):
                            for sc in range(NKC):
                                steps.append((h, qi, sc))
                    qcur = {}

                    def get_q(h):
                        if h not in qcur:
                            qt_, qr = qh.get()
                            fw.dma([], [qr], qt_[:], S.qT[h * 64:(h + 1) * 64, b * n:(b + 1) * n])
                            qcur[h] = (qt_, qr)
                        return qcur[h]

                    def issue_s(st):
                        h, qi, sc = st
                        qt_, qr = get_q(h)
                        pt, pr = fw.ps()
                        fw.mm([kr, qr], [pr], pt[:, 0:QW], [(kt[:, h // 4, sc * 128:(sc + 1) * 128], qt_[:, qi * QW:(qi + 1) * QW])])
                        return pt, pr

                    PRE = 3
                    pend = [issue_s(st) for st in steps[:PRE]]
                    po = por = None
                    for i, (h, qi, sc) in enumerate(steps):
                        kvh = h // 4
                        pt, pr = pend.pop(0)
                        if i + PRE < len(steps):
                            pend.append(issue_s(steps[i + PRE]))
                        if sc == 0:
                            po, por = fw.bank(4 + obank % 2)
                            obank += 1
                        e, er = eb.get()
                        fw.op(act, [pr], [er], "activation", e[:, 0:QW], pt[:, 0:QW], AF.Exp, scale=0.125)
                        fw.mm([vr, er], [por], po[:, 0:QW], [(vt[:, sc, kvh, :], e[:, 0:QW])], start=(sc == 0), stop=(sc == NKC - 1))
                        if sc == NKC - 1:
                            rc, rcr = rcp.get()
                            fw.op(dve, [por], [rcr], "reciprocal", rc[:, 0:QW], po[64:128, 0:QW])
                            a, ar = ao.get()
                            fw.op(dve, [por, rcr], [ar], "tensor_tensor", a[:, 0:QW], po[0:64, 0:QW], rc[:, 0:QW], ALU.mult)
                            fw.dma([ar], [], S.mixT[h * 64:(h + 1) * 64, b * n + qi * QW:b * n + (qi + 1) * QW], a[:, 0:QW])
                fw.ring(range(8))

        def hy_spectrum(S):
            n = S.n
            NCn = S.NC
            Wn = S.W
            pre = S.name + "_"
            with Phase(fw) as ph:
                feats, rf = ph.sb("feats", [33, n], F32)
                w1, rw = ph.sb("w1", [33, 64], F32)
                w2, _ = ph.sb("w2", [64, 64], F32)
                w3, _ = ph.sb("w3", [64, 1024], F32)
                b1, _ = ph.sb("b1", [64, 1], F32)
                b2, _ = ph.sb("b2", [64, 1], F32)
                fr, _ = ph.sb("fr", [64, 1], F32)
                dec2, _ = ph.sb("dec2", [128, NCn, 512], F32)
                rs_, _ = ph.sb("rs", [128, NCn, 2], F32)
                dB, _ = ph.sb("dB", [128, 512], F32)
                ld(feats[:], rw, C[pre + "featsT"][:, :])
                ld(w1[:], rw, W["hy_w1"][l])
                ld(w2[:], rw, W["hy_w2"][l])
                ld(w3[:], rw, W["hy_w3"][l])
                ld(b1[:], rw, W["hy_b1"][l])
                ld(b2[:], rw, W["hy_b2"][l])
                ld(fr[:], rw, W["hy_fr"][l])
                ld(dec2[:], rw, C[pre + "dec2"][:, :, :])
                ld(rs_[:], rw, C[pre + "rs"][:, :, :])
                ld(dB[:], rw, W["hy_bias"][l].partition_broadcast(128))
                fw.op(dve, [rw], [rw], "tensor_tensor", b1[:], b1[:], fr[:], ALU.mult)
                fw.op(dve, [rw], [rw], "tensor_tensor", b2[:], b2[:], fr[:], ALU.mult)
                h1, rh1 = ph.sb("h1", [64, n], F32)
                h2, rh2 = ph.sb("h2", [64, n], F32)
                zb = ph.buf("zb", [64, 512], F32, 2)
                mb = ph.buf("mb", [64, 512], F32, 2)

                def sin_layer(wt, K, src, rsrc, bt, dst, rdst):
                    for ti in range(n // Wn):
                        sl = slice(ti * Wn, (ti + 1) * Wn)
                        pt, pr = fw.ps()
                        fw.mm([rw, rsrc], [pr], pt[0:64, 0:Wn], [(wt[0:K, :], src[0:K, sl])])
                        z, zr = zb.get()
                        fw.op(dve, [pr, rw], [zr], "tensor_scalar", z[:, 0:Wn], pt[0:64, 0:Wn], fr[:, 0:1], bt[:, 0:1], ALU.mult, ALU.add)
                        for rep in range(2):
                            m, mr = mb.get()
                            fw.op(dve, [zr], [mr], "tensor_scalar", m[:, 0:Wn], z[:, 0:Wn], PI, -2 * PI, ALU.is_gt, ALU.mult)
                            fw.op(dve, [zr, mr], [zr], "tensor_tensor", z[:, 0:Wn], z[:, 0:Wn], m[:, 0:Wn], ALU.add)
                            m, mr = mb.get()
                            fw.op(dve, [zr], [mr], "tensor_scalar", m[:, 0:Wn], z[:, 0:Wn], -PI, 2 * PI, ALU.is_lt, ALU.mult)
                            fw.op(dve, [zr, mr], [zr], "tensor_tensor", z[:, 0:Wn], z[:, 0:Wn], m[:, 0:Wn], ALU.add)
                        fw.op(act, [zr], [rdst], "activation", dst[:, sl], z[:, 0:Wn], AF.Sin)

                sin_layer(w1, 33, feats, rw, b1, h1, rh1)
                sin_layer(w2, 64, h1, rh1, b2, h2, rh2)
                gsum, rgs = ph.sb("gsum", [128, NCn, 512], BF16)
                gdif, rgd = ph.sb("gdif", [128, NCn, 512], BF16)
                hfb = ph.buf("hf", [128, 512], F32, 2)
                hbb = ph.buf("hb", [128, 512], F32, 2)
                a1b = ph.buf("a1", [128, 512], F32, 2)
                a2b = ph.buf("a2", [128, 512], BF16, 2)
                a3b = ph.buf("a3", [128, 512], F32, 2)
                fw.ring([0, 1, 2, 3, 4, 5])
                pl1, pl1r = fw.bank(7)
                for mc in range(NCn):
                    pa, par = fw.ps()
                    pb, pbr = fw.ps()
                    fw.mm([rh2, rw], [par], pa[:, 0:512], [(h2[:, mc * 128:(mc + 1) * 128], w3[:, 0:512])])
                    fw.mm([rh2, rw], [pbr], pb[:, 0:512], [(h2[:, mc * 128:(mc + 1) * 128], w3[:, 512:1024])])
                    hf, hfr = hfb.get()
                    hb, hbr = hbb.get()
                    fw.op(dve, [par, rw], [hfr], "tensor_tensor", hf[:], pa[:, 0:512], dec2[:, mc, :], ALU.mult)
                    if mc == 0:
                        fw.op(dve, [pbr, rw, r_c], [hbr], "scalar_tensor_tensor", hb[:], pb[:, 0:512], mask0[:, 0:1], dec2[:, mc, :], ALU.mult, ALU.mult)
                    else:
                        fw.op(dve, [pbr, rw], [hbr], "tensor_tensor", hb[:], pb[:, 0:512], dec2[:, mc, :], ALU.mult)
                    fw.op(pool, [hfr, hbr], [rgs], "tensor_tensor", gsum[:, mc, :], hf[:], hb[:], ALU.add)
                    fw.op(pool, [hfr, hbr], [rgd], "tensor_tensor", gdif[:, mc, :], hf[:], hb[:], ALU.subtract)
                    a1, a1r = a1b.get()
                    a2, a2r = a2b.get()
                    a3, a3r = a3b.get()
                    fw.op(act, [hfr], [a1r], "activation", a1[:], hf[:], AF.Abs)
                    fw.op(act, [hbr], [a3r], "activation", a3[:], hb[:], AF.Abs)
                    fw.op(pool, [a1r, a3r], [a2r], "tensor_tensor", a2[:], a1[:], a3[:], ALU.add)
                    fw.mm([a2r, r_c], [pl1r], pl1[:, 0:512], [(ones_bf[:], a2[:])], start=(mc == 0), stop=(mc == NCn - 1))
                il1, ril = ph.sb("il1", [128, 512], F32)
                fw.op(dve, [pl1r], [ril], "reciprocal", il1[:], pl1[:, 0:512])
                cst = ph.buf("cst", [128, NCn, 128], BF16, 2)
                fst = ph.buf("fst", [128, NCn, 128], BF16, 2)
                ab = ph.buf("ab", [128, 512], F32, 2)
                bb = ph.buf("bb", [128, 512], F32, 2)
                for fc in range(NCn):
                    ct, cr = cst.get()
                    ft, fr_ = fst.get()
                    fw.dma([], [cr], ct[:], C[pre + "C_st"][fc])
                    fw.dma([], [fr_], ft[:], C[pre + "Fs_st"][fc])
                    pa, par = fw.ps()
                    pb, pbr = fw.ps()
                    fw.mm([cr, rgs], [par], pa[:, 0:512], [(ct[:, mc, :], gsum[:, mc, :]) for mc in range(NCn)])
                    fw.mm([fr_, rgd], [pbr], pb[:, 0:512], [(ft[:, mc, :], gdif[:, mc, :]) for mc in range(NCn)])
                    a, ar = ab.get()
                    fw.op(dve, [par, ril], [ar], "tensor_tensor", a[:], pa[:, 0:512], il1[:], ALU.mult)
                    fw.op(pool, [ar, rw], [ar], "tensor_tensor", a[:], a[:], dB[:], ALU.add)
                    fw.op(act, [ar, rw], [ar], "activation", a[:], a[:], AF.Identity, scale=rs_[:, fc, 0:1])
                    bt_, btr = bb.get()
                    fw.op(dve, [pbr, ril, rw], [btr], "scalar_tensor_tensor", bt_[:], pb[:, 0:512], rs_[:, fc, 1:2], il1[:], ALU.mult, ALU.mult)
                    fw.dma([ar], [], S.specA[fc * 128:(fc + 1) * 128, :], a[:])
                    fw.dma([btr], [], S.specB[fc * 128:(fc + 1) * 128, :], bt_[:])
                    if fc == 0:
                        pn, pnr = fw.ps()
                        fw.mm([rgs, r_c], [pnr], pn[0:1, 0:512], [(altcol[:, 0:1], gsum[:, mc, :]) for mc in range(NCn)])
                        a2_, a2r_ = ph.sb("a2c0", [128, 512], F32)
                        nq, nqr = ph.sb("nq", [1, 512], F32)
                        fw.op(dve, [pnr, ril], [nqr], "tensor_tensor", nq[:], pn[0:1, 0:512], il1[0:1, :], ALU.mult)
                        fw.op(dve, [nqr, rw], [nqr], "tensor_tensor", nq[:], nq[:], dB[0:1, :], ALU.add)
                        fw.op(dve, [ar], [a2r_], "tensor_copy", a2_[:], a[:])
                        fw.op(dve, [nqr, a2r_], [a2r_], "tensor_single_scalar", a2_[0:1, :], nq[:], 0.5 / n, ALU.mult)
                        fw.dma([a2r_], [], S.specA2[:, :], a2_[:])
                fw.ring(range(8))

        def hyena(S):
            n = S.n
            NCn = S.NC
            Wn = S.W
            pre = S.name + "_"
            with Phase(fw) as ph:
                vtok, rv = ph.sb("vtok", [128, NCn, 512], BF16)
                g1tok, rg1 = ph.sb("g1tok", [128, NCn, 512], BF16)
                g2T, rg2 = ph.sb("g2T", [128, 4, n], F32)
                ztok, rz = ph.sb("ztok", [128, NCn, 512], BF16)
                Y, rY = ph.sb("Y", [128, NCn, 2, 512], BF16)
                with Phase(fw) as p1:
                    ub = p1.buf("u", [128, n], F32, 2)
                    cb = p1.buf("cv", [128, n], F32, 2)
                    for b in range(2):
                        for hc in range(6):
                            u, ur = ub.get()
                            fw.dma([], [ur], u[:], S.hyu[hc * 128:(hc + 1) * 128, b * n:(b + 1) * n])
                            if hc >= 4:
                                orr = rg2
                                ct_ = None
                            else:
                                ct_, orr = cb.get()

                            def osl(a, b_):
                                return g2T[:, b * 2 + (hc - 4), a:b_] if hc >= 4 else ct_[:, a:b_]
                            fw.op(dve, [ur, r_c], [orr], "tensor_scalar", osl(0, n), u[:], hysw[:, l, hc, 1:2], hysb[:, l, hc:hc + 1], ALU.mult, ALU.add)
                            fw.op(dve, [ur, r_c, orr], [orr], "scalar_tensor_tensor", osl(1, n), u[:, 0:n - 1], hysw[:, l, hc, 0:1], osl(1, n), ALU.mult, ALU.add)
                            fw.op(dve, [ur, r_c, orr], [orr], "scalar_tensor_tensor", osl(0, n - 1), u[:, 1:n], hysw[:, l, hc, 2:3], osl(0, n - 1), ALU.mult, ALU.add)
                            if hc < 4:
                                dst, rdst = (vtok, rv) if hc < 2 else (g1tok, rg1)
                                cbase = b * 256 + (hc % 2) * 128
                                nq = min(4, NCn)
                                for t4 in range(NCn // nq):
                                    pt, pr = fw.ps()
                                    for q in range(nq):
                                        tc = t4 * nq + q
                                        fw.tr([orr, r_c], [pr], pt[:, q * 128:(q + 1) * 128], ct_[:, tc * 128:(tc + 1) * 128], ident_f[:])
                                    evac(t4, [pr], [rdst], dst[:, t4 * nq:t4 * nq + nq, cbase:cbase + 128],
                                         pt[:, 0:nq * 128].rearrange("p (a b) -> p a b", a=nq))
                with Phase(fw) as p2:
                    stA = p2.buf("stA", [128, NCn, 128], BF16, 2)
                    stB = p2.buf("stB", [128, NCn, 128], BF16, 2)
                    spA = p2.buf("spA", [128, 2, 256], F32, 2)
                    spB = p2.buf("spB", [128, 2, 256], F32, 2)
                    spA2 = p2.buf("spA2", [128, 2, 256], F32, 1)
                    t1b = p2.buf("t1", [128, 512], F32, 2)
                    t2b = p2.buf("t2", [128, 512], F32, 2)
                    t3b = p2.buf("t3", [128, 512], F32, 2)
                    t4b = p2.buf("t4", [128, 512], F32, 2)

                    def fwd(src, rsrc, o):
                        for fc in range(NCn):
                            ct, cr = stA.get()
                            ft, fr_ = stB.get()
                            fw.dma([], [cr], ct[:], C[pre + "C_st"][fc])
                            fw.dma([], [fr_], ft[:], C[pre + "Fs_st"][fc])
                            at, ar = spA.get()
                            bt_, br = spB.get()
                            for b in range(2):
                                fw.dma([], [ar], at[:, b, :], S.specA[fc * 128:(fc + 1) * 128, o * 256:(o + 1) * 256])
                                fw.dma([], [br], bt_[:, b, :], S.specB[fc * 128:(fc + 1) * 128, o * 256:(o + 1) * 256])
                            if fc == 0:
                                a2t, a2r = spA2.get()
                                for b in range(2):
                                    fw.dma([], [a2r], a2t[:, b, :], S.specA2[:, o * 256:(o + 1) * 256])
                            else:
                                a2t, a2r = at, ar
                            pzr, pzrr = fw.ps()
                            pzi, pzir = fw.ps()
                            fw.mm([cr, rsrc], [pzrr], pzr[:, 0:512], [(ct[:, tc, :], src[:, tc, :]) for tc in range(NCn)])
                            fw.mm([fr_, rsrc], [pzir], pzi[:, 0:512], [(ft[:, tc, :], src[:, tc, :]) for tc in range(NCn)])
                            af = at[:].rearrange("p a b -> p (a b)")
                            bf = bt_[:].rearrange("p a b -> p (a b)")
                            a2f = a2t[:].rearrange("p a b -> p (a b)")
                            t1, r1 = t1b.get()
                            t2, r2 = t2b.get()
                            t3, r3 = t3b.get()
                            t4, r4 = t4b.get()
                            fw.op(dve, [pzrr, ar], [r1], "tensor_tensor", t1[:], pzr[:, 0:512], af, ALU.mult)
                            fw.op(dve, [pzir, br], [r2], "tensor_tensor", t2[:], pzi[:, 0:512], bf, ALU.mult)
                            fw.op(dve, [pzrr, br], [r3], "tensor_tensor", t3[:], pzr[:, 0:512], bf, ALU.mult)
                            fw.op(dve, [pzir, a2r], [r4], "tensor_tensor", t4[:], pzi[:, 0:512], a2f, ALU.mult)
                            fw.op(pool, [r1, r2], [rY], "tensor_tensor", Y[:, fc, 0, :], t1[:], t2[:], ALU.subtract)
                            fw.op(pool, [r3, r4], [rY], "tensor_tensor", Y[:, fc, 1, :], t3[:], t4[:], ALU.add)

                    fwd(vtok, rv, 0)
                    for tc in range(NCn):
                        ct, cr = stA.get()
                        gt, gr = stB.get()
                        fw.dma([], [cr], ct[:], C[pre + "C_st"][tc])
                        fw.dma([], [gr], gt[:], C[pre + "Gs_st"][tc])
                        pz, pzr_ = fw.ps()
                        fw.mm([cr, gr, rY], [pzr_], pz[:, 0:512],
                              [(ct[:, fc, :], Y[:, fc, 0, :]) for fc in range(NCn)] + [(gt[:, fc, :], Y[:, fc, 1, :]) for fc in range(NCn)])
                        fw.op(dve, [pzr_, rg1], [rz], "tensor_tensor", ztok[:, tc, :], pz[:, 0:512], g1tok[:, tc, :], ALU.mult)
                    fwd(ztok, rz, 1)
                with Phase(fw) as p3:
                    mvA = p3.buf("mvA", [128, NCn, Wn], BF16, 1)
                    mvB = p3.buf("mvB", [128, NCn, Wn], BF16, 1)
                    yo = p3.buf("yo", [128, Wn], BF16, 2)
                    for tt in range(n // Wn):
                        ct, cr = mvA.get()
                        gt, gr = mvB.get()
                        fw.dma([], [cr], ct[:], C[pre + "C_mv"][tt])
                        fw.dma([], [gr], gt[:], C[pre + "Gs_mv"][tt])
                        for bcc in range(4):
                            py, pyr = fw.ps()
                            fw.mm([cr, gr, rY], [pyr], py[:, 0:Wn],
                                  [(Y[:, fc, 0, bcc * 128:(bcc + 1) * 128], ct[:, fc, :]) for fc in range(NCn)] +
                                  [(Y[:, fc, 1, bcc * 128:(bcc + 1) * 128], gt[:, fc, :]) for fc in range(NCn)])
                            o, orr = yo.get()
                            fw.op(dve, [pyr, rg2], [orr], "tensor_tensor", o[:], py[:, 0:Wn], g2T[:, bcc, tt * Wn:(tt + 1) * Wn], ALU.mult)
                            b, cc = bcc // 2, bcc % 2
                            fw.dma([orr], [], S.mixT[512 + cc * 128:512 + (cc + 1) * 128, b * n + tt * Wn:b * n + (tt + 1) * Wn], o[:])

        def fnet(S):
            n = S.n
            NCn = S.NC
            Wn = S.W
            pre = S.name + "_"
            with Phase(fw) as ph:
                us, rus = ph.sb("ucs", [128, 2, NCn, 2, 256], BF16)
                for b in range(2):
                    fw.dma([], [rus], us[:, b, :, :, :].rearrange("p c a d -> p c (a d)"), S.ucs[b * n:(b + 1) * n, :, :].rearrange("(c p) a d -> p c (a d)", p=128))
                mvA = ph.buf("fA", [128, NCn, Wn], BF16, 2)
                mvB = ph.buf("fB", [128, NCn, Wn], BF16, 2)
                yo = ph.buf("fyo", [128, Wn], BF16, 2)
                k = 0
                for kt in range(n // Wn):
                    ct, cr = mvA.get()
                    st_, sr = mvB.get()
                    fw.dma([], [cr], ct[:], C[pre + "Ct_mv"][kt])
                    fw.dma([], [sr], st_[:], C[pre + "St_mv"][kt])
                    for b in range(2):
                        for a in range(2):
                            py, pyr = fw.ps()
                            fw.mm([cr, sr, rus], [pyr], py[:, 0:Wn],
                                  [(us[:, b, tc, a, 0:128], ct[:, tc, :]) for tc in range(NCn)] +
                                  [(us[:, b, tc, a, 128:256], st_[:, tc, :]) for tc in range(NCn)])
                            o, orr = yo.get()
                            evac(k, [pyr], [orr], o[:], py[:, 0:Wn])
                            k += 1
                            fw.dma([orr], [], S.mixT[768 + a * 128:768 + (a + 1) * 128, b * n + kt * Wn:b * n + (kt + 1) * Wn], o[:])

        def merge():
            with Phase(fw) as ph:
                stage = None
                wbr, rwb = ph.sb("wbr", [128, 8, 1024], BF16)
                wo, rwo = ph.sb("wo", [128, 8, 1024], BF16)
                for i, (dst, rd, src) in enumerate(((wbr, rwb, W["w_branch"][l]), (wo, rwo, W["w_out"][l]))):
                    sv = src.rearrange("(c p) f -> p c f", p=128)
                    for hlf in range(2):
                        load_cast(stage, dst[:, :, hlf * 512:(hlf + 1) * 512], rd, sv[:, :, hlf * 512:(hlf + 1) * 512], [128, 8, 512], i * 2 + hlf)
                mixb = ph.buf("mixb", [128, 8, 512], BF16, 2)
                sgb = ph.buf("sgb", [128, 24, 512], BF16, 1)
                xb = ph.buf("xb", [128, 8, 512], F32, 2)
                mT = ph.buf("mT", [128, 8, 512], BF16, 1)
                tb = [ph.buf("mt%d" % i, [128, 512], F32, 2) for i in range(3)]
                for S in streams:
                    for tl in S.tiles:
                        sl = slice(tl[0], tl[0] + 512)
                        col = mcol(tl)
                        mx, mxr = mixb.get()
                        sg, sgr = sgb.get()
                        xt, xr = xb.get()
                        m, mr = mT.get()
                        fw.dma([], [mxr], mx[:], S.mixT.rearrange("(c p) t -> p c t", p=128)[:, :, sl])
                        fw.dma([], [sgr], sg[:], S.sg.rearrange("(c p) t -> p c t", p=128)[:, :, sl])
                        fw.dma([], [xr], xt[:], S.xT.rearrange("(c p) t -> p c t", p=128)[:, :, sl])
                        for dc in range(8):
                            ts = []
                            for bi, (k0, k1) in enumerate(((0, 4), (4, 6), (6, 8))):
                                pt, pr = fw.ps()
                                fw.mm([rwb, mxr], [pr], pt[:, 0:512], [(wbr[:, kc, dc * 128:(dc + 1) * 128], mx[:, kc, :]) for kc in range(k0, k1)])
                                t, tr_ = tb[bi].get()
                                fw.op(dve, [pr, sgr], [tr_], "tensor_tensor", t[:], pt[:, 0:512], sg[:, bi * 8 + dc, :], ALU.mult)
                                ts.append((t, tr_))
                            fw.op(pool, [ts[0][1], ts[1][1]], [ts[1][1]], "tensor_tensor", ts[1][0][:], ts[0][0][:], ts[1][0][:], ALU.add)
                            fw.op(pool, [ts[1][1], ts[2][1]], [mr], "tensor_tensor", m[:, dc, :], ts[1][0][:], ts[2][0][:], ALU.add)
                        for dc in range(8):
                            pt, pr = fw.ps()
                            fw.mm([rwo, mr], [pr], pt[:, 0:512], [(wo[:, kc, dc * 128:(dc + 1) * 128], m[:, kc, :]) for kc in range(8)])
                            fw.op(dve, [pr, xr, r_mod], [xr], "scalar_tensor_tensor", xt[:, dc, :], pt[:, 0:512], mod[:, 16 + dc, col:col + 1], xt[:, dc, :], ALU.mult, ALU.add)
                        fw.dma([xr], [], S.xT.rearrange("(c p) t -> p c t", p=128)[:, :, sl], xt[:])

        def moe():
            with Phase(fw) as PH:
                aff = {}
                for S in streams:
                    aff[S.name] = PH.sb("aff" + S.name, [128, S.NC, 2, 16], F32)
                wr_, rwr = PH.sb("wr", [128, 8, 16], F32)
                ld(wr_[:], rwr, W["w_router"][l].rearrange("(c p) e -> p c e", p=128))
                slotT = {}
                slot_tok = {}
                affb = {}
                affTb = {}
                for S in streams:
                    slotT[S.name] = PH.sb("slotT" + S.name, [128, S.n], BF16)
                    affTb[S.name] = PH.sb("affTb" + S.name, [128, S.n], BF16)
                    fw.op(pool, [], [slotT[S.name][1]], "memset", slotT[S.name][0][:], 0.0)
                    fw.op(pool, [], [affTb[S.name][1]], "memset", affTb[S.name][0][:], 0.0)
                    slot_tok[S.name] = PH.sb("slottok" + S.name, [128, S.NC, 32], F32)
                    affb[S.name] = PH.sb("affb" + S.name, [128, S.NC, 2, 16], BF16)
                with Phase(fw) as ph:
                    xin = ph.buf("xt2", [128, 8, 512], F32, 2)
                    h2b = ph.buf("h2T", [128, 8, 512], F32, 2)
                    pools = (ph.buf("sq2", [128, 8, 512], BF16, 1), ph.buf("rs2", [128, 512], F32, 2), ph.buf("tmp2", [128, 512], F32, 8))
                    hto = ph.buf("hto", [128, 4, 1024], BF16, 2)
                    sm = ph.buf("sm", [128, 4], F32, 4)
                    eb = ph.buf("ex", [128, 16], F32, 4)
                    for S in streams:
                        at, ar = aff[S.name]
                        for tl in S.tiles:
                            t, r = xin.get()
                            fw.dma([], [r], t[:], S.xT.rearrange("(c p) t -> p c t", p=128)[:, :, tl[0]:tl[0] + 512])
                            col = mcol(tl)
                            h2, h2r = h2b.get()
                            norm_mod(ph, t, r, lambda c: A2[:, c, col:col + 1], lambda c: mod[:, 24 + c, col:col + 1],
                                     lambda c: h2[:, c, :], h2r, 512, pools)
                            ho, hor = hto.get()
                            for tcq in range(4):
                                tok0 = tl[0] + tcq * 128
                                b = tok0 // S.n
                                tc = (tok0 % S.n) // 128
                                tsl = slice(tcq * 128, (tcq + 1) * 128)
                                pt, pr = fw.ps()
                                fw.mm([h2r, rwr], [pr], pt[:, 0:16], [(h2[:, dc, tsl], wr_[:, dc, :]) for dc in range(8)])
                                s, sr = sm.get()
                                fw.op(dve, [pr], [sr], "tensor_reduce", s[:, 0:1], pt[:, 0:16], AX.X, ALU.max, negate=True)
                                e, er = eb.get()
                                fw.op(act, [pr, sr], [er, sr], "activation", e[:], pt[:, 0:16], AF.Exp, bias=s[:, 0:1], accum_out=s[:, 1:2])
                                fw.op(dve, [sr], [sr], "reciprocal", s[:, 2:3], s[:, 1:2])
                                fw.op(dve, [er, sr], [ar], "tensor_scalar", at[:, tc, b, :], e[:], s[:, 2:3], None, ALU.mult)
                                for hh in range(2):
                                    p2, pr2 = fw.ps()
                                    for q in range(4):
                                        dc = hh * 4 + q
                                        fw.tr([h2r, r_c], [pr2], p2[:, q * 128:(q + 1) * 128], h2[:, dc, tsl], ident_f[:])
                                    evac(hh, [pr2], [hor], ho[:, tcq, hh * 512:(hh + 1) * 512], p2[:, 0:512])
                            fw.dma([hor], [], S.h2tok[tl[0]:tl[0] + 512, :].rearrange("(c p) d -> p c d", p=128), ho[:])
                with Phase(fw) as ph:
                    for S in streams:
                        n = S.n
                        at, ar = aff[S.name]
                        affT, rat = ph.sb("affT", [32, n], F32)
                        wk = ph.buf("wk", [32, n], F32, 2)
                        m8, rm8 = ph.sb("m8", [32, 8], F32)
                        msk, rmsk = ph.sb("msk", [32, n], F32)
                        rnk, rrnk = ph.sb("rnk", [32, n], F32)
                        slf, rslf = ph.sb("slf", [32, n], F32)
                        fw.op(dve, [ar], [affb[S.name][1]], "tensor_copy", affb[S.name][0][:], at[:])
                        for t4 in range(max(1, S.NC // 4)):
                            nq = min(4, S.NC)
                            pt, pr = fw.ps()
                            for q in range(nq):
                                tc = t4 * 4 + q
                                fw.tr([ar, r_c], [pr], pt[0:32, q * 128:(q + 1) * 128], at[:, tc, :, :].rearrange("p a b -> p (a b)"), ident_f[:])
                            evac(t4, [pr], [rat], affT[:, t4 * 512:t4 * 512 + nq * 128], pt[0:32, 0:nq * 128])
                        cur, curr = affT, rat
                        nit = S.cap // 8
                        for it in range(nit):
                            fw.op(dve, [curr], [rm8], "max", m8[:], cur[:])
                            if it < nit - 1:
                                nx, nxr = wk.get()
                                fw.op(dve, [curr, rm8], [nxr], "match_replace", nx[:], m8[:], cur[:], -1e30)
                                cur, curr = nx, nxr
                        fw.op(dve, [rat, rm8], [rmsk], "tensor_scalar", msk[:], affT[:], m8[:, 7:8], None, ALU.is_ge)
                        fw.op(dve, [rmsk, r_c], [rrnk], "tensor_tensor_scan", rnk[:], msk[:], zrow[:, 0:n], 0.0, ALU.add, ALU.add)
                        fw.op(dve, [rrnk], [rslf], "tensor_single_scalar", slf[:], rnk[:], float(S.cap) + 0.5, ALU.is_lt)
                        fw.op(dve, [rslf, rmsk], [rmsk], "tensor_tensor", msk[:], msk[:], slf[:], ALU.mult)
                        fw.op(dve, [rrnk, rmsk], [rslf], "tensor_tensor", slf[:], rnk[:], msk[:], ALU.mult)
                        fw.op(dve, [rslf], [rslf], "tensor_single_scalar", slf[:], slf[:], -1.0, ALU.add)
                        st_, rst = slotT[S.name]
                        fw.op(dve, [rslf], [rst], "tensor_copy", st_[0:32, :], slf[:])
                        fw.op(dve, [rat], [affTb[S.name][1]], "tensor_copy", affTb[S.name][0][0:32, :], affT[:])
                        sk, rsk = slot_tok[S.name]
                        for t4 in range(max(1, S.NC // 4)):
                            nq = min(4, S.NC)
                            pt, pr = fw.ps()
                            for q in range(nq):
                                tc = t4 * 4 + q
                                fw.tr([rslf, r_c], [pr], pt[:, q * 32:(q + 1) * 32], slf[:, tc * 128:(tc + 1) * 128], ident_f[0:32, 0:32])
                            evac(t4, [pr], [rsk], sk[:, t4 * 4:t4 * 4 + nq, :], pt[:, 0:nq * 32].rearrange("p (a b) -> p a b", a=nq))
                with Phase(fw) as ph:
                    stage = None
                    wgb = ph.buf("wg", [128, 8, 512], BF16, 3)
                    wub = ph.buf("wu", [128, 8, 512], BF16, 3)
                    wdb = ph.buf("wd", [128, 16, 512], BF16, 2)
                    sel = {}
                    selT = {}
                    xg = {}
                    actT = {}
                    wsl = {}
                    for S in streams:
                        sel[S.name] = ph.buf("sel" + S.name, [128, S.NC, S.cap], BF16, 1)
                        selT[S.name] = ph.buf("selT" + S.name, [S.SP, S.SC, S.n], BF16, 1)
                        xg[S.name] = ph.buf("xg" + S.name, [128, 8, 2 * S.cap], BF16, 1)
                        actT[S.name] = ph.buf("actT" + S.name, [128, 16, 2 * S.cap], BF16, 1)
                    h2t = ph.buf("h2t", [128, 16, 1024], BF16, 1)
                    abuf = ph.buf("abuf", [128, 512], F32, 2)
                    s0buf = ph.buf("s0buf", [128, 512], BF16, 3)
                    sa = ph.buf("sa", [128, 512], F32, 2)
                    ygb = ph.buf("ygb", [128, 512], BF16, 3)
                    cast_i = 0
                    ev_i = 0
                    for e in range(16):
                        XG = {}
                        AT = {}
                        WS = {}
                        for S in streams:
                            n, cap, SC, SP, NCn = S.n, S.cap, S.SC, S.SP, S.NC
                            xgt, xgr = xg[S.name].get()
                            XG[S.name] = (xgt, xgr)
                            sk, rsk = slot_tok[S.name]
                            st_, rst = slotT[S.name]
                            ab_, rab = affb[S.name]
                            for b in range(2):
                                q = b * 16 + e
                                se, ser = sel[S.name].get()
                                for tc in range(NCn):
                                    fw.op(dve, [rsk, r_c], [ser], "tensor_scalar", se[:, tc, :], iota_f[:, 0:cap], sk[:, tc, q:q + 1], None, ALU.is_equal)
                                sT, sTr = selT[S.name].get()
                                aTb, raT = affTb[S.name]
                                for tt in range(n // S.W):
                                    cs = slice(tt * S.W, (tt + 1) * S.W)
                                    pt, pr = fw.ps()
                                    fw.mm([rst, r_c], [pr], pt[0:SP, 0:S.W], [(esel[:, q * 128:q * 128 + SP], st_[:, cs])])
                                    pa, par = fw.ps()
                                    fw.mm([raT, r_c], [par], pa[0:SP, 0:S.W], [(esel[:, q * 128:q * 128 + SP], aTb[:, cs])])
                                    ab, abr = abuf.get()
                                    fw.op(act, [par], [abr], "activation", ab[0:SP, 0:S.W], pa[0:SP, 0:S.W], AF.Copy)
                                    for sc in range(SC):
                                        s0, s0r = s0buf.get()
                                        fw.op(dve, [pr, r_c], [s0r], "tensor_scalar", s0[0:SP, 0:S.W], pt[0:SP, 0:S.W],
                                              iota_p[0:SP, sc:sc + 1], None, ALU.is_equal)
                                        fw.op(pool, [s0r, abr], [sTr], "tensor_tensor", sT[:, sc, cs], s0[0:SP, 0:S.W], ab[0:SP, 0:S.W], ALU.mult)
                                fw.dma([sTr], [], S.selT[b, e].rearrange("s p t -> p s t"), sT[:])
                                pg = [fw.ps() for _ in range(4)] if cap == 256 else [fw.ps()]
                                ngrp = max(1, NCn // 4)
                                nq = min(4, NCn)
                                ht, hr = h2t.get()
                                for g in range(ngrp):
                                    fw.dma([], [hr], ht[:, g * nq:(g + 1) * nq, :], S.h2tok[b * n + g * 512:b * n + g * 512 + nq * 128, :].rearrange("(c p) d -> p c d", p=128))
                                for dc in range(8):
                                    if cap == 256:
                                        pt, pr = pg[dc // 2]
                                        oap = pt[:, (dc % 2) * 256:(dc % 2 + 1) * 256]
                                    else:
                                        pt, pr = pg[0]
                                        oap = pt[:, dc * cap:(dc + 1) * cap]
                                    fw.mm([hr, ser], [pr], oap, [(ht[:, tc, dc * 128:(dc + 1) * 128], se[:, tc, :]) for tc in range(NCn)])
                                for dc in range(8):
                                    if cap == 256:
                                        pt, pr = pg[dc // 2]
                                        oap = pt[:, (dc % 2) * 256:(dc % 2 + 1) * 256]
                                    else:
                                        pt, pr = pg[0]
                                        oap = pt[:, dc * cap:(dc + 1) * cap]
                                    evac(dc, [pr], [xgr], xgt[:, dc, b * cap:(b + 1) * cap], oap)
                        for S in streams:
                            AT[S.name] = actT[S.name].get()
                        wgs = W["w_gate"][l, e].rearrange("(c p) f -> p c f", p=128)
                        wus = W["w_up"][l, e].rearrange("(c p) f -> p c f", p=128)
                        for ft in range(4):
                            wg, wgr = wgb.get()
                            wu, wur = wub.get()
                            load_cast(stage, wg[:], wgr, wgs[:, :, ft * 512:(ft + 1) * 512], [128, 8, 512], cast_i); cast_i += 1
                            load_cast(stage, wu[:], wur, wus[:, :, ft * 512:(ft + 1) * 512], [128, 8, 512], cast_i); cast_i += 1
                            for S in streams:
                                N2 = 2 * S.cap
                                xgt, xgr = XG[S.name]
                                att, atr = AT[S.name]
                                for fq in range(4):
                                    fcx = ft * 4 + fq
                                    pa, par = fw.ps()
                                    pu, pur = fw.ps()
                                    fw.mm([wgr, xgr], [par], pa[:, 0:N2], [(wg[:, dc, fq * 128:(fq + 1) * 128], xgt[:, dc, :]) for dc in range(8)])
                                    fw.mm([wur, xgr], [pur], pu[:, 0:N2], [(wu[:, dc, fq * 128:(fq + 1) * 128], xgt[:, dc, :]) for dc in range(8)])
                                    s, sr = sa.get()
                                    fw.op(act, [par], [sr], "activation", s[:, 0:N2], pa[:, 0:N2], AF.Silu)
                                    fw.op(dve, [pur, sr], [atr], "tensor_tensor", att[:, fcx, :], pu[:, 0:N2], s[:, 0:N2], ALU.mult)
                        wds = W["w_down"][l, e].rearrange("(c p) d -> p c d", p=128)
                        for dh in range(2):
                            wd, wdr = wdb.get()
                            for hf in range(2):
                                load_cast(stage, wd[:, hf * 8:(hf + 1) * 8, :], wdr, wds[:, hf * 8:(hf + 1) * 8, dh * 512:(dh + 1) * 512], [128, 8, 512], cast_i); cast_i += 1
                            for S in streams:
                                att, atr = AT[S.name]
                                cap, SC, SP = S.cap, S.SC, S.SP
                                if S is SX:
                                    pt, pr = fw.ps()
                                    fw.mm([wdr, atr], [pr], pt[0:64, 0:512], [(att[:, fcx, 0:64], wd[:, fcx, :]) for fcx in range(16)])
                                    y, yr = ygb.get()
                                    evac(ev_i, [pr], [yr], y[0:64, :], pt[0:64, 0:512]); ev_i += 1
                                    for b in range(2):
                                        fw.dma([yr], [], S.yg[b, e, 0, :, dh * 512:(dh + 1) * 512], y[b * 32:(b + 1) * 32, :])
                                    continue
                                for b in range(2):
                                    for sc in range(SC):
                                        c0 = b * cap + sc * SP
                                        pt, pr = fw.ps()
                                        fw.mm([wdr, atr], [pr], pt[0:SP, 0:512], [(att[:, fcx, c0:c0 + SP], wd[:, fcx, :]) for fcx in range(16)])
                                        y, yr = ygb.get()
                                        evac(ev_i, [pr], [yr], y[0:SP, :], pt[0:SP, 0:512]); ev_i += 1
                                        fw.dma([yr], [], S.yg[b, e, sc, :, dh * 512:(dh + 1) * 512], y[0:SP, :])
                for S in streams:
                    n, SC, SP, Wn = S.n, S.SC, S.SP, S.W
                    with Phase(fw) as ph:
                        yga = ph.buf("yga", [SP, 16, SC, 1024], BF16, 1)
                        stl = ph.buf("stl", [SP, 16, SC, Wn], BF16, 1 if S is SL else 2)
                        xb = ph.buf("xc", [128, 8, Wn], F32, 2)
                        for b in range(2):
                            yt, yr = yga.get()
                            for e4 in range(4):
                                fw.dma([], [yr], yt[:, e4 * 4:(e4 + 1) * 4, :, :].rearrange("p e s d -> p (e s) d"), S.yg[b, e4 * 4:(e4 + 1) * 4].rearrange("e s p d -> p (e s) d"))
                            col = 2 if S is SX else b
                            for tt in range(n // Wn):
                                sl = slice(b * n + tt * Wn, b * n + (tt + 1) * Wn)
                                st_, sr = stl.get()
                                for e4 in range(4):
                                    fw.dma([], [sr], st_[:, e4 * 4:(e4 + 1) * 4, :, :].rearrange("p e s t -> p (e s) t"),
                                           S.selT[b, e4 * 4:(e4 + 1) * 4, :, :, tt * Wn:(tt + 1) * Wn].rearrange("e s p t -> p (e s) t"))
                                xt, xr = xb.get()
                                fw.dma([], [xr], xt[:], S.xT.rearrange("(c p) t -> p c t", p=128)[:, :, sl])
                                for dc in range(8):
                                    pt, pr = fw.ps()
                                    fw.mm([yr, sr], [pr], pt[:, 0:Wn],
                                          [(yt[:, e, sc, dc * 128:(dc + 1) * 128], st_[:, e, sc, :]) for e in range(16) for sc in range(SC)])
                                    fw.op(dve, [pr, xr, r_mod], [xr], "scalar_tensor_tensor", xt[:, dc, :], pt[:, 0:Wn], mod[:, 40 + dc, col:col + 1], xt[:, dc, :], ALU.mult, ALU.add)
                                fw.dma([xr], [], S.xT.rearrange("(c p) t -> p c t", p=128)[:, :, sl], xt[:])

        if not last:
            attention(SX)
        attention(SL)
        for S in streams:
            hy_spectrum(S)
            hyena(S)
            fnet(S)
        merge()
        moe()

    for l in range(nlayers):
        layer(l)

    with Phase(fw) as ph:
        xin = ph.buf("xf", [128, 8, 512], F32, 2)
        xnb = ph.buf("xn", [128, 8, 512], F32, 2)
        pools = (ph.buf("sqf", [128, 8, 512], BF16, 1), ph.buf("rsf", [128, 512], F32, 2), ph.buf("tmpf", [128, 512], F32, 8))
        ot = ph.buf("ot", [128, 4, 1024], F32, 2)
        oflat = out.rearrange("b t d -> (b t) d")
        for tl in SL.tiles:
            t, r = xin.get()
            fw.dma([], [r], t[:], SL.xT.rearrange("(c p) t -> p c t", p=128)[:, :, tl[0]:tl[0] + 512])
            xn, xnr = xnb.get()
            norm_mod(ph, t, r, lambda c: fing[:, c:c + 1], None, lambda c: xn[:, c, :], xnr, 512, pools)
            o, orr = ot.get()
            k = 0
            for tcq in range(4):
                for hh in range(2):
                    pt, pr = fw.ps()
                    for q in range(4):
                        dc = hh * 4 + q
                        fw.tr([xnr, r_c], [pr], pt[:, q * 128:(q + 1) * 128], xn[:, dc, tcq * 128:(tcq + 1) * 128], ident_f[:])
                    evac(k, [pr], [orr], o[:, tcq, hh * 512:(hh + 1) * 512], pt[:, 0:512])
                    k += 1
            fw.dma([orr], [], oflat[tl[0]:tl[0] + 512, :].rearrange("(c p) d -> p c d", p=128), o[:])
    fw.barrier()
    G.es.close()
    fw.close()
    print("built: n_ins", fw.n_ins, "n_wait", fw.n_wait)
    return nc

from concourse.bass_utils import run_bass_kernel_spmd


def kernel(**inputs):
    inp = {k: np.asarray(v) for k, v in inputs.items()}
    nc = build({"layers": DEPTH})
    shared = {}
    shared.update(shared_consts())
    shared.update(prep_weights(inp))
    in_maps = []
    for core in range(8):
        m = dict(shared)
        m.update(prep_core(inp, core))
        in_maps.append(m)
    res = run_bass_kernel_spmd(nc, in_maps, core_ids=list(range(8)))
    outs = [np.asarray(r["out"], dtype=np.float32) for r in res.results]
    return np.concatenate(outs, axis=0)
```

```python
import contextlib
import numpy as np
import concourse.bass as bass
import concourse.mybir as mybir

F32 = mybir.dt.float32
BF16 = mybir.dt.bfloat16
U32 = mybir.dt.uint32
I32 = mybir.dt.int32
ALU = mybir.AluOpType
AF = mybir.ActivationFunctionType
AX = mybir.AxisListType


class Res:
    __slots__ = ("w", "r")

    def __init__(self):
        self.w = {}
        self.r = {}


class Eng:
    def __init__(self, name, h, sem):
        self.name = name
        self.h = h
        self.sem = sem
        self.cnt = 0
        self.seen = {}


class DmaQ:
    def __init__(self, name, h, sems):
        self.name = name
        self.h = h
        self.sems = sems
        self.j = 0
        self.seen = {}


class FW:
    def __init__(self, nc, n_dma_sems=8):
        self.nc = nc
        self.es = contextlib.ExitStack()
        mk = lambda n: self.es.enter_context(nc.semaphore(n))
        self.pe = Eng("pe", nc.tensor, mk("s_pe"))
        self.act = Eng("act", nc.scalar, mk("s_act"))
        self.dve = Eng("dve", nc.vector, mk("s_dve"))
        self.pool = Eng("pool", nc.gpsimd, mk("s_pool"))
        self.engs = [self.pe, self.act, self.dve, self.pool]
        self.sp = DmaQ("sp", nc.sync, [mk("s_sp%d" % i) for i in range(n_dma_sems)])
        self.qs = [self.sp]
        self.semid = {}
        self.n_wait = 0
        self.n_ins = 0
        self._psum = []
        self._psum_i = 0
        self._ring = list(range(8))

    def sbuf(self, name, shape, dtype):
        return self.es.enter_context(self.nc.sbuf_tensor(name, list(shape), dtype))

    def psum_banks(self):
        for i in range(8):
            t = self.es.enter_context(self.nc.psum_tensor("ps%d" % i, [128, 512], F32))
            self._psum.append((t, Res()))

    def ps(self):
        ring = self._ring
        t, r = self._psum[ring[self._psum_i % len(ring)]]
        self._psum_i += 1
        return t, r

    def ring(self, banks):
        self._ring = list(banks)

    def bank(self, i):
        return self._psum[i]

    def tr(self, reads, writes, out, in_, ident):
        e = self.pe
        self._wait(e, self._deps(reads, writes))
        ins = e.h.transpose(out, in_, ident)
        e.cnt += 1
        ins.then_inc(e.sem, 1)
        self._mark(reads, writes, self._key(e.sem), e.cnt)
        self.n_ins += 1

    def close(self):
        self.es.close()

    def _key(self, sem):
        k = id(sem)
        self.semid[k] = sem
        return k

    def _deps(self, reads, writes):
        d = {}
        for r in reads:
            for k, v in r.w.items():
                if d.get(k, 0) < v:
                    d[k] = v
        for w in writes:
            for k, v in w.w.items():
                if d.get(k, 0) < v:
                    d[k] = v
            for k, v in w.r.items():
                if d.get(k, 0) < v:
                    d[k] = v
        return d

    def _wait(self, e, deps):
        for k, v in deps.items():
            if e is self.pe and self.semid[k] is e.sem:
                continue
            if e.seen.get(k, 0) < v:
                e.h.wait_ge(self.semid[k], v)
                e.seen[k] = v
                self.n_wait += 1

    def _mark(self, reads, writes, k, v):
        for r in reads:
            if r.r.get(k, 0) < v:
                r.r[k] = v
        for w in writes:
            w.w = {k: v}
            w.r = {}

    def op(self, e, reads, writes, name, *a, **kw):
        self._wait(e, self._deps(reads, writes))
        ins = getattr(e.h, name)(*a, **kw)
        e.cnt += 1
        ins.then_inc(e.sem, 1)
        k = self._key(e.sem)
        self._mark(reads, writes, k, e.cnt)
        self.n_ins += 1
        return ins

    def mm(self, reads, writes, out, pairs, start=True, stop=True):
        e = self.pe
        self._wait(e, self._deps(reads, writes))
        ins = None
        n = len(pairs)
        for i, (a, b) in enumerate(pairs):
            ins = e.h.matmul(out, a, b, start=(start and i == 0), stop=(stop and i == n - 1))
            self.n_ins += 1
        e.cnt += 1
        ins.then_inc(e.sem, 1)
        k = self._key(e.sem)
        self._mark(reads, writes, k, e.cnt)

    def dma(self, reads, writes, out, in_, q=None, **kw):
        q = q or self.sp
        deps = self._deps(reads, writes)
        n = len(q.sems)
        slot = q.j % n
        sem = q.sems[slot]
        k = self._key(sem)
        prev = 16 * (q.j // n)
        if prev > 0:
            deps[k] = max(deps.get(k, 0), prev)
        self._wait(q, deps)
        q.h.dma_start(out=out, in_=in_, **kw).then_inc(sem, 16)
        q.j += 1
        self._mark(reads, writes, k, prev + 16)
        self.n_ins += 1

    def barrier(self):
        d = {}
        for e in self.engs:
            if e.cnt:
                d[self._key(e.sem)] = e.cnt
        for q in self.qs:
            n = len(q.sems)
            for s in range(n):
                uses = (q.j - s + n - 1) // n
                if uses > 0:
                    d[self._key(q.sems[s])] = 16 * uses
        for e in self.engs + self.qs:
            self._wait(e, d)

import math
import numpy as np
import ml_dtypes

BF = ml_dtypes.bfloat16
D = 1024
DEPTH = 4
NL = 2048
NCX = 256
HD = 64


def col_order():
    cols = []
    types = []
    sw = lambda base, n: [base + (i ^ 1) for i in range(n)]
    for hp in range(4):
        b = hp * 128
        cols += list(range(b, b + 128)); types.append(("q", hp))
        cols += sw(b, 128); types.append(("qs", hp))
    cols += list(range(512, 640)); types.append(("k", 0))
    cols += sw(512, 128); types.append(("ks", 0))
    cols += list(range(640, 768)); types.append(("v", 0))
    for i in range(2):
        cols += list(range(1536 + i * 128, 1536 + (i + 1) * 128)); types.append(("fn", i))
    for i in range(6):
        cols += list(range(768 + i * 128, 768 + (i + 1) * 128)); types.append(("hy", i))
    for i in range(24):
        cols += list(range(1792 + i * 128, 1792 + (i + 1) * 128)); types.append(("g", i))
    return np.array(cols), types


def pvec(v, nch):
    return np.ascontiguousarray(v.reshape(nch, 128).T)


def tile_stationary(M):
    R, C = M.shape
    return np.ascontiguousarray(M.reshape(R // 128, 128, C // 128, 128).transpose(2, 1, 0, 3))


def tile_moving(M, w):
    R, C = M.shape
    return np.ascontiguousarray(M.reshape(R // 128, 128, C // w, w).transpose(2, 1, 0, 3))


def dft_consts(n, pre):
    a = np.arange(n, dtype=np.float64)
    ang = np.pi * np.outer(a, a) / n
    Cm = np.cos(ang)
    Fs = -np.sin(ang)
    Fs[:, 0] = (-1.0) ** a
    Gs = Fs.T.copy()
    ang2 = 2.0 * ang
    Ct = np.cos(ang2)
    Stn = -np.sin(ang2)
    w = min(512, n)
    c = {}
    c[pre + "C_st"] = tile_stationary(Cm).astype(BF)
    c[pre + "C_mv"] = tile_moving(Cm, w).astype(BF)
    c[pre + "Fs_st"] = tile_stationary(Fs).astype(BF)
    c[pre + "Gs_st"] = tile_stationary(Gs).astype(BF)
    c[pre + "Gs_mv"] = tile_moving(Gs, w).astype(BF)
    c[pre + "Ct_mv"] = tile_moving(Ct, w).astype(BF)
    c[pre + "St_mv"] = tile_moving(Stn, w).astype(BF)
    t = a / (n - 1)
    bands = np.linspace(1e-4, 15.0, 16)
    an = (2.0 * math.pi / n) * np.outer(a, bands)
    feats = np.concatenate([t[:, None], np.cos(an), -np.sin(an)], axis=-1)
    c[pre + "featsT"] = np.ascontiguousarray(feats.T).astype(np.float32)
    deltas = np.abs(np.linspace(math.log(1e-2) / 1.5, math.log(1e-2) / 0.3, 256))
    dec = np.exp(-t[:, None] * deltas[None, :])
    dec2 = np.concatenate([dec, dec], axis=1)
    c[pre + "dec2"] = np.ascontiguousarray(dec2.reshape(n // 128, 128, 512).transpose(1, 0, 2)).astype(np.float32)
    rs = np.zeros((n, 2))
    rs[:, 0] = 1.0 / n
    rs[0, 0] = 0.5 / n
    rs[:, 1] = 1.0 / n
    rs[0, 1] = 0.0
    c[pre + "rs"] = np.ascontiguousarray(rs.reshape(n // 128, 128, 2).transpose(1, 0, 2)).astype(np.float32)
    return c


def shared_consts():
    c = {}
    c.update(dft_consts(NL, "L_"))
    c.update(dft_consts(NCX, "X_"))
    tt = np.arange(NL)
    row = (tt // 64).astype(np.float64)
    col = (tt % 64).astype(np.float64)
    inv = 10000.0 ** (-np.arange(0, 32, 2, dtype=np.float64) / 32)
    ang = np.concatenate([row[:, None] * inv, col[:, None] * inv], axis=-1)
    p = np.arange(128)
    dim = p % 64
    i = dim // 2
    sgn = np.where(dim % 2 == 0, -1.0, 1.0)
    c["ropeC"] = np.cos(ang)[:, i].T.astype(np.float32).copy()
    c["ropeS"] = (np.sin(ang)[:, i] * sgn[None, :]).T.astype(np.float32).copy()
    cc = np.arange(64)
    th = 2 * np.pi * np.outer(cc, cc) / 64
    for pre, n in (("L_", NL), ("X_", NCX)):
        sc = 1.0 / math.sqrt(n * 64)
        Dc = np.zeros((128, 128)); Ds = np.zeros((128, 128))
        for g in range(2):
            Dc[g * 64:(g + 1) * 64, g * 64:(g + 1) * 64] = np.cos(th) * sc
            Ds[g * 64:(g + 1) * 64, g * 64:(g + 1) * 64] = np.sin(th) * sc
        c[pre + "DcDs"] = np.concatenate([Dc, Ds], axis=1).astype(BF)
    bo = np.zeros((128, 128)); bo[:64, :64] = 1; bo[64:, 64:] = 1
    c["blockones"] = bo.astype(BF)
    c["ones_bf"] = np.ones((128, 128)).astype(BF)
    alt = ((-1.0) ** np.arange(128))[:, None]
    c["altcol"] = alt.astype(BF)
    es = np.zeros((128, 32, 128))
    for q in range(32):
        es[q, q, :] = 1
    c["esel"] = es.reshape(128, 32 * 128).astype(BF)
    return c


def prep_weights(inp):
    w = {}
    cols, _ = col_order()
    w["w_mod"] = np.ascontiguousarray(inp["w_mod"])
    w["b_mod"] = np.stack([pvec(inp["b_mod"][l], 48) for l in range(DEPTH)])
    w["n1g"] = np.stack([pvec(inp["norm1_g"][l], 8) for l in range(DEPTH)])
    w["n2g"] = np.stack([pvec(inp["norm2_g"][l], 8) for l in range(DEPTH)])
    w["fing"] = pvec(inp["final_g"], 8)
    w["w_in"] = np.ascontiguousarray(inp["w_in"][:, :, cols])
    p = np.arange(128) % 64
    qg = inp["q_gain"]; kg = inp["k_gain"]
    w["qkg"] = np.ascontiguousarray(np.stack([qg[:, p], qg[:, p ^ 1], kg[:, p], kg[:, p ^ 1]], axis=-1))
    w["hy_sw"] = np.ascontiguousarray(inp["hy_short_w"].reshape(DEPTH, 3, 6, 128).transpose(0, 3, 2, 1))
    w["hy_sb"] = np.stack([pvec(inp["hy_short_b"][l], 6) for l in range(DEPTH)])
    w["hy_w1"] = np.ascontiguousarray(inp["hy_f_w1"])
    w["hy_w2"] = np.ascontiguousarray(inp["hy_f_w2"])
    w["hy_w3"] = np.ascontiguousarray(inp["hy_f_w3"])
    w["hy_b1"] = np.ascontiguousarray(inp["hy_f_b1"][:, :, None])
    w["hy_b2"] = np.ascontiguousarray(inp["hy_f_b2"][:, :, None])
    w["hy_fr"] = np.ascontiguousarray(inp["hy_f_freq"][:, :, None])
    w["hy_bias"] = np.ascontiguousarray(inp["hy_bias"].reshape(DEPTH, 1, 512))
    w["w_branch"] = np.ascontiguousarray(inp["w_branch"])
    w["w_out"] = np.ascontiguousarray(inp["w_out"])
    w["w_router"] = np.ascontiguousarray(inp["w_router"])
    w["w_gate"] = np.ascontiguousarray(inp["w_gate"])
    w["w_up"] = np.ascontiguousarray(inp["w_up"])
    w["w_down"] = np.ascontiguousarray(inp["w_down"])
    return w


def prep_core(inp, core):
    b0 = 2 * core
    m = {}
    m["x"] = np.ascontiguousarray(inp["x"][b0:b0 + 2])
    m["ctx"] = np.ascontiguousarray(inp["ctx"][b0:b0 + 2])
    c3 = np.stack([inp["c"][b0], inp["c"][b0 + 1], inp["c_ctx"]], axis=-1)
    m["cT"] = np.ascontiguousarray(c3.reshape(8, 128, 3).transpose(1, 0, 2))
    return m

import contextlib
import math
import numpy as np

EPS = 1e-6
PI = math.pi


class Buf:
    def __init__(self, ph, name, shape, dt, n=2):
        self.t = []
        self.r = []
        for i in range(n):
            t, r = ph.sb("%s_%d" % (name, i), shape, dt)
            self.t.append(t)
            self.r.append(r)
        self.i = 0

    def get(self):
        i = self.i % len(self.t)
        self.i += 1
        return self.t[i], self.r[i]


class Phase:
    cnt = 0

    def __init__(self, fw):
        self.fw = fw
        self.es = contextlib.ExitStack()

    def __enter__(self):
        return self

    def __exit__(self, *a):
        self.fw.barrier()
        self.es.close()
        return False

    def sb(self, name, shape, dt):
        Phase.cnt += 1
        t = self.es.enter_context(self.fw.nc.sbuf_tensor("%s_%d" % (name, Phase.cnt), list(shape), dt))
        return t, Res()

    def buf(self, name, shape, dt, n=2):
        return Buf(self, name, shape, dt, n)


class Stream:
    def __init__(self, nc, name, n, dumps=()):
        self.name = name
        self.n = n
        self.NT = 2 * n
        self.NC = n // 128
        self.W = min(512, n)
        self.cap = 2 * n // 16
        self.SC = (self.cap + 127) // 128
        self.SP = min(self.cap, 128)
        def d(nm, sh, dt):
            full = "%s_%s" % (name, nm)
            if full in dumps:
                return nc.dram_tensor(full, list(sh), dt, kind="ExternalOutput").ap()
            return nc.dram_tensor(full, list(sh), dt).ap()
        NT = self.NT
        self.xT = d("xT", [1024, NT], F32)
        self.qT = d("qT", [512, NT], BF16)
        self.kT = d("kT", [128, NT], BF16)
        self.v = d("v", [NT, 128], BF16)
        self.hyu = d("hyu", [768, NT], F32)
        self.ucs = d("ucs", [NT, 2, 256], BF16)
        self.sg = d("sg", [3072, NT], BF16)
        self.mixT = d("mixT", [1024, NT], BF16)
        self.specA = d("specA", [n, 512], F32)
        self.specB = d("specB", [n, 512], F32)
        self.specA2 = d("specA2", [128, 512], F32)
        self.h2tok = d("h2tok", [NT, 1024], BF16)
        self.selT = d("selT", [2, 16, self.SC, self.SP, n], BF16)
        self.yg = d("yg", [2, 16, self.SC, self.SP, 1024], BF16)
        if name == "L":
            self.tiles = [(i * 512, i // 4, (i % 4) * 512) for i in range(8)]
        else:
            self.tiles = [(0, None, 0)]


def build(cfg):
    nlayers = cfg.get("layers", 4)
    dumps = cfg.get("dump", [])
    nc = bass.Bass("TRN2", target_bir_lowering=False)
    fw = FW(nc, n_dma_sems=8)
    fw.psum_banks()
    pe, act, dve, pool = fw.pe, fw.act, fw.dve, fw.pool
    fw.gq = DmaQ("gq", nc.gpsimd, [fw.es.enter_context(nc.semaphore("s_gq%d" % i)) for i in range(3)])
    fw.qs.append(fw.gq)

    def ein(name, shape, dt):
        return nc.dram_tensor(name, list(shape), dt, kind="ExternalInput").ap()

    x_in = ein("x", [2, NL, D], F32)
    ctx_in = ein("ctx", [2, NCX, D], F32)
    cT_in = ein("cT", [128, 8, 3], F32)
    W = {}
    wshapes = dict(
        w_mod=([4, 1024, 6144], F32), b_mod=([4, 128, 48], F32), n1g=([4, 128, 8], F32), n2g=([4, 128, 8], F32),
        fing=([128, 8], F32), w_in=([4, 1024, 5504], F32), qkg=([4, 128, 4], F32), hy_sw=([4, 128, 6, 3], F32),
        hy_sb=([4, 128, 6], F32), hy_w1=([4, 33, 64], F32), hy_w2=([4, 64, 64], F32), hy_w3=([4, 64, 1024], F32),
        hy_b1=([4, 64, 1], F32), hy_b2=([4, 64, 1], F32), hy_fr=([4, 64, 1], F32), hy_bias=([4, 1, 512], F32),
        w_branch=([4, 1024, 1024], F32), w_out=([4, 1024, 1024], F32), w_router=([4, 1024, 16], F32),
        w_gate=([4, 16, 1024, 2048], F32), w_up=([4, 16, 1024, 2048], F32), w_down=([4, 16, 2048, 1024], F32))
    for k, (sh, dt) in wshapes.items():
        W[k] = ein(k, sh, dt)
    C = {}
    for pre, n in (("L_", NL), ("X_", NCX)):
        ncn = n // 128
        w = min(512, n)
        for nm in ("C_st", "Fs_st", "Gs_st"):
            C[pre + nm] = ein(pre + nm, [ncn, 128, ncn, 128], BF16)
        for nm in ("C_mv", "Gs_mv", "Ct_mv", "St_mv"):
            C[pre + nm] = ein(pre + nm, [n // w, 128, ncn, w], BF16)
        C[pre + "featsT"] = ein(pre + "featsT", [33, n], F32)
        C[pre + "dec2"] = ein(pre + "dec2", [128, ncn, 512], F32)
        C[pre + "rs"] = ein(pre + "rs", [128, ncn, 2], F32)
        C[pre + "DcDs"] = ein(pre + "DcDs", [128, 256], BF16)
    C["ropeC"] = ein("ropeC", [128, NL], F32)
    C["ropeS"] = ein("ropeS", [128, NL], F32)
    C["blockones"] = ein("blockones", [128, 128], BF16)
    C["ones_bf"] = ein("ones_bf", [128, 128], BF16)
    C["altcol"] = ein("altcol", [128, 1], BF16)
    C["esel"] = ein("esel", [128, 32 * 128], BF16)
    out = nc.dram_tensor("out", [2, NL, D], F32, kind="ExternalOutput").ap()

    SL = Stream(nc, "L", NL, dumps)
    SX = Stream(nc, "X", NCX, dumps)
    dump_aps = {}

    G = Phase(fw)
    ident_f, r_c = G.sb("ident_f", [128, 128], F32)
    ident_b, _ = G.sb("ident_b", [128, 128], BF16)
    ones_bf, _ = G.sb("ones_bf", [128, 128], BF16)
    blockones, _ = G.sb("blockones", [128, 128], BF16)
    altcol, _ = G.sb("altcol", [128, 1], BF16)
    esel, _ = G.sb("esel", [128, 32 * 128], BF16)
    iota_f, _ = G.sb("iota_f", [128, 256], F32)
    iota_p, _ = G.sb("iota_p", [128, 2], F32)
    mask0, _ = G.sb("mask0", [128, 1], F32)
    zrow, _ = G.sb("zrow", [32, NL], F32)
    bmodT, _ = G.sb("bmodT", [128, 4, 48], F32)
    n1g, _ = G.sb("n1g", [128, 4, 8], F32)
    n2g, _ = G.sb("n2g", [128, 4, 8], F32)
    fing, _ = G.sb("fing", [128, 8], F32)
    qkg, _ = G.sb("qkg", [128, 4, 4], F32)
    hysw, _ = G.sb("hysw", [128, 4, 6, 3], F32)
    hysb, _ = G.sb("hysb", [128, 4, 6], F32)
    scT, r_scT = G.sb("scT", [128, 8, 3], F32)
    mod, r_mod = G.sb("mod", [128, 48, 3], F32)
    A1, _ = G.sb("A1", [128, 8, 3], F32)
    A2, _ = G.sb("A2", [128, 8, 3], F32)
    one3, _ = G.sb("one3", [128, 8, 3], F32)

    def ld(t_ap, res, src):
        fw.dma([], [res], t_ap, src)

    ld(ones_bf[:], r_c, C["ones_bf"][:, :])
    ld(blockones[:], r_c, C["blockones"][:, :])
    ld(altcol[:], r_c, C["altcol"][:, :])
    ld(esel[:], r_c, C["esel"][:, :])
    ld(bmodT[:], r_c, W["b_mod"].rearrange("l p c -> p l c"))
    ld(n1g[:], r_c, W["n1g"].rearrange("l p c -> p l c"))
    ld(n2g[:], r_c, W["n2g"].rearrange("l p c -> p l c"))
    ld(fing[:], r_c, W["fing"][:, :])
    ld(qkg[:], r_c, W["qkg"].rearrange("l p c -> p l c"))
    ld(hysw[:].rearrange("p l c j -> p l (c j)"), r_c, W["hy_sw"].rearrange("l p c j -> p l (c j)"))
    ld(hysb[:], r_c, W["hy_sb"].rearrange("l p c -> p l c"))
    ld(scT[:], r_scT, cT_in[:, :, :])
    fw.op(pool, [], [r_c], "iota", iota_f[:], [[1, 256]], base=0, channel_multiplier=0, allow_small_or_imprecise_dtypes=True)
    fw.op(pool, [], [r_c], "iota", iota_p[:], [[128, 2]], base=0, channel_multiplier=1, allow_small_or_imprecise_dtypes=True)
    fw.op(pool, [], [r_c], "memset", zrow[:], 0.0)
    fw.op(pool, [], [r_c], "memset", one3[:], 1.0)
    fw.op(dve, [r_c], [r_c], "tensor_scalar", ident_f[:], iota_f[:, 0:128], iota_p[:, 0:1], None, ALU.is_equal)
    fw.op(dve, [r_c], [r_c], "tensor_copy", ident_b[:], ident_f[:])
    fw.op(dve, [r_c], [r_c], "tensor_single_scalar", mask0[:], iota_p[:, 0:1], 0.5, ALU.is_gt)
    fw.op(act, [r_scT], [r_scT], "activation", scT[:], scT[:], AF.Silu)
    fw.barrier()

    def evac(i, reads, writes, out_ap, in_ap):
        if i % 2 == 0:
            fw.op(act, reads, writes, "activation", out_ap, in_ap, AF.Copy)
        else:
            fw.op(dve, reads, writes, "tensor_copy", out_ap, in_ap)

    with Phase(fw) as ph:
        xin = ph.buf("xin", [128, 4, 1024], F32, 2)
        xo = ph.buf("xo", [128, 8, 512], F32, 2)
        for S, src in ((SL, x_in), (SX, ctx_in)):
            flat = src.rearrange("b t d -> (b t) d")
            for ti in range(S.NT // 512):
                t, r = xin.get()
                fw.dma([], [r], t[:], flat[ti * 512:(ti + 1) * 512, :].rearrange("(c p) d -> p c d", p=128))
                o, orr = xo.get()
                for dc in range(8):
                    pt, pr = fw.ps()
                    for tcq in range(4):
                        fw.tr([r, r_c], [pr], pt[:, tcq * 128:(tcq + 1) * 128], t[:, tcq, dc * 128:(dc + 1) * 128], ident_f[:])
                    evac(dc, [pr], [orr], o[:, dc, :], pt[:, 0:512])
                fw.dma([orr], [], S.xT.rearrange("(c p) t -> p c t", p=128)[:, :, ti * 512:(ti + 1) * 512], o[:])

    def norm_mod(ph, xt, xr, Asc, Bsc, outs, out_res, width, pools):
        sqp, rsp, tmpp = pools
        sq, sqr = sqp.get()
        fw.op(act, [xr], [sqr], "activation", sq[:, :, 0:width], xt[:, :, 0:width], AF.Square)
        pt, pr = fw.ps()
        fw.mm([sqr, r_c], [pr], pt[:, 0:width], [(ones_bf[:], sq[:, c, 0:width]) for c in range(8)])
        rs, rsr = rsp.get()
        fw.op(act, [pr], [rsr], "activation", rs[:, 0:width], pt[:, 0:width], AF.Sqrt, scale=1.0 / 1024, bias=EPS)
        fw.op(dve, [rsr], [rsr], "reciprocal", rs[:, 0:width], rs[:, 0:width])
        for c in range(8):
            tmp, tr_ = tmpp.get()
            fw.op(dve, [xr, rsr], [tr_], "tensor_tensor", tmp[:, 0:width], xt[:, c, 0:width], rs[:, 0:width], ALU.mult)
            if Bsc is None:
                fw.op(act, [tr_, r_c], [out_res], "activation", outs(c), tmp[:, 0:width], AF.Identity, scale=Asc(c))
            else:
                fw.op(act, [tr_, r_c, r_mod], [out_res], "activation", outs(c), tmp[:, 0:width], AF.Identity,
                      scale=Asc(c), bias=Bsc(c))

    def load_cast(stage, dst_ap, dst_res, src_ap, shape, idx):
        fw.dma([], [dst_res], dst_ap, src_ap, q=fw.gq)

    _, ctypes = col_order()

    def layer(l):
        last = (l == nlayers - 1) and (l == DEPTH - 1)
        streams = [SL] if last else [SX, SL]

        with Phase(fw) as ph:
            wst = ph.buf("wm", [128, 8, 512], F32, 2)
            mrow = ph.buf("mrow", [128, 512], F32, 2)
            scTp, r_sp = ph.sb("scTp", [128, 8, 128], F32)
            fw.op(pool, [], [r_sp], "memset", scTp[:], 0.0)
            fw.op(dve, [r_scT, r_sp], [r_sp], "tensor_copy", scTp[:, :, 0:3], scT[:])
            wm = W["w_mod"][l].rearrange("(c p) f -> p c f", p=128)
            for blk in range(12):
                t, r = wst.get()
                fw.dma([], [r], t[:], wm[:, :, blk * 512:(blk + 1) * 512])
                pt, pr = fw.ps()
                fw.mm([r, r_sp], [pr], pt[:, 0:512], [(scTp[:, dc, :], t[:, dc, :]) for dc in range(8)])
                mr_, mrr = mrow.get()
                evac(blk, [pr], [mrr], mr_[:], pt[:, 0:512])
                p2, pr2 = fw.ps()
                for j in range(4):
                    fw.tr([mrr, r_c], [pr2], p2[:, j * 128:(j + 1) * 128], mr_[:, j * 128:(j + 1) * 128], ident_f[:])
                for col in range(3):
                    fw.op(dve, [pr2, r_c], [r_mod], "tensor_tensor", mod[:, blk * 4:(blk + 1) * 4, col],
                          p2[:, 0:512].rearrange("p (a b) -> p a b", a=4)[:, :, col], bmodT[:, l, blk * 4:(blk + 1) * 4], ALU.add)
            for (Ax, ng, base) in ((A1, n1g, 8), (A2, n2g, 32)):
                fw.op(dve, [r_mod], [r_mod], "tensor_tensor", Ax[:], mod[:, base:base + 8, :], one3[:], ALU.add)
                for col in range(3):
                    fw.op(dve, [r_mod, r_c], [r_mod], "tensor_tensor", Ax[:, :, col], Ax[:, :, col], ng[:, l, :], ALU.mult)

        def mcol(tile):
            return 2 if tile[1] is None else tile[1]

        with Phase(fw) as ph:
            hT, r_hT = ph.sb("hT", [128, 8, 4608], BF16)
            ropeC, _ = ph.sb("ropeC", [128, NL], F32)
            ropeS, _ = ph.sb("ropeS", [128, NL], F32)
            dcds = {}
            for S in (SL, SX):
                dcds[S.name], _ = ph.sb("dcds" + S.name, [128, 256], BF16)
                ld(dcds[S.name][:], r_c, C[S.name + "_DcDs"][:, :])
            ld(ropeC[:], r_c, C["ropeC"][:, :])
            ld(ropeS[:], r_c, C["ropeS"][:, :])
            alltiles = [(SL, tl) for tl in SL.tiles] + [(SX, tl) for tl in SX.tiles]
            hoff = {"L": 0, "X": 4096}
            with Phase(fw) as p1:
                xin = p1.buf("xt", [128, 8, 512], F32, 2)
                pools = (p1.buf("sq", [128, 8, 512], BF16, 1), p1.buf("rs", [128, 512], F32, 2), p1.buf("tmp", [128, 512], F32, 8))
                for S, tl in alltiles:
                    t, r = xin.get()
                    fw.dma([], [r], t[:], S.xT.rearrange("(c p) t -> p c t", p=128)[:, :, tl[0]:tl[0] + 512])
                    col = mcol(tl)
                    c0 = hoff[S.name] + tl[0]
                    norm_mod(p1, t, r, lambda c: A1[:, c, col:col + 1], lambda c: mod[:, c, col:col + 1],
                             lambda c: hT[:, c, c0:c0 + 512], r_hT, 512, pools)
            with Phase(fw) as p2:
                stage = None
                wb = p2.buf("wbf", [128, 8, 512], BF16, 3)
                sqb = p2.buf("sqb", [128, 512], BF16, 2)
                rb = p2.buf("rb", [128, 512], F32, 2)
                t1b = p2.buf("t1b", [128, 512], F32, 2)
                t2b = p2.buf("t2b", [128, 512], F32, 2)
                ob = p2.buf("ob", [128, 512], BF16, 3)
                of = p2.buf("of", [128, 512], F32, 2)
                vb = p2.buf("vb", [128, 4, 128], BF16, 2)
                ub = p2.buf("ub", [128, 4, 256], BF16, 2)
                win = W["w_in"][l].rearrange("(c p) f -> p c f", p=128)
                nchunks = len(ctypes)
                for blk in range((nchunks + 3) // 4):
                    nb = min(4, nchunks - blk * 4)
                    wt, wr = wb.get()
                    load_cast(stage, wt[:, :, 0:nb * 128], wr, win[:, :, blk * 512:blk * 512 + nb * 128], [128, 8, nb * 128], blk)
                    for S, tl in alltiles:
                        c0 = hoff[S.name] + tl[0]
                        isx = S is SX
                        pos0 = tl[2]
                        held = None
                        for j in range(nb):
                            ty, idx = ctypes[blk * 4 + j]
                            if isx and ty in ("qs", "ks"):
                                continue
                            if isx and last and ty not in ("k", "v"):
                                continue
                            wj = lambda dc: wt[:, dc, j * 128:(j + 1) * 128]
                            if ty == "v":
                                vt, vr = vb.get()
                                pt, pr = fw.ps()
                                for tcq in range(4):
                                    fw.mm([wr, r_hT], [pr], pt[:, tcq * 128:(tcq + 1) * 128],
                                          [(hT[:, dc, c0 + tcq * 128:c0 + (tcq + 1) * 128], wj(dc)) for dc in range(8)])
                                evac(0, [pr], [vr], vt[:], pt[:, 0:512].rearrange("p (a b) -> p a b", a=4))
                                fw.dma([vr], [], S.v[tl[0]:tl[0] + 512, :].rearrange("(c p) d -> p c d", p=128), vt[:])
                                continue
                            pt, pr = fw.ps()
                            fw.mm([wr, r_hT], [pr], pt[:, 0:512], [(wj(dc), hT[:, dc, c0:c0 + 512]) for dc in range(8)])
                            if ty in ("q", "k"):
                                gi = 0 if ty == "q" else 2
                                dst = S.qT[idx * 128:(idx + 1) * 128, tl[0]:tl[0] + 512] if ty == "q" else S.kT[:, tl[0]:tl[0] + 512]
                                sq, sqr = sqb.get()
                                fw.op(act, [pr], [sqr], "activation", sq[:], pt[:, 0:512], AF.Square)
                                p2, pr2 = fw.ps()
                                fw.mm([sqr, r_c], [pr2], p2[:, 0:512], [(blockones[:], sq[:])])
                                rt, rr = rb.get()
                                fw.op(act, [pr2], [rr], "activation", rt[:], p2[:, 0:512], AF.Sqrt, scale=1.0 / 64, bias=EPS)
                                fw.op(dve, [rr], [rr], "reciprocal", rt[:], rt[:])
                                if isx:
                                    o, orr = ob.get()
                                    fw.op(dve, [pr, rr, r_c], [orr], "scalar_tensor_tensor", o[:], pt[:, 0:512], qkg[:, l, gi:gi + 1], rt[:], ALU.mult, ALU.mult)
                                    fw.dma([orr], [], dst, o[:])
                                else:
                                    t1, t1r = t1b.get()
                                    fw.op(dve, [pr, r_c], [t1r], "scalar_tensor_tensor", t1[:], pt[:, 0:512], qkg[:, l, gi:gi + 1],
                                          ropeC[:, pos0:pos0 + 512], ALU.mult, ALU.mult)
                                    held = (t1, t1r, rt, rr, dst)
                            elif ty in ("qs", "ks"):
                                gi = 1 if ty == "qs" else 3
                                t1, t1r, rt, rr, dst = held
                                t2, t2r = t2b.get()
                                fw.op(dve, [pr, r_c], [t2r], "scalar_tensor_tensor", t2[:], pt[:, 0:512], qkg[:, l, gi:gi + 1],
                                      ropeS[:, pos0:pos0 + 512], ALU.mult, ALU.mult)
                                fw.op(pool, [t1r, t2r], [t2r], "tensor_tensor", t2[:], t1[:], t2[:], ALU.add)
                                o, orr = ob.get()
                                fw.op(pool, [t2r, rr], [orr], "tensor_tensor", o[:], t2[:], rt[:], ALU.mult)
                                fw.dma([orr], [], dst, o[:])
                            elif ty == "hy":
                                o, orr = of.get()
                                evac(j, [pr], [orr], o[:], pt[:, 0:512])
                                fw.dma([orr], [], S.hyu[idx * 128:(idx + 1) * 128, tl[0]:tl[0] + 512], o[:])
                            elif ty == "g":
                                o, orr = ob.get()
                                fw.op(act, [pr], [orr], "activation", o[:], pt[:, 0:512], AF.Sigmoid)
                                fw.dma([orr], [], S.sg[idx * 128:(idx + 1) * 128, tl[0]:tl[0] + 512], o[:])
                            elif ty == "fn":
                                o, orr = ob.get()
                                evac(j, [pr], [orr], o[:], pt[:, 0:512])
                                ut, ur = ub.get()
                                for half in range(2):
                                    p2, pr2 = fw.ps()
                                    for q2 in range(2):
                                        tcq = half * 2 + q2
                                        fw.mm([orr, r_c], [pr2], p2[:, q2 * 256:(q2 + 1) * 256],
                                              [(o[:, tcq * 128:(tcq + 1) * 128], dcds[S.name][:])])
                                    evac(half, [pr2], [ur], ut[:, half * 2:half * 2 + 2, :], p2[:, 0:512].rearrange("p (a b) -> p a b", a=2))
                                fw.dma([ur], [], S.ucs[tl[0]:tl[0] + 512, idx, :].rearrange("(c p) d -> p c d", p=128), ut[:])

        def attention(S):
            n = S.n
            NK = n + (NCX if S is SL else 0)
            NKC = NK // 128
            QW = S.W
            with Phase(fw) as ph:
                kTs = ph.buf("kTs", [64, 2, NK], BF16, 2)
                vaug = ph.buf("vaug", [128, NKC, 2, 128], BF16, 2)
                for t_ in vaug.t:
                    fw.op(pool, [], [vaug.r[vaug.t.index(t_)]], "memset", t_[:], 1.0)
                qh = ph.buf("qh", [64, n], BF16, 3)
                eb = ph.buf("eb", [128, 512], BF16, 6)
                rcp = ph.buf("rcp", [64, 512], F32, 2)
                ao = ph.buf("ao", [64, 512], BF16, 2)
                fw.ring([0, 1, 2, 3, 6, 7])
                obank = 0
                for b in range(2):
                    kt, kr = kTs.get()
                    vt, vr = vaug.get()
                    fw.dma([], [kr], kt[:, :, 0:n], S.kT[:, b * n:(b + 1) * n].rearrange("(k d) t -> d k t", k=2))
                    for kk in range(2):
                        fw.dma([], [vr], vt[:, 0:n // 128, kk, 0:64],
                               S.v[b * n:(b + 1) * n, kk * 64:(kk + 1) * 64].rearrange("(c p) d -> p c d", p=128))
                    if S is SL:
                        fw.dma([], [kr], kt[:, :, n:NK], SX.kT[:, b * NCX:(b + 1) * NCX].rearrange("(k d) t -> d k t", k=2))
                        for kk in range(2):
                            fw.dma([], [vr], vt[:, n // 128:NKC, kk, 0:64],
                                   SX.v[b * NCX:(b + 1) * NCX, kk * 64:(kk + 1) * 64].rearrange("(c p) d -> p c d", p=128))
                    steps = []
                    for h in range(8):
                        for qi in range(n // QW):
                            for sc in range(NKC):
                                steps.append((h, qi, sc))
                    qcur = {}

                    def get_q(h):
                        if h not in qcur:
                            qt_, qr = qh.get()
                            fw.dma([], [qr], qt_[:], S.qT[h * 64:(h + 1) * 64, b * n:(b + 1) * n])
                            qcur[h] = (qt_, qr)
                        return qcur[h]

                    def issue_s(st):
                        h, qi, sc = st
                        qt_, qr = get_q(h)
                        pt, pr = fw.ps()
                        fw.mm([kr, qr], [pr], pt[:, 0:QW], [(kt[:, h // 4, sc * 128:(sc + 1) * 128], qt_[:, qi * QW:(qi + 1) * QW])])
                        return pt, pr

                    PRE = 3
                    pend = [issue_s(st) for st in steps[:PRE]]
                    po = por = None
                    for i, (h, qi, sc) in enumerate(steps):
                        kvh = h // 4
                        pt, pr = pend.pop(0)
                        if i + PRE < len(steps):
                            pend.append(issue_s(steps[i + PRE]))
                        if sc == 0:
                            po, por = fw.bank(4 + obank % 2)
                            obank += 1
                        e, er = eb.get()
                        fw.op(act, [pr], [er], "activation", e[:, 0:QW], pt[:, 0:QW], AF.Exp, scale=0.125)
                        fw.mm([vr, er], [por], po[:, 0:QW], [(vt[:, sc, kvh, :], e[:, 0:QW])], start=(sc == 0), stop=(sc == NKC - 1))
                        if sc == NKC - 1:
                            rc, rcr = rcp.get()
                            fw.op(dve, [por], [rcr], "reciprocal", rc[:, 0:QW], po[64:128, 0:QW])
                            a, ar = ao.get()
                            fw.op(dve, [por, rcr], [ar], "tensor_tensor", a[:, 0:QW], po[0:64, 0:QW], rc[:, 0:QW], ALU.mult)
                            fw.dma([ar], [], S.mixT[h * 64:(h + 1) * 64, b * n + qi * QW:b * n + (qi + 1) * QW], a[:, 0:QW])
                fw.ring(range(8))

        def hy_spectrum(S):
            n = S.n
            NCn = S.NC
            Wn = S.W
            pre = S.name + "_"
            with Phase(fw) as ph:
                feats, rf = ph.sb("feats", [33, n], F32)
                w1, rw = ph.sb("w1", [33, 64], F32)
                w2, _ = ph.sb("w2", [64, 64], F32)
                w3, _ = ph.sb("w3", [64, 1024], F32)
                b1, _ = ph.sb("b1", [64, 1], F32)
                b2, _ = ph.sb("b2", [64, 1], F32)
                fr, _ = ph.sb("fr", [64, 1], F32)
                dec2, _ = ph.sb("dec2", [128, NCn, 512], F32)
                rs_, _ = ph.sb("rs", [128, NCn, 2], F32)
                dB, _ = ph.sb("dB", [128, 512], F32)
                ld(feats[:], rw, C[pre + "featsT"][:, :])
                ld(w1[:], rw, W["hy_w1"][l])
                ld(w2[:], rw, W["hy_w2"][l])
                ld(w3[:], rw, W["hy_w3"][l])
                ld(b1[:], rw, W["hy_b1"][l])
                ld(b2[:], rw, W["hy_b2"][l])
                ld(fr[:], rw, W["hy_fr"][l])
                ld(dec2[:], rw, C[pre + "dec2"][:, :, :])
                ld(rs_[:], rw, C[pre + "rs"][:, :, :])
                ld(dB[:], rw, W["hy_bias"][l].partition_broadcast(128))
                fw.op(dve, [rw], [rw], "tensor_tensor", b1[:], b1[:], fr[:], ALU.mult)
                fw.op(dve, [rw], [rw], "tensor_tensor", b2[:], b2[:], fr[:], ALU.mult)
                h1, rh1 = ph.sb("h1", [64, n], F32)
                h2, rh2 = ph.sb("h2", [64, n], F32)
                zb = ph.buf("zb", [64, 512], F32, 2)
                mb = ph.buf("mb", [64, 512], F32, 2)

                def sin_layer(wt, K, src, rsrc, bt, dst, rdst):
                    for ti in range(n // Wn):
                        sl = slice(ti * Wn, (ti + 1) * Wn)
                        pt, pr = fw.ps()
                        fw.mm([rw, rsrc], [pr], pt[0:64, 0:Wn], [(wt[0:K, :], src[0:K, sl])])
                        z, zr = zb.get()
                        fw.op(dve, [pr, rw], [zr], "tensor_scalar", z[:, 0:Wn], pt[0:64, 0:Wn], fr[:, 0:1], bt[:, 0:1], ALU.mult, ALU.add)
                        for rep in range(2):
                            m, mr = mb.get()
                            fw.op(dve, [zr], [mr], "tensor_scalar", m[:, 0:Wn], z[:, 0:Wn], PI, -2 * PI, ALU.is_gt, ALU.mult)
                            fw.op(dve, [zr, mr], [zr], "tensor_tensor", z[:, 0:Wn], z[:, 0:Wn], m[:, 0:Wn], ALU.add)
                            m, mr = mb.get()
                            fw.op(dve, [zr], [mr], "tensor_scalar", m[:, 0:Wn], z[:, 0:Wn], -PI, 2 * PI, ALU.is_lt, ALU.mult)
                            fw.op(dve, [zr, mr], [zr], "tensor_tensor", z[:, 0:Wn], z[:, 0:Wn], m[:, 0:Wn], ALU.add)
                        fw.op(act, [zr], [rdst], "activation", dst[:, sl], z[:, 0:Wn], AF.Sin)

                sin_layer(w1, 33, feats, rw, b1, h1, rh1)
                sin_layer(w2, 64, h1, rh1, b2, h2, rh2)
                gsum, rgs = ph.sb("gsum", [128, NCn, 512], BF16)
                gdif, rgd = ph.sb("gdif", [128, NCn, 512], BF16)
                hfb = ph.buf("hf", [128, 512], F32, 2)
                hbb = ph.buf("hb", [128, 512], F32, 2)
                a1b = ph.buf("a1", [128, 512], F32, 2)
                a2b = ph.buf("a2", [128, 512], BF16, 2)
                a3b = ph.buf("a3", [128, 512], F32, 2)
                fw.ring([0, 1, 2, 3, 4, 5])
                pl1, pl1r = fw.bank(7)
                for mc in range(NCn):
                    pa, par = fw.ps()
                    pb, pbr = fw.ps()
                    fw.mm([rh2, rw], [par], pa[:, 0:512], [(h2[:, mc * 128:(mc + 1) * 128], w3[:, 0:512])])
                    fw.mm([rh2, rw], [pbr], pb[:, 0:512], [(h2[:, mc * 128:(mc + 1) * 128], w3[:, 512:1024])])
                    hf, hfr = hfb.get()
                    hb, hbr = hbb.get()
                    fw.op(dve, [par, rw], [hfr], "tensor_tensor", hf[:], pa[:, 0:512], dec2[:, mc, :], ALU.mult)
                    if mc == 0:
                        fw.op(dve, [pbr, rw, r_c], [hbr], "scalar_tensor_tensor", hb[:], pb[:, 0:512], mask0[:, 0:1], dec2[:, mc, :], ALU.mult, ALU.mult)
                    else:
                        fw.op(dve, [pbr, rw], [hbr], "tensor_tensor", hb[:], pb[:, 0:512], dec2[:, mc, :], ALU.mult)
                    fw.op(pool, [hfr, hbr], [rgs], "tensor_tensor", gsum[:, mc, :], hf[:], hb[:], ALU.add)
                    fw.op(pool, [hfr, hbr], [rgd], "tensor_tensor", gdif[:, mc, :], hf[:], hb[:], ALU.subtract)
                    a1, a1r = a1b.get()
                    a2, a2r = a2b.get()
                    a3, a3r = a3b.get()
                    fw.op(act, [hfr], [a1r], "activation", a1[:], hf[:], AF.Abs)
                    fw.op(act, [hbr], [a3r], "activation", a3[:], hb[:], AF.Abs)
                    fw.op(pool, [a1r, a3r], [a2r], "tensor_tensor", a2[:], a1[:], a3[:], ALU.add)
                    fw.mm([a2r, r_c], [pl1r], pl1[:, 0:512], [(ones_bf[:], a2[:])], start=(mc == 0), stop=(mc == NCn - 1))
                il1, ril = ph.sb("il1", [128, 512], F32)
                fw.op(dve, [pl1r], [ril], "reciprocal", il1[:], pl1[:, 0:512])
                cst = ph.buf("cst", [128, NCn, 128], BF16, 2)
                fst = ph.buf("fst", [128, NCn, 128], BF16, 2)
                ab = ph.buf("ab", [128, 512], F32, 2)
                bb = ph.buf("bb", [128, 512], F32, 2)
                for fc in range(NCn):
                    ct, cr = cst.get()
                    ft, fr_ = fst.get()
                    fw.dma([], [cr], ct[:], C[pre + "C_st"][fc])
                    fw.dma([], [fr_], ft[:], C[pre + "Fs_st"][fc])
                    pa, par = fw.ps()
                    pb, pbr = fw.ps()
                    fw.mm([cr, rgs], [par], pa[:, 0:512], [(ct[:, mc, :], gsum[:, mc, :]) for mc in range(NCn)])
                    fw.mm([fr_, rgd], [pbr], pb[:, 0:512], [(ft[:, mc, :], gdif[:, mc, :]) for mc in range(NCn)])
                    a, ar = ab.get()
                    fw.op(dve, [par, ril], [ar], "tensor_tensor", a[:], pa[:, 0:512], il1[:], ALU.mult)
                    fw.op(pool, [ar, rw], [ar], "tensor_tensor", a[:], a[:], dB[:], ALU.add)
                    fw.op(act, [ar, rw], [ar], "activation", a[:], a[:], AF.Identity, scale=rs_[:, fc, 0:1])
                    bt_, btr = bb.get()
                    fw.op(dve, [pbr, ril, rw], [btr], "scalar_tensor_tensor", bt_[:], pb[:, 0:512], rs_[:, fc, 1:2], il1[:], ALU.mult, ALU.mult)
                    fw.dma([ar], [], S.specA[fc * 128:(fc + 1) * 128, :], a[:])
                    fw.dma([btr], [], S.specB[fc * 128:(fc + 1) * 128, :], bt_[:])
                    if fc == 0:
                        pn, pnr = fw.ps()
                        fw.mm([rgs, r_c], [pnr], pn[0:1, 0:512], [(altcol[:, 0:1], gsum[:, mc, :]) for mc in range(NCn)])
                        a2_, a2r_ = ph.sb("a2c0", [128, 512], F32)
                        nq, nqr = ph.sb("nq", [1, 512], F32)
                        fw.op(dve, [pnr, ril], [nqr], "tensor_tensor", nq[:], pn[0:1, 0:512], il1[0:1, :], ALU.mult)
                        fw.op(dve, [nqr, rw], [nqr], "tensor_tensor", nq[:], nq[:], dB[0:1, :], ALU.add)
                        fw.op(dve, [ar], [a2r_], "tensor_copy", a2_[:], a[:])
                        fw.op(dve, [nqr, a2r_], [a2r_], "tensor_single_scalar", a2_[0:1, :], nq[:], 0.5 / n, ALU.mult)
                        fw.dma([a2r_], [], S.specA2[:, :], a2_[:])
                fw.ring(range(8))

        def hyena(S):
            n = S.n
            NCn = S.NC
            Wn = S.W
            pre = S.name + "_"
            with Phase(fw) as ph:
                vtok, rv = ph.sb("vtok", [128, NCn, 512], BF16)
                g1tok, rg1 = ph.sb("g1tok", [128, NCn, 512], BF16)
                g2T, rg2 = ph.sb("g2T", [128, 4, n], F32)
                ztok, rz = ph.sb("ztok", [128, NCn, 512], BF16)
                Y, rY = ph.sb("Y", [128, NCn, 2, 512], BF16)
                with Phase(fw) as p1:
                    ub = p1.buf("u", [128, n], F32, 2)
                    cb = p1.buf("cv", [128, n], F32, 2)
                    for b in range(2):
                        for hc in range(6):
                            u, ur = ub.get()
                            fw.dma([], [ur], u[:], S.hyu[hc * 128:(hc + 1) * 128, b * n:(b + 1) * n])
                            if hc >= 4:
                                orr = rg2
                                ct_ = None
                            else:
                                ct_, orr = cb.get()

                            def osl(a, b_):
                                return g2T[:, b * 2 + (hc - 4), a:b_] if hc >= 4 else ct_[:, a:b_]
                            fw.op(dve, [ur, r_c], [orr], "tensor_scalar", osl(0, n), u[:], hysw[:, l, hc, 1:2], hysb[:, l, hc:hc + 1], ALU.mult, ALU.add)
                            fw.op(dve, [ur, r_c, orr], [orr], "scalar_tensor_tensor", osl(1, n), u[:, 0:n - 1], hysw[:, l, hc, 0:1], osl(1, n), ALU.mult, ALU.add)
                            fw.op(dve, [ur, r_c, orr], [orr], "scalar_tensor_tensor", osl(0, n - 1), u[:, 1:n], hysw[:, l, hc, 2:3], osl(0, n - 1), ALU.mult, ALU.add)
                            if hc < 4:
                                dst, rdst = (vtok, rv) if hc < 2 else (g1tok, rg1)
                                cbase = b * 256 + (hc % 2) * 128
                                nq = min(4, NCn)
                                for t4 in range(NCn // nq):
                                    pt, pr = fw.ps()
                                    for q in range(nq):
                                        tc = t4 * nq + q
                                        fw.tr([orr, r_c], [pr], pt[:, q * 128:(q + 1) * 128], ct_[:, tc * 128:(tc + 1) * 128], ident_f[:])
                                    evac(t4, [pr], [rdst], dst[:, t4 * nq:t4 * nq + nq, cbase:cbase + 128],
                                         pt[:, 0:nq * 128].rearrange("p (a b) -> p a b", a=nq))
                with Phase(fw) as p2:
                    stA = p2.buf("stA", [128, NCn, 128], BF16, 2)
                    stB = p2.buf("stB", [128, NCn, 128], BF16, 2)
                    spA = p2.buf("spA", [128, 2, 256], F32, 2)
                    spB = p2.buf("spB", [128, 2, 256], F32, 2)
                    spA2 = p2.buf("spA2", [128, 2, 256], F32, 1)
                    t1b = p2.buf("t1", [128, 512], F32, 2)
                    t2b = p2.buf("t2", [128, 512], F32, 2)
                    t3b = p2.buf("t3", [128, 512], F32, 2)
                    t4b = p2.buf("t4", [128, 512], F32, 2)

                    def fwd(src, rsrc, o):
                        for fc in range(NCn):
                            ct, cr = stA.get()
                            ft, fr_ = stB.get()
                            fw.dma([], [cr], ct[:], C[pre + "C_st"][fc])
                            fw.dma([], [fr_], ft[:], C[pre + "Fs_st"][fc])
                            at, ar = spA.get()
                            bt_, br = spB.get()
                            for b in range(2):
                                fw.dma([], [ar], at[:, b, :], S.specA[fc * 128:(fc + 1) * 128, o * 256:(o + 1) * 256])
                                fw.dma([], [br], bt_[:, b, :], S.specB[fc * 128:(fc + 1) * 128, o * 256:(o + 1) * 256])
                            if fc == 0:
                                a2t, a2r = spA2.get()
                                for b in range(2):
                                    fw.dma([], [a2r], a2t[:, b, :], S.specA2[:, o * 256:(o + 1) * 256])
                            else:
                                a2t, a2r = at, ar
                            pzr, pzrr = fw.ps()
                            pzi, pzir = fw.ps()
                            fw.mm([cr, rsrc], [pzrr], pzr[:, 0:512], [(ct[:, tc, :], src[:, tc, :]) for tc in range(NCn)])
                            fw.mm([fr_, rsrc], [pzir], pzi[:, 0:512], [(ft[:, tc, :], src[:, tc, :]) for tc in range(NCn)])
                            af = at[:].rearrange("p a b -> p (a b)")
                            bf = bt_[:].rearrange("p a b -> p (a b)")
                            a2f = a2t[:].rearrange("p a b -> p (a b)")
                            t1, r1 = t1b.get()
                            t2, r2 = t2b.get()
                            t3, r3 = t3b.get()
                            t4, r4 = t4b.get()
                            fw.op(dve, [pzrr, ar], [r1], "tensor_tensor", t1[:], pzr[:, 0:512], af, ALU.mult)
                            fw.op(dve, [pzir, br], [r2], "tensor_tensor", t2[:], pzi[:, 0:512], bf, ALU.mult)
                            fw.op(dve, [pzrr, br], [r3], "tensor_tensor", t3[:], pzr[:, 0:512], bf, ALU.mult)
                            fw.op(dve, [pzir, a2r], [r4], "tensor_tensor", t4[:], pzi[:, 0:512], a2f, ALU.mult)
                            fw.op(pool, [r1, r2], [rY], "tensor_tensor", Y[:, fc, 0, :], t1[:], t2[:], ALU.subtract)
                            fw.op(pool, [r3, r4], [rY], "tensor_tensor", Y[:, fc, 1, :], t3[:], t4[:], ALU.add)

                    fwd(vtok, rv, 0)
                    for tc in range(NCn):
                        ct, cr = stA.get()
                        gt, gr = stB.get()
                        fw.dma([], [cr], ct[:], C[pre + "C_st"][tc])
                        fw.dma([], [gr], gt[:], C[pre + "Gs_st"][tc])
                        pz, pzr_ = fw.ps()
                        fw.mm([cr, gr, rY], [pzr_], pz[:, 0:512],
                              [(ct[:, fc, :], Y[:, fc, 0, :]) for fc in range(NCn)] + [(gt[:, fc, :], Y[:, fc, 1, :]) for fc in range(NCn)])
                        fw.op(dve, [pzr_, rg1], [rz], "tensor_tensor", ztok[:, tc, :], pz[:, 0:512], g1tok[:, tc, :], ALU.mult)
                    fwd(ztok, rz, 1)
                with Phase(fw) as p3:
                    mvA = p3.buf("mvA", [128, NCn, Wn], BF16, 1)
                    mvB = p3.buf("mvB", [128, NCn, Wn], BF16, 1)
                    yo = p3.buf("yo", [128, Wn], BF16, 2)
                    for tt in range(n // Wn):
                        ct, cr = mvA.get()
                        gt, gr = mvB.get()
                        fw.dma([], [cr], ct[:], C[pre + "C_mv"][tt])
                        fw.dma([], [gr], gt[:], C[pre + "Gs_mv"][tt])
                        for bcc in range(4):
                            py, pyr = fw.ps()
                            fw.mm([cr, gr, rY], [pyr], py[:, 0:Wn],
                                  [(Y[:, fc, 0, bcc * 128:(bcc + 1) * 128], ct[:, fc, :]) for fc in range(NCn)] +
                                  [(Y[:, fc, 1, bcc * 128:(bcc + 1) * 128], gt[:, fc, :]) for fc in range(NCn)])
                            o, orr = yo.get()
                            fw.op(dve, [pyr, rg2], [orr], "tensor_tensor", o[:], py[:, 0:Wn], g2T[:, bcc, tt * Wn:(tt + 1) * Wn], ALU.mult)
                            b, cc = bcc // 2, bcc % 2
                            fw.dma([orr], [], S.mixT[512 + cc * 128:512 + (cc + 1) * 128, b * n + tt * Wn:b * n + (tt + 1) * Wn], o[:])

        def fnet(S):
            n = S.n
            NCn = S.NC
            Wn = S.W
            pre = S.name + "_"
            with Phase(fw) as ph:
                us, rus = ph.sb("ucs", [128, 2, NCn, 2, 256], BF16)
                for b in range(2):
                    fw.dma([], [rus], us[:, b, :, :, :].rearrange("p c a d -> p c (a d)"), S.ucs[b * n:(b + 1) * n, :, :].rearrange("(c p) a d -> p c (a d)", p=128))
                mvA = ph.buf("fA", [128, NCn, Wn], BF16, 2)
                mvB = ph.buf("fB", [128, NCn, Wn], BF16, 2)
                yo = ph.buf("fyo", [128, Wn], BF16, 2)
                k = 0
                for kt in range(n // Wn):
                    ct, cr = mvA.get()
                    st_, sr = mvB.get()
                    fw.dma([], [cr], ct[:], C[pre + "Ct_mv"][kt])
                    fw.dma([], [sr], st_[:], C[pre + "St_mv"][kt])
                    for b in range(2):
                        for a in range(2):
                            py, pyr = fw.ps()
                            fw.mm([cr, sr, rus], [pyr], py[:, 0:Wn],
                                  [(us[:, b, tc, a, 0:128], ct[:, tc, :]) for tc in range(NCn)] +
                                  [(us[:, b, tc, a, 128:256], st_[:, tc, :]) for tc in range(NCn)])
                            o, orr = yo.get()
                            evac(k, [pyr], [orr], o[:], py[:, 0:Wn])
                            k += 1
                            fw.dma([orr], [], S.mixT[768 + a * 128:768 + (a + 1) * 128, b * n + kt * Wn:b * n + (kt + 1) * Wn], o[:])

        def merge():
            with Phase(fw) as ph:
                stage = None
                wbr, rwb = ph.sb("wbr", [128, 8, 1024], BF16)
                wo, rwo = ph.sb("wo", [128, 8, 1024], BF16)
                for i, (dst, rd, src) in enumerate(((wbr, rwb, W["w_branch"][l]), (wo, rwo, W["w_out"][l]))):
                    sv = src.rearrange("(c p) f -> p c f", p=128)
                    for hlf in range(2):
                        load_cast(stage, dst[:, :, hlf * 512:(hlf + 1) * 512], rd, sv[:, :, hlf * 512:(hlf + 1) * 512], [128, 8, 512], i * 2 + hlf)
                mixb = ph.buf("mixb", [128, 8, 512], BF16, 2)
                sgb = ph.buf("sgb", [128, 24, 512], BF16, 1)
                xb = ph.buf("xb", [128, 8, 512], F32, 2)
                mT = ph.buf("mT", [128, 8, 512], BF16, 1)
                tb = [ph.buf("mt%d" % i, [128, 512], F32, 2) for i in range(3)]
                for S in streams:
                    for tl in S.tiles:
                        sl = slice(tl[0], tl[0] + 512)
                        col = mcol(tl)
                        mx, mxr = mixb.get()
                        sg, sgr = sgb.get()
                        xt, xr = xb.get()
                        m, mr = mT.get()
                        fw.dma([], [mxr], mx[:], S.mixT.rearrange("(c p) t -> p c t", p=128)[:, :, sl])
                        fw.dma([], [sgr], sg[:], S.sg.rearrange("(c p) t -> p c t", p=128)[:, :, sl])
                        fw.dma([], [xr], xt[:], S.xT.rearrange("(c p) t -> p c t", p=128)[:, :, sl])
                        for dc in range(8):
                            ts = []
                            for bi, (k0, k1) in enumerate(((0, 4), (4, 6), (6, 8))):
                                pt, pr = fw.ps()
                                fw.mm([rwb, mxr], [pr], pt[:, 0:512], [(wbr[:, kc, dc * 128:(dc + 1) * 128], mx[:, kc, :]) for kc in range(k0, k1)])
                                t, tr_ = tb[bi].get()
                                fw.op(dve, [pr, sgr], [tr_], "tensor_tensor", t[:], pt[:, 0:512], sg[:, bi * 8 + dc, :], ALU.mult)
                                ts.append((t, tr_))
                            fw.op(pool, [ts[0][1], ts[1][1]], [ts[1][1]], "tensor_tensor", ts[1][0][:], ts[0][0][:], ts[1][0][:], ALU.add)
                            fw.op(pool, [ts[1][1], ts[2][1]], [mr], "tensor_tensor", m[:, dc, :], ts[1][0][:], ts[2][0][:], ALU.add)
                        for dc in range(8):
                            pt, pr = fw.ps()
                            fw.mm([rwo, mr], [pr], pt[:, 0:512], [(wo[:, kc, dc * 128:(dc + 1) * 128], m[:, kc, :]) for kc in range(8)])
                            fw.op(dve, [pr, xr, r_mod], [xr], "scalar_tensor_tensor", xt[:, dc, :], pt[:, 0:512], mod[:, 16 + dc, col:col + 1], xt[:, dc, :], ALU.mult, ALU.add)
                        fw.dma([xr], [], S.xT.rearrange("(c p) t -> p c t", p=128)[:, :, sl], xt[:])

        def moe():
            with Phase(fw) as PH:
                aff = {}
                for S in streams:
                    aff[S.name] = PH.sb("aff" + S.name, [128, S.NC, 2, 16], F32)
                wr_, rwr = PH.sb("wr", [128, 8, 16], F32)
                ld(wr_[:], rwr, W["w_router"][l].rearrange("(c p) e -> p c e", p=128))
                slotT = {}
                slot_tok = {}
                affb = {}
                affTb = {}
                for S in streams:
                    slotT[S.name] = PH.sb("slotT" + S.name, [128, S.n], BF16)
                    affTb[S.name] = PH.sb("affTb" + S.name, [128, S.n], BF16)
                    fw.op(pool, [], [slotT[S.name][1]], "memset", slotT[S.name][0][:], 0.0)
                    fw.op(pool, [], [affTb[S.name][1]], "memset", affTb[S.name][0][:], 0.0)
                    slot_tok[S.name] = PH.sb("slottok" + S.name, [128, S.NC, 32], F32)
                    affb[S.name] = PH.sb("affb" + S.name, [128, S.NC, 2, 16], BF16)
                with Phase(fw) as ph:
                    xin = ph.buf("xt2", [128, 8, 512], F32, 2)
                    h2b = ph.buf("h2T", [128, 8, 512], F32, 2)
                    pools = (ph.buf("sq2", [128, 8, 512], BF16, 1), ph.buf("rs2", [128, 512], F32, 2), ph.buf("tmp2", [128, 512], F32, 8))
                    hto = ph.buf("hto", [128, 4, 1024], BF16, 2)
                    sm = ph.buf("sm", [128, 4], F32, 4)
                    eb = ph.buf("ex", [128, 16], F32, 4)
                    for S in streams:
                        at, ar = aff[S.name]
                        for tl in S.tiles:
                            t, r = xin.get()
                            fw.dma([], [r], t[:], S.xT.rearrange("(c p) t -> p c t", p=128)[:, :, tl[0]:tl[0] + 512])
                            col = mcol(tl)
                            h2, h2r = h2b.get()
                            norm_mod(ph, t, r, lambda c: A2[:, c, col:col + 1], lambda c: mod[:, 24 + c, col:col + 1],
                                     lambda c: h2[:, c, :], h2r, 512, pools)
                            ho, hor = hto.get()
                            st4 = []
                            for tcq in range(4):
                                tok0 = tl[0] + tcq * 128
                                b = tok0 // S.n
                                tc = (tok0 % S.n) // 128
                                tsl = slice(tcq * 128, (tcq + 1) * 128)
                                pt, pr = fw.ps()
                                fw.mm([h2r, rwr], [pr], pt[:, 0:16], [(h2[:, dc, tsl], wr_[:, dc, :]) for dc in range(8)])
                                s, sr = sm.get()
                                e, er = eb.get()
                                st4.append((b, tc, tsl, pt, pr, s, sr, e, er))
                            for (b, tc, tsl, pt, pr, s, sr, e, er) in st4:
                                fw.op(dve, [pr], [sr], "tensor_reduce", s[:, 0:1], pt[:, 0:16], AX.X, ALU.max, negate=True)
                            for (b, tc, tsl, pt, pr, s, sr, e, er) in st4:
                                fw.op(act, [pr, sr], [er, sr], "activation", e[:], pt[:, 0:16], AF.Exp, bias=s[:, 0:1], accum_out=s[:, 1:2])
                            for (b, tc, tsl, pt, pr, s, sr, e, er) in st4:
                                fw.op(dve, [sr], [sr], "reciprocal", s[:, 2:3], s[:, 1:2])
                            for (b, tc, tsl, pt, pr, s, sr, e, er) in st4:
                                fw.op(dve, [er, sr], [ar], "tensor_scalar", at[:, tc, b, :], e[:], s[:, 2:3], None, ALU.mult)
                            for tcq in range(4):
                                tsl = slice(tcq * 128, (tcq + 1) * 128)
                                for hh in range(2):
                                    p2, pr2 = fw.ps()
                                    for q in range(4):
                                        dc = hh * 4 + q
                                        fw.tr([h2r, r_c], [pr2], p2[:, q * 128:(q + 1) * 128], h2[:, dc, tsl], ident_f[:])
                                    evac(hh, [pr2], [hor], ho[:, tcq, hh * 512:(hh + 1) * 512], p2[:, 0:512])
                            fw.dma([hor], [], S.h2tok[tl[0]:tl[0] + 512, :].rearrange("(c p) d -> p c d", p=128), ho[:])
                with Phase(fw) as ph:
                    for S in streams:
                        n = S.n
                        at, ar = aff[S.name]
                        affT, rat = ph.sb("affT", [32, n], F32)
                        wk = ph.buf("wk", [32, n], F32, 2)
                        m8, rm8 = ph.sb("m8", [32, 8], F32)
                        msk, rmsk = ph.sb("msk", [32, n], F32)
                        rnk, rrnk = ph.sb("rnk", [32, n], F32)
                        slf, rslf = ph.sb("slf", [32, n], F32)
                        fw.op(dve, [ar], [affb[S.name][1]], "tensor_copy", affb[S.name][0][:], at[:])
                        for t4 in range(max(1, S.NC // 4)):
                            nq = min(4, S.NC)
                            pt, pr = fw.ps()
                            for q in range(nq):
                                tc = t4 * 4 + q
                                fw.tr([ar, r_c], [pr], pt[0:32, q * 128:(q + 1) * 128], at[:, tc, :, :].rearrange("p a b -> p (a b)"), ident_f[:])
                            evac(t4, [pr], [rat], affT[:, t4 * 512:t4 * 512 + nq * 128], pt[0:32, 0:nq * 128])
                        cur, curr = affT, rat
                        nit = S.cap // 8
                        for it in range(nit):
                            fw.op(dve, [curr], [rm8], "max", m8[:], cur[:])
                            if it < nit - 1:
                                nx, nxr = wk.get()
                                fw.op(dve, [curr, rm8], [nxr], "match_replace", nx[:], m8[:], cur[:], -1e30)
                                cur, curr = nx, nxr
                        fw.op(dve, [rat, rm8], [rmsk], "tensor_scalar", msk[:], affT[:], m8[:, 7:8], None, ALU.is_ge)
                        fw.op(dve, [rmsk, r_c], [rrnk], "tensor_tensor_scan", rnk[:], msk[:], zrow[:, 0:n], 0.0, ALU.add, ALU.add)
                        fw.op(dve, [rrnk], [rslf], "tensor_single_scalar", slf[:], rnk[:], float(S.cap) + 0.5, ALU.is_lt)
                        fw.op(dve, [rslf, rmsk], [rmsk], "tensor_tensor", msk[:], msk[:], slf[:], ALU.mult)
                        fw.op(dve, [rrnk, rmsk], [rslf], "tensor_tensor", slf[:], rnk[:], msk[:], ALU.mult)
                        fw.op(dve, [rslf], [rslf], "tensor_single_scalar", slf[:], slf[:], -1.0, ALU.add)
                        st_, rst = slotT[S.name]
                        fw.op(dve, [rslf], [rst], "tensor_copy", st_[0:32, :], slf[:])
                        fw.op(dve, [rat], [affTb[S.name][1]], "tensor_copy", affTb[S.name][0][0:32, :], affT[:])
                        sk, rsk = slot_tok[S.name]
                        for t4 in range(max(1, S.NC // 4)):
                            nq = min(4, S.NC)
                            pt, pr = fw.ps()
                            for q in range(nq):
                                tc = t4 * 4 + q
                                fw.tr([rslf, r_c], [pr], pt[:, q * 32:(q + 1) * 32], slf[:, tc * 128:(tc + 1) * 128], ident_f[0:32, 0:32])
                            evac(t4, [pr], [rsk], sk[:, t4 * 4:t4 * 4 + nq, :], pt[:, 0:nq * 32].rearrange("p (a b) -> p a b", a=nq))
                with Phase(fw) as ph:
                    stage = None
                    wgb = ph.buf("wg", [128, 8, 512], BF16, 3)
                    wub = ph.buf("wu", [128, 8, 512], BF16, 3)
                    wdb = ph.buf("wd", [128, 16, 512], BF16, 2)
                    sel = {}
                    selT = {}
                    xg = {}
                    actT = {}
                    wsl = {}
                    for S in streams:
                        sel[S.name] = ph.buf("sel" + S.name, [128, S.NC, S.cap], BF16, 1)
                        selT[S.name] = ph.buf("selT" + S.name, [S.SP, S.SC, S.n], BF16, 1)
                        xg[S.name] = ph.buf("xg" + S.name, [128, 8, 2 * S.cap], BF16, 1)
                        actT[S.name] = ph.buf("actT" + S.name, [128, 16, 2 * S.cap], BF16, 1)
                    h2t = ph.buf("h2t", [128, 16, 1024], BF16, 1)
                    abuf = ph.buf("abuf", [128, 512], F32, 2)
                    s0buf = ph.buf("s0buf", [128, 512], BF16, 3)
                    sa = ph.buf("sa", [128, 512], F32, 2)
                    ygb = ph.buf("ygb", [128, 512], BF16, 3)
                    cast_i = 0
                    ev_i = 0
                    for e in range(16):
                        XG = {}
                        AT = {}
                        WS = {}
                        for S in streams:
                            n, cap, SC, SP, NCn = S.n, S.cap, S.SC, S.SP, S.NC
                            xgt, xgr = xg[S.name].get()
                            XG[S.name] = (xgt, xgr)
                            sk, rsk = slot_tok[S.name]
                            st_, rst = slotT[S.name]
                            ab_, rab = affb[S.name]
                            for b in range(2):
                                q = b * 16 + e
                                se, ser = sel[S.name].get()
                                for tc in range(NCn):
                                    fw.op(dve, [rsk, r_c], [ser], "tensor_scalar", se[:, tc, :], iota_f[:, 0:cap], sk[:, tc, q:q + 1], None, ALU.is_equal)
                                sT, sTr = selT[S.name].get()
                                aTb, raT = affTb[S.name]
                                for tt in range(n // S.W):
                                    cs = slice(tt * S.W, (tt + 1) * S.W)
                                    pt, pr = fw.ps()
                                    fw.mm([rst, r_c], [pr], pt[0:SP, 0:S.W], [(esel[:, q * 128:q * 128 + SP], st_[:, cs])])
                                    pa, par = fw.ps()
                                    fw.mm([raT, r_c], [par], pa[0:SP, 0:S.W], [(esel[:, q * 128:q * 128 + SP], aTb[:, cs])])
                                    ab, abr = abuf.get()
                                    fw.op(act, [par], [abr], "activation", ab[0:SP, 0:S.W], pa[0:SP, 0:S.W], AF.Copy)
                                    for sc in range(SC):
                                        s0, s0r = s0buf.get()
                                        fw.op(dve, [pr, r_c], [s0r], "tensor_scalar", s0[0:SP, 0:S.W], pt[0:SP, 0:S.W],
                                              iota_p[0:SP, sc:sc + 1], None, ALU.is_equal)
                                        fw.op(pool, [s0r, abr], [sTr], "tensor_tensor", sT[:, sc, cs], s0[0:SP, 0:S.W], ab[0:SP, 0:S.W], ALU.mult)
                                fw.dma([sTr], [], S.selT[b, e].rearrange("s p t -> p s t"), sT[:])
                                pg = [fw.ps() for _ in range(4)] if cap == 256 else [fw.ps()]
                                ngrp = max(1, NCn // 4)
                                nq = min(4, NCn)
                                ht, hr = h2t.get()
                                for g in range(ngrp):
                                    fw.dma([], [hr], ht[:, g * nq:(g + 1) * nq, :], S.h2tok[b * n + g * 512:b * n + g * 512 + nq * 128, :].rearrange("(c p) d -> p c d", p=128))
                                for dc in range(8):
                                    if cap == 256:
                                        pt, pr = pg[dc // 2]
                                        oap = pt[:, (dc % 2) * 256:(dc % 2 + 1) * 256]
                                    else:
                                        pt, pr = pg[0]
                                        oap = pt[:, dc * cap:(dc + 1) * cap]
                                    fw.mm([hr, ser], [pr], oap, [(ht[:, tc, dc * 128:(dc + 1) * 128], se[:, tc, :]) for tc in range(NCn)])
                                for dc in range(8):
                                    if cap == 256:
                                        pt, pr = pg[dc // 2]
                                        oap = pt[:, (dc % 2) * 256:(dc % 2 + 1) * 256]
                                    else:
                                        pt, pr = pg[0]
                                        oap = pt[:, dc * cap:(dc + 1) * cap]
                                    evac(dc, [pr], [xgr], xgt[:, dc, b * cap:(b + 1) * cap], oap)
                        for S in streams:
                            AT[S.name] = actT[S.name].get()
                        wgs = W["w_gate"][l, e].rearrange("(c p) f -> p c f", p=128)
                        wus = W["w_up"][l, e].rearrange("(c p) f -> p c f", p=128)
                        for ft in range(4):
                            wg, wgr = wgb.get()
                            wu, wur = wub.get()
                            load_cast(stage, wg[:], wgr, wgs[:, :, ft * 512:(ft + 1) * 512], [128, 8, 512], cast_i); cast_i += 1
                            load_cast(stage, wu[:], wur, wus[:, :, ft * 512:(ft + 1) * 512], [128, 8, 512], cast_i); cast_i += 1
                            for S in streams:
                                N2 = 2 * S.cap
                                xgt, xgr = XG[S.name]
                                att, atr = AT[S.name]
                                for fq in range(4):
                                    fcx = ft * 4 + fq
                                    pa, par = fw.ps()
                                    pu, pur = fw.ps()
                                    fw.mm([wgr, xgr], [par], pa[:, 0:N2], [(wg[:, dc, fq * 128:(fq + 1) * 128], xgt[:, dc, :]) for dc in range(8)])
                                    fw.mm([wur, xgr], [pur], pu[:, 0:N2], [(wu[:, dc, fq * 128:(fq + 1) * 128], xgt[:, dc, :]) for dc in range(8)])
                                    s, sr = sa.get()
                                    fw.op(act, [par], [sr], "activation", s[:, 0:N2], pa[:, 0:N2], AF.Silu)
                                    fw.op(dve, [pur, sr], [atr], "tensor_tensor", att[:, fcx, :], pu[:, 0:N2], s[:, 0:N2], ALU.mult)
                        wds = W["w_down"][l, e].rearrange("(c p) d -> p c d", p=128)
                        for dh in range(2):
                            wd, wdr = wdb.get()
                            for hf in range(2):
                                load_cast(stage, wd[:, hf * 8:(hf + 1) * 8, :], wdr, wds[:, hf * 8:(hf + 1) * 8, dh * 512:(dh + 1) * 512], [128, 8, 512], cast_i); cast_i += 1
                            for S in streams:
                                att, atr = AT[S.name]
                                cap, SC, SP = S.cap, S.SC, S.SP
                                if S is SX:
                                    pt, pr = fw.ps()
                                    fw.mm([wdr, atr], [pr], pt[0:64, 0:512], [(att[:, fcx, 0:64], wd[:, fcx, :]) for fcx in range(16)])
                                    y, yr = ygb.get()
                                    evac(ev_i, [pr], [yr], y[0:64, :], pt[0:64, 0:512]); ev_i += 1
                                    for b in range(2):
                                        fw.dma([yr], [], S.yg[b, e, 0, :, dh * 512:(dh + 1) * 512], y[b * 32:(b + 1) * 32, :])
                                    continue
                                for b in range(2):
                                    for sc in range(SC):
                                        c0 = b * cap + sc * SP
                                        pt, pr = fw.ps()
                                        fw.mm([wdr, atr], [pr], pt[0:SP, 0:512], [(att[:, fcx, c0:c0 + SP], wd[:, fcx, :]) for fcx in range(16)])
                                        y, yr = ygb.get()
                                        evac(ev_i, [pr], [yr], y[0:SP, :], pt[0:SP, 0:512]); ev_i += 1
                                        fw.dma([yr], [], S.yg[b, e, sc, :, dh * 512:(dh + 1) * 512], y[0:SP, :])
                for S in streams:
                    n, SC, SP, Wn = S.n, S.SC, S.SP, S.W
                    with Phase(fw) as ph:
                        yga = ph.buf("yga", [SP, 16, SC, 1024], BF16, 1)
                        stl = ph.buf("stl", [SP, 16, SC, Wn], BF16, 1 if S is SL else 2)
                        xb = ph.buf("xc", [128, 8, Wn], F32, 2)
                        for b in range(2):
                            yt, yr = yga.get()
                            for e4 in range(4):
                                fw.dma([], [yr], yt[:, e4 * 4:(e4 + 1) * 4, :, :].rearrange("p e s d -> p (e s) d"), S.yg[b, e4 * 4:(e4 + 1) * 4].rearrange("e s p d -> p (e s) d"))
                            col = 2 if S is SX else b
                            for tt in range(n // Wn):
                                sl = slice(b * n + tt * Wn, b * n + (tt + 1) * Wn)
                                st_, sr = stl.get()
                                for e4 in range(4):
                                    fw.dma([], [sr], st_[:, e4 * 4:(e4 + 1) * 4, :, :].rearrange("p e s t -> p (e s) t"),
                                           S.selT[b, e4 * 4:(e4 + 1) * 4, :, :, tt * Wn:(tt + 1) * Wn].rearrange("e s p t -> p (e s) t"))
                                xt, xr = xb.get()
                                fw.dma([], [xr], xt[:], S.xT.rearrange("(c p) t -> p c t", p=128)[:, :, sl])
                                for dc in range(8):
                                    pt, pr = fw.ps()
                                    fw.mm([yr, sr], [pr], pt[:, 0:Wn],
                                          [(yt[:, e, sc, dc * 128:(dc + 1) * 128], st_[:, e, sc, :]) for e in range(16) for sc in range(SC)])
                                    fw.op(dve, [pr, xr, r_mod], [xr], "scalar_tensor_tensor", xt[:, dc, :], pt[:, 0:Wn], mod[:, 40 + dc, col:col + 1], xt[:, dc, :], ALU.mult, ALU.add)
                                fw.dma([xr], [], S.xT.rearrange("(c p) t -> p c t", p=128)[:, :, sl], xt[:])

        if not last:
            attention(SX)
        attention(SL)
        for S in streams:
            hy_spectrum(S)
            hyena(S)
            fnet(S)
        merge()
        moe()

    for l in range(nlayers):
        layer(l)

    with Phase(fw) as ph:
        xin = ph.buf("xf", [128, 8, 512], F32, 2)
        xnb = ph.buf("xn", [128, 8, 512], F32, 2)
        pools = (ph.buf("sqf", [128, 8, 512], BF16, 1), ph.buf("rsf", [128, 512], F32, 2), ph.buf("tmpf", [128, 512], F32, 8))
        ot = ph.buf("ot", [128, 4, 1024], F32, 2)
        oflat = out.rearrange("b t d -> (b t) d")
        for tl in SL.tiles:
            t, r = xin.get()
            fw.dma([], [r], t[:], SL.xT.rearrange("(c p) t -> p c t", p=128)[:, :, tl[0]:tl[0] + 512])
            xn, xnr = xnb.get()
            norm_mod(ph, t, r, lambda c: fing[:, c:c + 1], None, lambda c: xn[:, c, :], xnr, 512, pools)
            o, orr = ot.get()
            k = 0
            for tcq in range(4):
                for hh in range(2):
                    pt, pr = fw.ps()
                    for q in range(4):
                        dc = hh * 4 + q
                        fw.tr([xnr, r_c], [pr], pt[:, q * 128:(q + 1) * 128], xn[:, dc, tcq * 128:(tcq + 1) * 128], ident_f[:])
                    evac(k, [pr], [orr], o[:, tcq, hh * 512:(hh + 1) * 512], pt[:, 0:512])
                    k += 1
            fw.dma([orr], [], oflat[tl[0]:tl[0] + 512, :].rearrange("(c p) d -> p c d", p=128), o[:])
    fw.barrier()
    G.es.close()
    fw.close()
    print("built: n_ins", fw.n_ins, "n_wait", fw.n_wait)
    return nc

from concourse.bass_utils import run_bass_kernel_spmd


def kernel(**inputs):
    inp = {k: np.asarray(v) for k, v in inputs.items()}
    nc = build({"layers": DEPTH})
    shared = {}
    shared.update(shared_consts())
    shared.update(prep_weights(inp))
    in_maps = []
    for core in range(8):
        m = dict(shared)
        m.update(prep_core(inp, core))
        in_maps.append(m)
    res = run_bass_kernel_spmd(nc, in_maps, core_ids=list(range(8)))
    outs = [np.asarray(r["out"], dtype=np.float32) for r in res.results]
    return np.concatenate(outs, axis=0)
```

```python
import contextlib
import numpy as np
import concourse.bass as bass
import concourse.mybir as mybir

F32 = mybir.dt.float32
BF16 = mybir.dt.bfloat16
U32 = mybir.dt.uint32
I32 = mybir.dt.int32
ALU = mybir.AluOpType
AF = mybir.ActivationFunctionType
AX = mybir.AxisListType


class Res:
    __slots__ = ("w", "r")

    def __init__(self):
        self.w = {}
        self.r = {}


class Eng:
    def __init__(self, name, h, sem):
        self.name = name
        self.h = h
        self.sem = sem
        self.cnt = 0
        self.seen = {}


class DmaQ:
    def __init__(self, name, h, sems):
        self.name = name
        self.h = h
        self.sems = sems
        self.j = 0
        self.seen = {}


class FW:
    def __init__(self, nc, n_dma_sems=8):
        self.nc = nc
        self.es = contextlib.ExitStack()
        mk = lambda n: self.es.enter_context(nc.semaphore(n))
        self.pe = Eng("pe", nc.tensor, mk("s_pe"))
        self.act = Eng("act", nc.scalar, mk("s_act"))
        self.dve = Eng("dve", nc.vector, mk("s_dve"))
        self.pool = Eng("pool", nc.gpsimd, mk("s_pool"))
        self.engs = [self.pe, self.act, self.dve, self.pool]
        self.sp = DmaQ("sp", nc.sync, [mk("s_sp%d" % i) for i in range(n_dma_sems)])
        self.qs = [self.sp]
        self.semid = {}
        self.n_wait = 0
        self.n_ins = 0
        self._psum = []
        self._psum_i = 0
        self._ring = list(range(8))

    def sbuf(self, name, shape, dtype):
        return self.es.enter_context(self.nc.sbuf_tensor(name, list(shape), dtype))

    def psum_banks(self):
        for i in range(8):
            t = self.es.enter_context(self.nc.psum_tensor("ps%d" % i, [128, 512], F32))
            self._psum.append((t, Res()))

    def ps(self):
        ring = self._ring
        t, r = self._psum[ring[self._psum_i % len(ring)]]
        self._psum_i += 1
        return t, r

    def ring(self, banks):
        self._ring = list(banks)

    def bank(self, i):
        return self._psum[i]

    def tr(self, reads, writes, out, in_, ident):
        e = self.pe
        self._wait(e, self._deps(reads, writes))
        ins = e.h.transpose(out, in_, ident)
        e.cnt += 1
        ins.then_inc(e.sem, 1)
        self._mark(reads, writes, self._key(e.sem), e.cnt)
        self.n_ins += 1

    def close(self):
        self.es.close()

    def _key(self, sem):
        k = id(sem)
        self.semid[k] = sem
        return k

    def _deps(self, reads, writes):
        d = {}
        for r in reads:
            for k, v in r.w.items():
                if d.get(k, 0) < v:
                    d[k] = v
        for w in writes:
            for k, v in w.w.items():
                if d.get(k, 0) < v:
                    d[k] = v
            for k, v in w.r.items():
                if d.get(k, 0) < v:
                    d[k] = v
        return d

    def _wait(self, e, deps):
        for k, v in deps.items():
            if e is self.pe and self.semid[k] is e.sem:
                continue
            if e.seen.get(k, 0) < v:
                e.h.wait_ge(self.semid[k], v)
                e.seen[k] = v
                self.n_wait += 1

    def _mark(self, reads, writes, k, v):
        for r in reads:
            if r.r.get(k, 0) < v:
                r.r[k] = v
        for w in writes:
            w.w = {k: v}
            w.r = {}

    def op(self, e, reads, writes, name, *a, **kw):
        self._wait(e, self._deps(reads, writes))
        ins = getattr(e.h, name)(*a, **kw)
        e.cnt += 1
        ins.then_inc(e.sem, 1)
        k = self._key(e.sem)
        self._mark(reads, writes, k, e.cnt)
        self.n_ins += 1
        return ins

    def mm(self, reads, writes, out, pairs, start=True, stop=True):
        e = self.pe
        self._wait(e, self._deps(reads, writes))
        ins = None
        n = len(pairs)
        for i, (a, b) in enumerate(pairs):
            ins = e.h.matmul(out, a, b, start=(start and i == 0), stop=(stop and i == n - 1))
            self.n_ins += 1
        e.cnt += 1
        ins.then_inc(e.sem, 1)
        k = self._key(e.sem)
        self._mark(reads, writes, k, e.cnt)

    def dma(self, reads, writes, out, in_, q=None, **kw):
        q = q or self.sp
        deps = self._deps(reads, writes)
        n = len(q.sems)
        slot = q.j % n
        sem = q.sems[slot]
        k = self._key(sem)
        prev = 16 * (q.j // n)
        if prev > 0:
            deps[k] = max(deps.get(k, 0), prev)
        self._wait(q, deps)
        q.h.dma_start(out=out, in_=in_, **kw).then_inc(sem, 16)
        q.j += 1
        self._mark(reads, writes, k, prev + 16)
        self.n_ins += 1

    def barrier(self):
        d = {}
        for e in self.engs:
            if e.cnt:
                d[self._key(e.sem)] = e.cnt
        for q in self.qs:
            n = len(q.sems)
            for s in range(n):
                uses = (q.j - s + n - 1) // n
                if uses > 0:
                    d[self._key(q.sems[s])] = 16 * uses
        for e in self.engs + self.qs:
            self._wait(e, d)

import math
import numpy as np
import ml_dtypes

BF = ml_dtypes.bfloat16
D = 1024
DEPTH = 4
NL = 2048
NCX = 256
HD = 64


def col_order():
    cols = []
    types = []
    sw = lambda base, n: [base + (i ^ 1) for i in range(n)]
    for hp in range(4):
        b = hp * 128
        cols += list(range(b, b + 128)); types.append(("q", hp))
        cols += sw(b, 128); types.append(("qs", hp))
    cols += list(range(512, 640)); types.append(("k", 0))
    cols += sw(512, 128); types.append(("ks", 0))
    cols += list(range(640, 768)); types.append(("v", 0))
    for i in range(2):
        cols += list(range(1536 + i * 128, 1536 + (i + 1) * 128)); types.append(("fn", i))
    for i in range(6):
        cols += list(range(768 + i * 128, 768 + (i + 1) * 128)); types.append(("hy", i))
    for i in range(24):
        cols += list(range(1792 + i * 128, 1792 + (i + 1) * 128)); types.append(("g", i))
    return np.array(cols), types


def pvec(v, nch):
    return np.ascontiguousarray(v.reshape(nch, 128).T)


def tile_stationary(M):
    R, C = M.shape
    return np.ascontiguousarray(M.reshape(R // 128, 128, C // 128, 128).transpose(2, 1, 0, 3))


def tile_moving(M, w):
    R, C = M.shape
    return np.ascontiguousarray(M.reshape(R // 128, 128, C // w, w).transpose(2, 1, 0, 3))


def dft_consts(n, pre):
    a = np.arange(n, dtype=np.float64)
    ang = np.pi * np.outer(a, a) / n
    Cm = np.cos(ang)
    Fs = -np.sin(ang)
    Fs[:, 0] = (-1.0) ** a
    Gs = Fs.T.copy()
    ang2 = 2.0 * ang
    Ct = np.cos(ang2)
    Stn = -np.sin(ang2)
    w = min(512, n)
    c = {}
    c[pre + "C_st"] = tile_stationary(Cm).astype(BF)
    c[pre + "C_mv"] = tile_moving(Cm, w).astype(BF)
    c[pre + "Fs_st"] = tile_stationary(Fs).astype(BF)
    c[pre + "Gs_st"] = tile_stationary(Gs).astype(BF)
    c[pre + "Gs_mv"] = tile_moving(Gs, w).astype(BF)
    c[pre + "Ct_mv"] = tile_moving(Ct, w).astype(BF)
    c[pre + "St_mv"] = tile_moving(Stn, w).astype(BF)
    t = a / (n - 1)
    bands = np.linspace(1e-4, 15.0, 16)
    an = (2.0 * math.pi / n) * np.outer(a, bands)
    feats = np.concatenate([t[:, None], np.cos(an), -np.sin(an)], axis=-1)
    c[pre + "featsT"] = np.ascontiguousarray(feats.T).astype(np.float32)
    deltas = np.abs(np.linspace(math.log(1e-2) / 1.5, math.log(1e-2) / 0.3, 256))
    dec = np.exp(-t[:, None] * deltas[None, :])
    dec2 = np.concatenate([dec, dec], axis=1)
    c[pre + "dec2"] = np.ascontiguousarray(dec2.reshape(n // 128, 128, 512).transpose(1, 0, 2)).astype(np.float32)
    rs = np.zeros((n, 2))
    rs[:, 0] = 1.0 / n
    rs[0, 0] = 0.5 / n
    rs[:, 1] = 1.0 / n
    rs[0, 1] = 0.0
    c[pre + "rs"] = np.ascontiguousarray(rs.reshape(n // 128, 128, 2).transpose(1, 0, 2)).astype(np.float32)
    return c


def shared_consts():
    c = {}
    c.update(dft_consts(NL, "L_"))
    c.update(dft_consts(NCX, "X_"))
    tt = np.arange(NL)
    row = (tt // 64).astype(np.float64)
    col = (tt % 64).astype(np.float64)
    inv = 10000.0 ** (-np.arange(0, 32, 2, dtype=np.float64) / 32)
    ang = np.concatenate([row[:, None] * inv, col[:, None] * inv], axis=-1)
    p = np.arange(128)
    dim = p % 64
    i = dim // 2
    sgn = np.where(dim % 2 == 0, -1.0, 1.0)
    c["ropeC"] = np.cos(ang)[:, i].T.astype(np.float32).copy()
    c["ropeS"] = (np.sin(ang)[:, i] * sgn[None, :]).T.astype(np.float32).copy()
    cc = np.arange(64)
    th = 2 * np.pi * np.outer(cc, cc) / 64
    for pre, n in (("L_", NL), ("X_", NCX)):
        sc = 1.0 / math.sqrt(n * 64)
        Dc = np.zeros((128, 128)); Ds = np.zeros((128, 128))
        for g in range(2):
            Dc[g * 64:(g + 1) * 64, g * 64:(g + 1) * 64] = np.cos(th) * sc
            Ds[g * 64:(g + 1) * 64, g * 64:(g + 1) * 64] = np.sin(th) * sc
        c[pre + "DcDs"] = np.concatenate([Dc, Ds], axis=1).astype(BF)
    bo = np.zeros((128, 128)); bo[:64, :64] = 1; bo[64:, 64:] = 1
    c["blockones"] = bo.astype(BF)
    c["ones_bf"] = np.ones((128, 128)).astype(BF)
    alt = ((-1.0) ** np.arange(128))[:, None]
    c["altcol"] = alt.astype(BF)
    es = np.zeros((128, 32, 128))
    for q in range(32):
        es[q, q, :] = 1
    c["esel"] = es.reshape(128, 32 * 128).astype(BF)
    return c


def prep_weights(inp):
    w = {}
    cols, _ = col_order()
    w["w_mod"] = np.ascontiguousarray(inp["w_mod"])
    w["b_mod"] = np.stack([pvec(inp["b_mod"][l], 48) for l in range(DEPTH)])
    w["n1g"] = np.stack([pvec(inp["norm1_g"][l], 8) for l in range(DEPTH)])
    w["n2g"] = np.stack([pvec(inp["norm2_g"][l], 8) for l in range(DEPTH)])
    w["fing"] = pvec(inp["final_g"], 8)
    w["w_in"] = np.ascontiguousarray(inp["w_in"][:, :, cols])
    p = np.arange(128) % 64
    qg = inp["q_gain"]; kg = inp["k_gain"]
    w["qkg"] = np.ascontiguousarray(np.stack([qg[:, p], qg[:, p ^ 1], kg[:, p], kg[:, p ^ 1]], axis=-1))
    w["hy_sw"] = np.ascontiguousarray(inp["hy_short_w"].reshape(DEPTH, 3, 6, 128).transpose(0, 3, 2, 1))
    w["hy_sb"] = np.stack([pvec(inp["hy_short_b"][l], 6) for l in range(DEPTH)])
    w["hy_w1"] = np.ascontiguousarray(inp["hy_f_w1"])
    w["hy_w2"] = np.ascontiguousarray(inp["hy_f_w2"])
    w["hy_w3"] = np.ascontiguousarray(inp["hy_f_w3"])
    w["hy_b1"] = np.ascontiguousarray(inp["hy_f_b1"][:, :, None])
    w["hy_b2"] = np.ascontiguousarray(inp["hy_f_b2"][:, :, None])
    w["hy_fr"] = np.ascontiguousarray(inp["hy_f_freq"][:, :, None])
    w["hy_bias"] = np.ascontiguousarray(inp["hy_bias"].reshape(DEPTH, 1, 512))
    w["w_branch"] = np.ascontiguousarray(inp["w_branch"])
    w["w_out"] = np.ascontiguousarray(inp["w_out"])
    w["w_router"] = np.ascontiguousarray(inp["w_router"])
    w["w_gate"] = np.ascontiguousarray(inp["w_gate"])
    w["w_up"] = np.ascontiguousarray(inp["w_up"])
    w["w_down"] = np.ascontiguousarray(inp["w_down"])
    return w


def prep_core(inp, core):
    b0 = 2 * core
    m = {}
    m["x"] = np.ascontiguousarray(inp["x"][b0:b0 + 2])
    m["ctx"] = np.ascontiguousarray(inp["ctx"][b0:b0 + 2])
    c3 = np.stack([inp["c"][b0], inp["c"][b0 + 1], inp["c_ctx"]], axis=-1)
    m["cT"] = np.ascontiguousarray(c3.reshape(8, 128, 3).transpose(1, 0, 2))
    return m

import contextlib
import math
import numpy as np

EPS = 1e-6
PI = math.pi


class Buf:
    def __init__(self, ph, name, shape, dt, n=2):
        self.t = []
        self.r = []
        for i in range(n):
            t, r = ph.sb("%s_%d" % (name, i), shape, dt)
            self.t.append(t)
            self.r.append(r)
        self.i = 0

    def get(self):
        i = self.i % len(self.t)
        self.i += 1
        return self.t[i], self.r[i]


class Phase:
    cnt = 0

    def __init__(self, fw):
        self.fw = fw
        self.es = contextlib.ExitStack()

    def __enter__(self):
        return self

    def __exit__(self, *a):
        self.fw.barrier()
        self.es.close()
        return False

    def sb(self, name, shape, dt):
        Phase.cnt += 1
        t = self.es.enter_context(self.fw.nc.sbuf_tensor("%s_%d" % (name, Phase.cnt), list(shape), dt))
        return t, Res()

    def buf(self, name, shape, dt, n=2):
        return Buf(self, name, shape, dt, n)


class Stream:
    def __init__(self, nc, name, n, dumps=()):
        self.name = name
        self.n = n
        self.NT = 2 * n
        self.NC = n // 128
        self.W = min(512, n)
        self.cap = 2 * n // 16
        self.SC = (self.cap + 127) // 128
        self.SP = min(self.cap, 128)
        def d(nm, sh, dt):
            full = "%s_%s" % (name, nm)
            if full in dumps:
                return nc.dram_tensor(full, list(sh), dt, kind="ExternalOutput").ap()
            return nc.dram_tensor(full, list(sh), dt).ap()
        NT = self.NT
        self.xT = d("xT", [1024, NT], F32)
        self.qT = d("qT", [512, NT], BF16)
        self.kT = d("kT", [128, NT], BF16)
        self.v = d("v", [NT, 128], BF16)
        self.hyu = d("hyu", [768, NT], F32)
        self.ucs = d("ucs", [NT, 2, 256], BF16)
        self.sg = d("sg", [3072, NT], BF16)
        self.mixT = d("mixT", [1024, NT], BF16)
        self.specA = d("specA", [n, 512], F32)
        self.specB = d("specB", [n, 512], F32)
        self.specA2 = d("specA2", [128, 512], F32)
        self.h2tok = d("h2tok", [NT, 1024], BF16)
        self.selT = d("selT", [2, 16, self.SC, self.SP, n], BF16)
        self.yg = d("yg", [2, 16, self.SC, self.SP, 1024], BF16)
        if name == "L":
            self.tiles = [(i * 512, i // 4, (i % 4) * 512) for i in range(8)]
        else:
            self.tiles = [(0, None, 0)]


def build(cfg):
    nlayers = cfg.get("layers", 4)
    dumps = cfg.get("dump", [])
    nc = bass.Bass("TRN2", target_bir_lowering=False)
    fw = FW(nc, n_dma_sems=8)
    fw.psum_banks()
    pe, act, dve, pool = fw.pe, fw.act, fw.dve, fw.pool
    fw.gq = DmaQ("gq", nc.gpsimd, [fw.es.enter_context(nc.semaphore("s_gq%d" % i)) for i in range(3)])
    fw.qs.append(fw.gq)

    def ein(name, shape, dt):
        return nc.dram_tensor(name, list(shape), dt, kind="ExternalInput").ap()

    x_in = ein("x", [2, NL, D], F32)
    ctx_in = ein("ctx", [2, NCX, D], F32)
    cT_in = ein("cT", [128, 8, 3], F32)
    W = {}
    wshapes = dict(
        w_mod=([4, 1024, 6144], F32), b_mod=([4, 128, 48], F32), n1g=([4, 128, 8], F32), n2g=([4, 128, 8], F32),
        fing=([128, 8], F32), w_in=([4, 1024, 5504], F32), qkg=([4, 128, 4], F32), hy_sw=([4, 128, 6, 3], F32),
        hy_sb=([4, 128, 6], F32), hy_w1=([4, 33, 64], F32), hy_w2=([4, 64, 64], F32), hy_w3=([4, 64, 1024], F32),
        hy_b1=([4, 64, 1], F32), hy_b2=([4, 64, 1], F32), hy_fr=([4, 64, 1], F32), hy_bias=([4, 1, 512], F32),
        w_branch=([4, 1024, 1024], F32), w_out=([4, 1024, 1024], F32), w_router=([4, 1024, 16], F32),
        w_gate=([4, 16, 1024, 2048], F32), w_up=([4, 16, 1024, 2048], F32), w_down=([4, 16, 2048, 1024], F32))
    for k, (sh, dt) in wshapes.items():
        W[k] = ein(k, sh, dt)
    C = {}
    for pre, n in (("L_", NL), ("X_", NCX)):
        ncn = n // 128
        w = min(512, n)
        for nm in ("C_st", "Fs_st", "Gs_st"):
            C[pre + nm] = ein(pre + nm, [ncn, 128, ncn, 128], BF16)
        for nm in ("C_mv", "Gs_mv", "Ct_mv", "St_mv"):
            C[pre + nm] = ein(pre + nm, [n // w, 128, ncn, w], BF16)
        C[pre + "featsT"] = ein(pre + "featsT", [33, n], F32)
        C[pre + "dec2"] = ein(pre + "dec2", [128, ncn, 512], F32)
        C[pre + "rs"] = ein(pre + "rs", [128, ncn, 2], F32)
        C[pre + "DcDs"] = ein(pre + "DcDs", [128, 256], BF16)
    C["ropeC"] = ein("ropeC", [128, NL], F32)
    C["ropeS"] = ein("ropeS", [128, NL], F32)
    C["blockones"] = ein("blockones", [128, 128], BF16)
    C["ones_bf"] = ein("ones_bf", [128, 128], BF16)
    C["altcol"] = ein("altcol", [128, 1], BF16)
    C["esel"] = ein("esel", [128, 32 * 128], BF16)
    out = nc.dram_tensor("out", [2, NL, D], F32, kind="ExternalOutput").ap()

    SL = Stream(nc, "L", NL, dumps)
    SX = Stream(nc, "X", NCX, dumps)
    dump_aps = {}

    G = Phase(fw)
    ident_f, r_c = G.sb("ident_f", [128, 128], F32)
    ident_b, _ = G.sb("ident_b", [128, 128], BF16)
    ones_bf, _ = G.sb("ones_bf", [128, 128], BF16)
    blockones, _ = G.sb("blockones", [128, 128], BF16)
    altcol, _ = G.sb("altcol", [128, 1], BF16)
    esel, _ = G.sb("esel", [128, 32 * 128], BF16)
    iota_f, _ = G.sb("iota_f", [128, 256], F32)
    iota_p, _ = G.sb("iota_p", [128, 2], F32)
    mask0, _ = G.sb("mask0", [128, 1], F32)
    zrow, _ = G.sb("zrow", [32, NL], F32)
    bmodT, _ = G.sb("bmodT", [128, 4, 48], F32)
    n1g, _ = G.sb("n1g", [128, 4, 8], F32)
    n2g, _ = G.sb("n2g", [128, 4, 8], F32)
    fing, _ = G.sb("fing", [128, 8], F32)
    qkg, _ = G.sb("qkg", [128, 4, 4], F32)
    hysw, _ = G.sb("hysw", [128, 4, 6, 3], F32)
    hysb, _ = G.sb("hysb", [128, 4, 6], F32)
    scT, r_scT = G.sb("scT", [128, 8, 3], F32)
    mod, r_mod = G.sb("mod", [128, 48, 3], F32)
    A1, _ = G.sb("A1", [128, 8, 3], F32)
    A2, _ = G.sb("A2", [128, 8, 3], F32)
    one3, _ = G.sb("one3", [128, 8, 3], F32)

    def ld(t_ap, res, src):
        fw.dma([], [res], t_ap, src)

    ld(ones_bf[:], r_c, C["ones_bf"][:, :])
    ld(blockones[:], r_c, C["blockones"][:, :])
    ld(altcol[:], r_c, C["altcol"][:, :])
    ld(esel[:], r_c, C["esel"][:, :])
    ld(bmodT[:], r_c, W["b_mod"].rearrange("l p c -> p l c"))
    ld(n1g[:], r_c, W["n1g"].rearrange("l p c -> p l c"))
    ld(n2g[:], r_c, W["n2g"].rearrange("l p c -> p l c"))
    ld(fing[:], r_c, W["fing"][:, :])
    ld(qkg[:], r_c, W["qkg"].rearrange("l p c -> p l c"))
    ld(hysw[:].rearrange("p l c j -> p l (c j)"), r_c, W["hy_sw"].rearrange("l p c j -> p l (c j)"))
    ld(hysb[:], r_c, W["hy_sb"].rearrange("l p c -> p l c"))
    ld(scT[:], r_scT, cT_in[:, :, :])
    fw.op(pool, [], [r_c], "iota", iota_f[:], [[1, 256]], base=0, channel_multiplier=0, allow_small_or_imprecise_dtypes=True)
    fw.op(pool, [], [r_c], "iota", iota_p[:], [[128, 2]], base=0, channel_multiplier=1, allow_small_or_imprecise_dtypes=True)
    fw.op(pool, [], [r_c], "memset", zrow[:], 0.0)
    fw.op(pool, [], [r_c], "memset", one3[:], 1.0)
    fw.op(dve, [r_c], [r_c], "tensor_scalar", ident_f[:], iota_f[:, 0:128], iota_p[:, 0:1], None, ALU.is_equal)
    fw.op(dve, [r_c], [r_c], "tensor_copy", ident_b[:], ident_f[:])
    fw.op(dve, [r_c], [r_c], "tensor_single_scalar", mask0[:], iota_p[:, 0:1], 0.5, ALU.is_gt)
    fw.op(act, [r_scT], [r_scT], "activation", scT[:], scT[:], AF.Silu)
    fw.barrier()

    def evac(i, reads, writes, out_ap, in_ap):
        if i % 2 == 0:
            fw.op(act, reads, writes, "activation", out_ap, in_ap, AF.Copy)
        else:
            fw.op(dve, reads, writes, "tensor_copy", out_ap, in_ap)

    with Phase(fw) as ph:
        xin = ph.buf("xin", [128, 4, 1024], F32, 2)
        xo = ph.buf("xo", [128, 8, 512], F32, 2)
        for S, src in ((SL, x_in), (SX, ctx_in)):
            flat = src.rearrange("b t d -> (b t) d")
            for ti in range(S.NT // 512):
                t, r = xin.get()
                fw.dma([], [r], t[:], flat[ti * 512:(ti + 1) * 512, :].rearrange("(c p) d -> p c d", p=128))
                o, orr = xo.get()
                for dc in range(8):
                    pt, pr = fw.ps()
                    for tcq in range(4):
                        fw.tr([r, r_c], [pr], pt[:, tcq * 128:(tcq + 1) * 128], t[:, tcq, dc * 128:(dc + 1) * 128], ident_f[:])
                    evac(dc, [pr], [orr], o[:, dc, :], pt[:, 0:512])
                fw.dma([orr], [], S.xT.rearrange("(c p) t -> p c t", p=128)[:, :, ti * 512:(ti + 1) * 512], o[:])

    def norm_mod(ph, xt, xr, Asc, Bsc, outs, out_res, width, pools):
        sqp, rsp, tmpp = pools
        sq, sqr = sqp.get()
        fw.op(act, [xr], [sqr], "activation", sq[:, :, 0:width], xt[:, :, 0:width], AF.Square)
        pt, pr = fw.ps()
        fw.mm([sqr, r_c], [pr], pt[:, 0:width], [(ones_bf[:], sq[:, c, 0:width]) for c in range(8)])
        rs, rsr = rsp.get()
        fw.op(act, [pr], [rsr], "activation", rs[:, 0:width], pt[:, 0:width], AF.Sqrt, scale=1.0 / 1024, bias=EPS)
        fw.op(dve, [rsr], [rsr], "reciprocal", rs[:, 0:width], rs[:, 0:width])
        for c in range(8):
            tmp, tr_ = tmpp.get()
            fw.op(dve, [xr, rsr], [tr_], "tensor_tensor", tmp[:, 0:width], xt[:, c, 0:width], rs[:, 0:width], ALU.mult)
            if Bsc is None:
                fw.op(act, [tr_, r_c], [out_res], "activation", outs(c), tmp[:, 0:width], AF.Identity, scale=Asc(c))
            else:
                fw.op(act, [tr_, r_c, r_mod], [out_res], "activation", outs(c), tmp[:, 0:width], AF.Identity,
                      scale=Asc(c), bias=Bsc(c))

    def load_cast(stage, dst_ap, dst_res, src_ap, shape, idx):
        fw.dma([], [dst_res], dst_ap, src_ap, q=fw.gq)

    _, ctypes = col_order()

    def layer(l):
        last = (l == nlayers - 1) and (l == DEPTH - 1)
        streams = [SL] if last else [SX, SL]

        with Phase(fw) as ph:
            wst = ph.buf("wm", [128, 8, 512], F32, 2)
            mrow = ph.buf("mrow", [128, 512], F32, 2)
            scTp, r_sp = ph.sb("scTp", [128, 8, 128], F32)
            fw.op(pool, [], [r_sp], "memset", scTp[:], 0.0)
            fw.op(dve, [r_scT, r_sp], [r_sp], "tensor_copy", scTp[:, :, 0:3], scT[:])
            wm = W["w_mod"][l].rearrange("(c p) f -> p c f", p=128)
            for blk in range(12):
                t, r = wst.get()
                fw.dma([], [r], t[:], wm[:, :, blk * 512:(blk + 1) * 512])
                pt, pr = fw.ps()
                fw.mm([r, r_sp], [pr], pt[:, 0:512], [(scTp[:, dc, :], t[:, dc, :]) for dc in range(8)])
                mr_, mrr = mrow.get()
                evac(blk, [pr], [mrr], mr_[:], pt[:, 0:512])
                p2, pr2 = fw.ps()
                for j in range(4):
                    fw.tr([mrr, r_c], [pr2], p2[:, j * 128:(j + 1) * 128], mr_[:, j * 128:(j + 1) * 128], ident_f[:])
                for col in range(3):
                    fw.op(dve, [pr2, r_c], [r_mod], "tensor_tensor", mod[:, blk * 4:(blk + 1) * 4, col],
                          p2[:, 0:512].rearrange("p (a b) -> p a b", a=4)[:, :, col], bmodT[:, l, blk * 4:(blk + 1) * 4], ALU.add)
            for (Ax, ng, base) in ((A1, n1g, 8), (A2, n2g, 32)):
                fw.op(dve, [r_mod], [r_mod], "tensor_tensor", Ax[:], mod[:, base:base + 8, :], one3[:], ALU.add)
                for col in range(3):
                    fw.op(dve, [r_mod, r_c], [r_mod], "tensor_tensor", Ax[:, :, col], Ax[:, :, col], ng[:, l, :], ALU.mult)

        def mcol(tile):
            return 2 if tile[1] is None else tile[1]

        with Phase(fw) as ph:
            hT, r_hT = ph.sb("hT", [128, 8, 4608], BF16)
            ropeC, _ = ph.sb("ropeC", [128, NL], F32)
            ropeS, _ = ph.sb("ropeS", [128, NL], F32)
            dcds = {}
            for S in (SL, SX):
                dcds[S.name], _ = ph.sb("dcds" + S.name, [128, 256], BF16)
                ld(dcds[S.name][:], r_c, C[S.name + "_DcDs"][:, :])
            ld(ropeC[:], r_c, C["ropeC"][:, :])
            ld(ropeS[:], r_c, C["ropeS"][:, :])
            alltiles = [(SL, tl) for tl in SL.tiles] + [(SX, tl) for tl in SX.tiles]
            hoff = {"L": 0, "X": 4096}
            with Phase(fw) as p1:
                xin = p1.buf("xt", [128, 8, 512], F32, 2)
                pools = (p1.buf("sq", [128, 8, 512], BF16, 1), p1.buf("rs", [128, 512], F32, 2), p1.buf("tmp", [128, 512], F32, 8))
                for S, tl in alltiles:
                    t, r = xin.get()
                    fw.dma([], [r], t[:], S.xT.rearrange("(c p) t -> p c t", p=128)[:, :, tl[0]:tl[0] + 512])
                    col = mcol(tl)
                    c0 = hoff[S.name] + tl[0]
                    norm_mod(p1, t, r, lambda c: A1[:, c, col:col + 1], lambda c: mod[:, c, col:col + 1],
                             lambda c: hT[:, c, c0:c0 + 512], r_hT, 512, pools)
            with Phase(fw) as p2:
                stage = None
                wb = p2.buf("wbf", [128, 8, 512], BF16, 3)
                sqb = p2.buf("sqb", [128, 512], BF16, 2)
                rb = p2.buf("rb", [128, 512], F32, 2)
                t1b = p2.buf("t1b", [128, 512], F32, 2)
                t2b = p2.buf("t2b", [128, 512], F32, 2)
                ob = p2.buf("ob", [128, 512], BF16, 3)
                of = p2.buf("of", [128, 512], F32, 2)
                vb = p2.buf("vb", [128, 4, 128], BF16, 2)
                ub = p2.buf("ub", [128, 4, 256], BF16, 2)
                win = W["w_in"][l].rearrange("(c p) f -> p c f", p=128)
                nchunks = len(ctypes)
                for blk in range((nchunks + 3) // 4):
                    nb = min(4, nchunks - blk * 4)
                    wt, wr = wb.get()
                    load_cast(stage, wt[:, :, 0:nb * 128], wr, win[:, :, blk * 512:blk * 512 + nb * 128], [128, 8, nb * 128], blk)
                    for S, tl in alltiles:
                        c0 = hoff[S.name] + tl[0]
                        isx = S is SX
                        pos0 = tl[2]
                        held = None
                        for j in range(nb):
                            ty, idx = ctypes[blk * 4 + j]
                            if isx and ty in ("qs", "ks"):
                                continue
                            if isx and last and ty not in ("k", "v"):
                                continue
                            wj = lambda dc: wt[:, dc, j * 128:(j + 1) * 128]
                            if ty == "v":
                                vt, vr = vb.get()
                                pt, pr = fw.ps()
                                for tcq in range(4):
                                    fw.mm([wr, r_hT], [pr], pt[:, tcq * 128:(tcq + 1) * 128],
                                          [(hT[:, dc, c0 + tcq * 128:c0 + (tcq + 1) * 128], wj(dc)) for dc in range(8)])
                                evac(0, [pr], [vr], vt[:], pt[:, 0:512].rearrange("p (a b) -> p a b", a=4))
                                fw.dma([vr], [], S.v[tl[0]:tl[0] + 512, :].rearrange("(c p) d -> p c d", p=128), vt[:])
                                continue
                            pt, pr = fw.ps()
                            fw.mm([wr, r_hT], [pr], pt[:, 0:512], [(wj(dc), hT[:, dc, c0:c0 + 512]) for dc in range(8)])
                            if ty in ("q", "k"):
                                gi = 0 if ty == "q" else 2
                                dst = S.qT[idx * 128:(idx + 1) * 128, tl[0]:tl[0] + 512] if ty == "q" else S.kT[:, tl[0]:tl[0] + 512]
                                sq, sqr = sqb.get()
                                fw.op(act, [pr], [sqr], "activation", sq[:], pt[:, 0:512], AF.Square)
                                p2, pr2 = fw.ps()
                                fw.mm([sqr, r_c], [pr2], p2[:, 0:512], [(blockones[:], sq[:])])
                                rt, rr = rb.get()
                                fw.op(act, [pr2], [rr], "activation", rt[:], p2[:, 0:512], AF.Sqrt, scale=1.0 / 64, bias=EPS)
                                fw.op(dve, [rr], [rr], "reciprocal", rt[:], rt[:])
                                if isx:
                                    o, orr = ob.get()
                                    fw.op(dve, [pr, rr, r_c], [orr], "scalar_tensor_tensor", o[:], pt[:, 0:512], qkg[:, l, gi:gi + 1], rt[:], ALU.mult, ALU.mult)
                                    fw.dma([orr], [], dst, o[:])
                                else:
                                    t1, t1r = t1b.get()
                                    fw.op(dve, [pr, r_c], [t1r], "scalar_tensor_tensor", t1[:], pt[:, 0:512], qkg[:, l, gi:gi + 1],
                                          ropeC[:, pos0:pos0 + 512], ALU.mult, ALU.mult)
                                    held = (t1, t1r, rt, rr, dst)
                            elif ty in ("qs", "ks"):
                                gi = 1 if ty == "qs" else 3
                                t1, t1r, rt, rr, dst = held
                                t2, t2r = t2b.get()
                                fw.op(dve, [pr, r_c], [t2r], "scalar_tensor_tensor", t2[:], pt[:, 0:512], qkg[:, l, gi:gi + 1],
                                      ropeS[:, pos0:pos0 + 512], ALU.mult, ALU.mult)
                                fw.op(pool, [t1r, t2r], [t2r], "tensor_tensor", t2[:], t1[:], t2[:], ALU.add)
                                o, orr = ob.get()
                                fw.op(pool, [t2r, rr], [orr], "tensor_tensor", o[:], t2[:], rt[:], ALU.mult)
                                fw.dma([orr], [], dst, o[:])
                            elif ty == "hy":
                                o, orr = of.get()
                                evac(j, [pr], [orr], o[:], pt[:, 0:512])
                                fw.dma([orr], [], S.hyu[idx * 128:(idx + 1) * 128, tl[0]:tl[0] + 512], o[:])
                            elif ty == "g":
                                o, orr = ob.get()
                                fw.op(act, [pr], [orr], "activation", o[:], pt[:, 0:512], AF.Sigmoid)
                                fw.dma([orr], [], S.sg[idx * 128:(idx + 1) * 128, tl[0]:tl[0] + 512], o[:])
                            elif ty == "fn":
                                o, orr = ob.get()
                                evac(j, [pr], [orr], o[:], pt[:, 0:512])
                                ut, ur = ub.get()
                                for half in range(2):
                                    p2, pr2 = fw.ps()
                                    for q2 in range(2):
                                        tcq = half * 2 + q2
                                        fw.mm([orr, r_c], [pr2], p2[:, q2 * 256:(q2 + 1) * 256],
                                              [(o[:, tcq * 128:(tcq + 1) * 128], dcds[S.name][:])])
                                    evac(half, [pr2], [ur], ut[:, half * 2:half * 2 + 2, :], p2[:, 0:512].rearrange("p (a b) -> p a b", a=2))
                                fw.dma([ur], [], S.ucs[tl[0]:tl[0] + 512, idx, :].rearrange("(c p) d -> p c d", p=128), ut[:])

        def attention(S):
            n = S.n
            NK = n + (NCX if S is SL else 0)
            NKC = NK // 128
            QW = S.W
            with Phase(fw) as ph:
                kTs = ph.buf("kTs", [128, 2, NK], BF16, 2)
                for i_ in range(2):
                    fw.op(pool, [], [kTs.r[i_]], "memset", kTs.t[i_][:], 0.0)
                vaug = ph.buf("vaug", [128, NKC, 2, 128], BF16, 2)
                for t_ in vaug.t:
                    fw.op(pool, [], [vaug.r[vaug.t.index(t_)]], "memset", t_[:], 1.0)
                qh = ph.buf("qh", [128, n], BF16, 3)
                for i_ in range(3):
                    fw.op(pool, [], [qh.r[i_]], "memset", qh.t[i_][:], 0.0)
                eb = ph.buf("eb", [128, 512], BF16, 6)
                rcp = ph.buf("rcp", [64, 512], F32, 2)
                ao = ph.buf("ao", [64, 512], BF16, 2)
                fw.ring([0, 1, 2, 3, 6, 7])
                obank = 0
                for b in range(2):
                    kt, kr = kTs.get()
                    vt, vr = vaug.get()
                    fw.dma([], [kr], kt[0:64, :, 0:n], S.kT[:, b * n:(b + 1) * n].rearrange("(k d) t -> d k t", k=2))
                    for kk in range(2):
                        fw.dma([], [vr], vt[:, 0:n // 128, kk, 0:64],
                               S.v[b * n:(b + 1) * n, kk * 64:(kk + 1) * 64].rearrange("(c p) d -> p c d", p=128))
                    if S is SL:
                        fw.dma([], [kr], kt[0:64, :, n:NK], SX.kT[:, b * NCX:(b + 1) * NCX].rearrange("(k d) t -> d k t", k=2))
                        for kk in range(2):
                            fw.dma([], [vr], vt[:, n // 128:NKC, kk, 0:64],
                                   SX.v[b * NCX:(b + 1) * NCX, kk * 64:(kk + 1) * 64].rearrange("(c p) d -> p c d", p=128))
                    steps = []
                    for h in range(8):
                        for qi in range(n // QW):
                            for sc in range(NKC):
                                steps.append((h, qi, sc))
                    qcur = {}

                    def get_q(h):
                        if h not in qcur:
                            qt_, qr = qh.get()
                            fw.dma([], [qr], qt_[0:64, :], S.qT[h * 64:(h + 1) * 64, b * n:(b + 1) * n])
                            qcur[h] = (qt_, qr)
                        return qcur[h]

                    def issue_s(st):
                        h, qi, sc = st
                        qt_, qr = get_q(h)
                        pt, pr = fw.ps()
                        fw.mm([kr, qr], [pr], pt[:, 0:QW], [(kt[:, h // 4, sc * 128:(sc + 1) * 128], qt_[:, qi * QW:(qi + 1) * QW])])
                        return pt, pr

                    PRE = 3
                    pend = [issue_s(st) for st in steps[:PRE]]
                    po = por = None
                    for i, (h, qi, sc) in enumerate(steps):
                        kvh = h // 4
                        pt, pr = pend.pop(0)
                        if i + PRE < len(steps):
                            pend.append(issue_s(steps[i + PRE]))
                        if sc == 0:
                            po, por = fw.bank(4 + obank % 2)
                            obank += 1
                        e, er = eb.get()
                        fw.op(act, [pr], [er], "activation", e[:, 0:QW], pt[:, 0:QW], AF.Exp, scale=0.125)
                        fw.mm([vr, er], [por], po[:, 0:QW], [(vt[:, sc, kvh, :], e[:, 0:QW])], start=(sc == 0), stop=(sc == NKC - 1))
                        if sc == NKC - 1:
                            rc, rcr = rcp.get()
                            fw.op(dve, [por], [rcr], "reciprocal", rc[:, 0:QW], po[64:128, 0:QW])
                            a, ar = ao.get()
                            fw.op(dve, [por, rcr], [ar], "tensor_tensor", a[:, 0:QW], po[0:64, 0:QW], rc[:, 0:QW], ALU.mult)
                            fw.dma([ar], [], S.mixT[h * 64:(h + 1) * 64, b * n + qi * QW:b * n + (qi + 1) * QW], a[:, 0:QW])
                fw.ring(range(8))

        def hy_spectrum(S):
            n = S.n
            NCn = S.NC
            Wn = S.W
            pre = S.name + "_"
            with Phase(fw) as ph:
                feats, rf = ph.sb("feats", [33, n], F32)
                w1, rw = ph.sb("w1", [33, 64], F32)
                w2, _ = ph.sb("w2", [64, 64], F32)
                w3, _ = ph.sb("w3", [64, 1024], F32)
                b1, _ = ph.sb("b1", [64, 1], F32)
                b2, _ = ph.sb("b2", [64, 1], F32)
                fr, _ = ph.sb("fr", [64, 1], F32)
                dec2, _ = ph.sb("dec2", [128, NCn, 512], F32)
                rs_, _ = ph.sb("rs", [128, NCn, 2], F32)
                dB, _ = ph.sb("dB", [128, 512], F32)
                ld(feats[:], rw, C[pre + "featsT"][:, :])
                ld(w1[:], rw, W["hy_w1"][l])
                ld(w2[:], rw, W["hy_w2"][l])
                ld(w3[:], rw, W["hy_w3"][l])
                ld(b1[:], rw, W["hy_b1"][l])
                ld(b2[:], rw, W["hy_b2"][l])
                ld(fr[:], rw, W["hy_fr"][l])
                ld(dec2[:], rw, C[pre + "dec2"][:, :, :])
                ld(rs_[:], rw, C[pre + "rs"][:, :, :])
                ld(dB[:], rw, W["hy_bias"][l].partition_broadcast(128))
                fw.op(dve, [rw], [rw], "tensor_tensor", b1[:], b1[:], fr[:], ALU.mult)
                fw.op(dve, [rw], [rw], "tensor_tensor", b2[:], b2[:], fr[:], ALU.mult)
                h1, rh1 = ph.sb("h1", [64, n], F32)
                h2, rh2 = ph.sb("h2", [64, n], F32)
                zb = ph.buf("zb", [64, 512], F32, 2)
                mb = ph.buf("mb", [64, 512], F32, 2)

                def sin_layer(wt, K, src, rsrc, bt, dst, rdst):
                    for ti in range(n // Wn):
                        sl = slice(ti * Wn, (ti + 1) * Wn)
                        pt, pr = fw.ps()
                        fw.mm([rw, rsrc], [pr], pt[0:64, 0:Wn], [(wt[0:K, :], src[0:K, sl])])
                        z, zr = zb.get()
                        fw.op(dve, [pr, rw], [zr], "tensor_scalar", z[:, 0:Wn], pt[0:64, 0:Wn], fr[:, 0:1], bt[:, 0:1], ALU.mult, ALU.add)
                        for rep in range(2):
                            m, mr = mb.get()
                            fw.op(dve, [zr], [mr], "tensor_scalar", m[:, 0:Wn], z[:, 0:Wn], PI, -2 * PI, ALU.is_gt, ALU.mult)
                            fw.op(dve, [zr, mr], [zr], "tensor_tensor", z[:, 0:Wn], z[:, 0:Wn], m[:, 0:Wn], ALU.add)
                            m, mr = mb.get()
                            fw.op(dve, [zr], [mr], "tensor_scalar", m[:, 0:Wn], z[:, 0:Wn], -PI, 2 * PI, ALU.is_lt, ALU.mult)
                            fw.op(dve, [zr, mr], [zr], "tensor_tensor", z[:, 0:Wn], z[:, 0:Wn], m[:, 0:Wn], ALU.add)
                        fw.op(act, [zr], [rdst], "activation", dst[:, sl], z[:, 0:Wn], AF.Sin)

                sin_layer(w1, 33, feats, rw, b1, h1, rh1)
                sin_layer(w2, 64, h1, rh1, b2, h2, rh2)
                gsum, rgs = ph.sb("gsum", [128, NCn, 512], BF16)
                gdif, rgd = ph.sb("gdif", [128, NCn, 512], BF16)
                hfb = ph.buf("hf", [128, 512], F32, 2)
                hbb = ph.buf("hb", [128, 512], F32, 2)
                a1b = ph.buf("a1", [128, 512], F32, 2)
                a2b = ph.buf("a2", [128, 512], BF16, 2)
                a3b = ph.buf("a3", [128, 512], F32, 2)
                fw.ring([0, 1, 2, 3, 4, 5])
                pl1, pl1r = fw.bank(7)
                for mc in range(NCn):
                    pa, par = fw.ps()
                    pb, pbr = fw.ps()
                    fw.mm([rh2, rw], [par], pa[:, 0:512], [(h2[:, mc * 128:(mc + 1) * 128], w3[:, 0:512])])
                    fw.mm([rh2, rw], [pbr], pb[:, 0:512], [(h2[:, mc * 128:(mc + 1) * 128], w3[:, 512:1024])])
                    hf, hfr = hfb.get()
                    hb, hbr = hbb.get()
                    fw.op(dve, [par, rw], [hfr], "tensor_tensor", hf[:], pa[:, 0:512], dec2[:, mc, :], ALU.mult)
                    if mc == 0:
                        fw.op(dve, [pbr, rw, r_c], [hbr], "scalar_tensor_tensor", hb[:], pb[:, 0:512], mask0[:, 0:1], dec2[:, mc, :], ALU.mult, ALU.mult)
                    else:
                        fw.op(dve, [pbr, rw], [hbr], "tensor_tensor", hb[:], pb[:, 0:512], dec2[:, mc, :], ALU.mult)
                    fw.op(pool, [hfr, hbr], [rgs], "tensor_tensor", gsum[:, mc, :], hf[:], hb[:], ALU.add)
                    fw.op(pool, [hfr, hbr], [rgd], "tensor_tensor", gdif[:, mc, :], hf[:], hb[:], ALU.subtract)
                    a1, a1r = a1b.get()
                    a2, a2r = a2b.get()
                    a3, a3r = a3b.get()
                    fw.op(act, [hfr], [a1r], "activation", a1[:], hf[:], AF.Abs)
                    fw.op(act, [hbr], [a3r], "activation", a3[:], hb[:], AF.Abs)
                    fw.op(pool, [a1r, a3r], [a2r], "tensor_tensor", a2[:], a1[:], a3[:], ALU.add)
                    fw.mm([a2r, r_c], [pl1r], pl1[:, 0:512], [(ones_bf[:], a2[:])], start=(mc == 0), stop=(mc == NCn - 1))
                il1, ril = ph.sb("il1", [128, 512], F32)
                fw.op(dve, [pl1r], [ril], "reciprocal", il1[:], pl1[:, 0:512])
                cst = ph.buf("cst", [128, NCn, 128], BF16, 2)
                fst = ph.buf("fst", [128, NCn, 128], BF16, 2)
                ab = ph.buf("ab", [128, 512], F32, 2)
                bb = ph.buf("bb", [128, 512], F32, 2)
                for fc in range(NCn):
                    ct, cr = cst.get()
                    ft, fr_ = fst.get()
                    fw.dma([], [cr], ct[:], C[pre + "C_st"][fc])
                    fw.dma([], [fr_], ft[:], C[pre + "Fs_st"][fc])
                    pa, par = fw.ps()
                    pb, pbr = fw.ps()
                    fw.mm([cr, rgs], [par], pa[:, 0:512], [(ct[:, mc, :], gsum[:, mc, :]) for mc in range(NCn)])
                    fw.mm([fr_, rgd], [pbr], pb[:, 0:512], [(ft[:, mc, :], gdif[:, mc, :]) for mc in range(NCn)])
                    a, ar = ab.get()
                    fw.op(dve, [par, ril], [ar], "tensor_tensor", a[:], pa[:, 0:512], il1[:], ALU.mult)
                    fw.op(pool, [ar, rw], [ar], "tensor_tensor", a[:], a[:], dB[:], ALU.add)
                    fw.op(act, [ar, rw], [ar], "activation", a[:], a[:], AF.Identity, scale=rs_[:, fc, 0:1])
                    bt_, btr = bb.get()
                    fw.op(dve, [pbr, ril, rw], [btr], "scalar_tensor_tensor", bt_[:], pb[:, 0:512], rs_[:, fc, 1:2], il1[:], ALU.mult, ALU.mult)
                    fw.dma([ar], [], S.specA[fc * 128:(fc + 1) * 128, :], a[:])
                    fw.dma([btr], [], S.specB[fc * 128:(fc + 1) * 128, :], bt_[:])
                    if fc == 0:
                        pn, pnr = fw.ps()
                        fw.mm([rgs, r_c], [pnr], pn[0:1, 0:512], [(altcol[:, 0:1], gsum[:, mc, :]) for mc in range(NCn)])
                        a2_, a2r_ = ph.sb("a2c0", [128, 512], F32)
                        nq, nqr = ph.sb("nq", [1, 512], F32)
                        fw.op(dve, [pnr, ril], [nqr], "tensor_tensor", nq[:], pn[0:1, 0:512], il1[0:1, :], ALU.mult)
                        fw.op(dve, [nqr, rw], [nqr], "tensor_tensor", nq[:], nq[:], dB[0:1, :], ALU.add)
                        fw.op(dve, [ar], [a2r_], "tensor_copy", a2_[:], a[:])
                        fw.op(dve, [nqr, a2r_], [a2r_], "tensor_single_scalar", a2_[0:1, :], nq[:], 0.5 / n, ALU.mult)
                        fw.dma([a2r_], [], S.specA2[:, :], a2_[:])
                fw.ring(range(8))

        def hyena(S):
            n = S.n
            NCn = S.NC
            Wn = S.W
            pre = S.name + "_"
            with Phase(fw) as ph:
                vtok, rv = ph.sb("vtok", [128, NCn, 512], BF16)
                g1tok, rg1 = ph.sb("g1tok", [128, NCn, 512], BF16)
                g2T, rg2 = ph.sb("g2T", [128, 4, n], F32)
                ztok, rz = ph.sb("ztok", [128, NCn, 512], BF16)
                Y, rY = ph.sb("Y", [128, NCn, 2, 512], BF16)
                with Phase(fw) as p1:
                    ub = p1.buf("u", [128, n], F32, 2)
                    cb = p1.buf("cv", [128, n], F32, 2)
                    for b in range(2):
                        for hc in range(6):
                            u, ur = ub.get()
                            fw.dma([], [ur], u[:], S.hyu[hc * 128:(hc + 1) * 128, b * n:(b + 1) * n])
                            if hc >= 4:
                                orr = rg2
                                ct_ = None
                            else:
                                ct_, orr = cb.get()

                            def osl(a, b_):
                                return g2T[:, b * 2 + (hc - 4), a:b_] if hc >= 4 else ct_[:, a:b_]
                            fw.op(dve, [ur, r_c], [orr], "tensor_scalar", osl(0, n), u[:], hysw[:, l, hc, 1:2], hysb[:, l, hc:hc + 1], ALU.mult, ALU.add)
                            fw.op(dve, [ur, r_c, orr], [orr], "scalar_tensor_tensor", osl(1, n), u[:, 0:n - 1], hysw[:, l, hc, 0:1], osl(1, n), ALU.mult, ALU.add)
                            fw.op(dve, [ur, r_c, orr], [orr], "scalar_tensor_tensor", osl(0, n - 1), u[:, 1:n], hysw[:, l, hc, 2:3], osl(0, n - 1), ALU.mult, ALU.add)
                            if hc < 4:
                                dst, rdst = (vtok, rv) if hc < 2 else (g1tok, rg1)
                                cbase = b * 256 + (hc % 2) * 128
                                nq = min(4, NCn)
                                for t4 in range(NCn // nq):
                                    pt, pr = fw.ps()
                                    for q in range(nq):
                                        tc = t4 * nq + q
                                        fw.tr([orr, r_c], [pr], pt[:, q * 128:(q + 1) * 128], ct_[:, tc * 128:(tc + 1) * 128], ident_f[:])
                                    evac(t4, [pr], [rdst], dst[:, t4 * nq:t4 * nq + nq, cbase:cbase + 128],
                                         pt[:, 0:nq * 128].rearrange("p (a b) -> p a b", a=nq))
                with Phase(fw) as p2:
                    stA = p2.buf("stA", [128, NCn, 128], BF16, 2)
                    stB = p2.buf("stB", [128, NCn, 128], BF16, 2)
                    spA = p2.buf("spA", [128, 2, 256], F32, 2)
                    spB = p2.buf("spB", [128, 2, 256], F32, 2)
                    spA2 = p2.buf("spA2", [128, 2, 256], F32, 1)
                    t1b = p2.buf("t1", [128, 512], F32, 2)
                    t2b = p2.buf("t2", [128, 512], F32, 2)
                    t3b = p2.buf("t3", [128, 512], F32, 2)
                    t4b = p2.buf("t4", [128, 512], F32, 2)

                    def fwd(src, rsrc, o):
                        for fc in range(NCn):
                            ct, cr = stA.get()
                            ft, fr_ = stB.get()
                            fw.dma([], [cr], ct[:], C[pre + "C_st"][fc])
                            fw.dma([], [fr_], ft[:], C[pre + "Fs_st"][fc])
                            at, ar = spA.get()
                            bt_, br = spB.get()
                            for b in range(2):
                                fw.dma([], [ar], at[:, b, :], S.specA[fc * 128:(fc + 1) * 128, o * 256:(o + 1) * 256])
                                fw.dma([], [br], bt_[:, b, :], S.specB[fc * 128:(fc + 1) * 128, o * 256:(o + 1) * 256])
                            if fc == 0:
                                a2t, a2r = spA2.get()
                                for b in range(2):
                                    fw.dma([], [a2r], a2t[:, b, :], S.specA2[:, o * 256:(o + 1) * 256])
                            else:
                                a2t, a2r = at, ar
                            pzr, pzrr = fw.ps()
                            pzi, pzir = fw.ps()
                            fw.mm([cr, rsrc], [pzrr], pzr[:, 0:512], [(ct[:, tc, :], src[:, tc, :]) for tc in range(NCn)])
                            fw.mm([fr_, rsrc], [pzir], pzi[:, 0:512], [(ft[:, tc, :], src[:, tc, :]) for tc in range(NCn)])
                            af = at[:].rearrange("p a b -> p (a b)")
                            bf = bt_[:].rearrange("p a b -> p (a b)")
                            a2f = a2t[:].rearrange("p a b -> p (a b)")
                            t1, r1 = t1b.get()
                            t2, r2 = t2b.get()
                            t3, r3 = t3b.get()
                            t4, r4 = t4b.get()
                            fw.op(dve, [pzrr, ar], [r1], "tensor_tensor", t1[:], pzr[:, 0:512], af, ALU.mult)
                            fw.op(dve, [pzir, br], [r2], "tensor_tensor", t2[:], pzi[:, 0:512], bf, ALU.mult)
                            fw.op(dve, [pzrr, br], [r3], "tensor_tensor", t3[:], pzr[:, 0:512], bf, ALU.mult)
                            fw.op(dve, [pzir, a2r], [r4], "tensor_tensor", t4[:], pzi[:, 0:512], a2f, ALU.mult)
                            fw.op(pool, [r1, r2], [rY], "tensor_tensor", Y[:, fc, 0, :], t1[:], t2[:], ALU.subtract)
                            fw.op(pool, [r3, r4], [rY], "tensor_tensor", Y[:, fc, 1, :], t3[:], t4[:], ALU.add)

                    fwd(vtok, rv, 0)
                    for tc in range(NCn):
                        ct, cr = stA.get()
                        gt, gr = stB.get()
                        fw.dma([], [cr], ct[:], C[pre + "C_st"][tc])
                        fw.dma([], [gr], gt[:], C[pre + "Gs_st"][tc])
                        pz, pzr_ = fw.ps()
                        fw.mm([cr, gr, rY], [pzr_], pz[:, 0:512],
                              [(ct[:, fc, :], Y[:, fc, 0, :]) for fc in range(NCn)] + [(gt[:, fc, :], Y[:, fc, 1, :]) for fc in range(NCn)])
                        fw.op(dve, [pzr_, rg1], [rz], "tensor_tensor", ztok[:, tc, :], pz[:, 0:512], g1tok[:, tc, :], ALU.mult)
                    fwd(ztok, rz, 1)
                with Phase(fw) as p3:
                    mvA = p3.buf("mvA", [128, NCn, Wn], BF16, 1)
                    mvB = p3.buf("mvB", [128, NCn, Wn], BF16, 1)
                    yo = p3.buf("yo", [128, Wn], BF16, 2)
                    for tt in range(n // Wn):
                        ct, cr = mvA.get()
                        gt, gr = mvB.get()
                        fw.dma([], [cr], ct[:], C[pre + "C_mv"][tt])
                        fw.dma([], [gr], gt[:], C[pre + "Gs_mv"][tt])
                        for bcc in range(4):
                            py, pyr = fw.ps()
                            fw.mm([cr, gr, rY], [pyr], py[:, 0:Wn],
                                  [(Y[:, fc, 0, bcc * 128:(bcc + 1) * 128], ct[:, fc, :]) for fc in range(NCn)] +
                                  [(Y[:, fc, 1, bcc * 128:(bcc + 1) * 128], gt[:, fc, :]) for fc in range(NCn)])
                            o, orr = yo.get()
                            fw.op(dve, [pyr, rg2], [orr], "tensor_tensor", o[:], py[:, 0:Wn], g2T[:, bcc, tt * Wn:(tt + 1) * Wn], ALU.mult)
                            b, cc = bcc // 2, bcc % 2
                            fw.dma([orr], [], S.mixT[512 + cc * 128:512 + (cc + 1) * 128, b * n + tt * Wn:b * n + (tt + 1) * Wn], o[:])

        def fnet(S):
            n = S.n
            NCn = S.NC
            Wn = S.W
            pre = S.name + "_"
            with Phase(fw) as ph:
                us, rus = ph.sb("ucs", [128, 2, NCn, 2, 256], BF16)
                for b in range(2):
                    fw.dma([], [rus], us[:, b, :, :, :].rearrange("p c a d -> p c (a d)"), S.ucs[b * n:(b + 1) * n, :, :].rearrange("(c p) a d -> p c (a d)", p=128))
                mvA = ph.buf("fA", [128, NCn, Wn], BF16, 2)
                mvB = ph.buf("fB", [128, NCn, Wn], BF16, 2)
                yo = ph.buf("fyo", [128, Wn], BF16, 2)
                k = 0
                for kt in range(n // Wn):
                    ct, cr = mvA.get()
                    st_, sr = mvB.get()
                    fw.dma([], [cr], ct[:], C[pre + "Ct_mv"][kt])
                    fw.dma([], [sr], st_[:], C[pre + "St_mv"][kt])
                    for b in range(2):
                        for a in range(2):
                            py, pyr = fw.ps()
                            fw.mm([cr, sr, rus], [pyr], py[:, 0:Wn],
                                  [(us[:, b, tc, a, 0:128], ct[:, tc, :]) for tc in range(NCn)] +
                                  [(us[:, b, tc, a, 128:256], st_[:, tc, :]) for tc in range(NCn)])
                            o, orr = yo.get()
                            evac(k, [pyr], [orr], o[:], py[:, 0:Wn])
                            k += 1
                            fw.dma([orr], [], S.mixT[768 + a * 128:768 + (a + 1) * 128, b * n + kt * Wn:b * n + (kt + 1) * Wn], o[:])

        def merge():
            with Phase(fw) as ph:
                stage = None
                wbr, rwb = ph.sb("wbr", [128, 8, 1024], BF16)
                wo, rwo = ph.sb("wo", [128, 8, 1024], BF16)
                for i, (dst, rd, src) in enumerate(((wbr, rwb, W["w_branch"][l]), (wo, rwo, W["w_out"][l]))):
                    sv = src.rearrange("(c p) f -> p c f", p=128)
                    for hlf in range(2):
                        load_cast(stage, dst[:, :, hlf * 512:(hlf + 1) * 512], rd, sv[:, :, hlf * 512:(hlf + 1) * 512], [128, 8, 512], i * 2 + hlf)
                mixb = ph.buf("mixb", [128, 8, 512], BF16, 2)
                sgb = ph.buf("sgb", [128, 24, 512], BF16, 1)
                xb = ph.buf("xb", [128, 8, 512], F32, 2)
                mT = ph.buf("mT", [128, 8, 512], BF16, 1)
                tb = [ph.buf("mt%d" % i, [128, 512], F32, 2) for i in range(3)]
                for S in streams:
                    for tl in S.tiles:
                        sl = slice(tl[0], tl[0] + 512)
                        col = mcol(tl)
                        mx, mxr = mixb.get()
                        sg, sgr = sgb.get()
                        xt, xr = xb.get()
                        m, mr = mT.get()
                        fw.dma([], [mxr], mx[:], S.mixT.rearrange("(c p) t -> p c t", p=128)[:, :, sl])
                        fw.dma([], [sgr], sg[:], S.sg.rearrange("(c p) t -> p c t", p=128)[:, :, sl])
                        fw.dma([], [xr], xt[:], S.xT.rearrange("(c p) t -> p c t", p=128)[:, :, sl])
                        for dc in range(8):
                            ts = []
                            for bi, (k0, k1) in enumerate(((0, 4), (4, 6), (6, 8))):
                                pt, pr = fw.ps()
                                fw.mm([rwb, mxr], [pr], pt[:, 0:512], [(wbr[:, kc, dc * 128:(dc + 1) * 128], mx[:, kc, :]) for kc in range(k0, k1)])
                                t, tr_ = tb[bi].get()
                                fw.op(dve, [pr, sgr], [tr_], "tensor_tensor", t[:], pt[:, 0:512], sg[:, bi * 8 + dc, :], ALU.mult)
                                ts.append((t, tr_))
                            fw.op(pool, [ts[0][1], ts[1][1]], [ts[1][1]], "tensor_tensor", ts[1][0][:], ts[0][0][:], ts[1][0][:], ALU.add)
                            fw.op(pool, [ts[1][1], ts[2][1]], [mr], "tensor_tensor", m[:, dc, :], ts[1][0][:], ts[2][0][:], ALU.add)
                        for dc in range(8):
                            pt, pr = fw.ps()
                            fw.mm([rwo, mr], [pr], pt[:, 0:512], [(wo[:, kc, dc * 128:(dc + 1) * 128], m[:, kc, :]) for kc in range(8)])
                            fw.op(dve, [pr, xr, r_mod], [xr], "scalar_tensor_tensor", xt[:, dc, :], pt[:, 0:512], mod[:, 16 + dc, col:col + 1], xt[:, dc, :], ALU.mult, ALU.add)
                        fw.dma([xr], [], S.xT.rearrange("(c p) t -> p c t", p=128)[:, :, sl], xt[:])

        def moe():
            with Phase(fw) as PH:
                aff = {}
                for S in streams:
                    aff[S.name] = PH.sb("aff" + S.name, [128, S.NC, 2, 16], F32)
                wr_, rwr = PH.sb("wr", [128, 8, 16], F32)
                ld(wr_[:], rwr, W["w_router"][l].rearrange("(c p) e -> p c e", p=128))
                slotT = {}
                slot_tok = {}
                affb = {}
                affTb = {}
                for S in streams:
                    slotT[S.name] = PH.sb("slotT" + S.name, [128, S.n], BF16)
                    affTb[S.name] = PH.sb("affTb" + S.name, [128, S.n], BF16)
                    fw.op(pool, [], [slotT[S.name][1]], "memset", slotT[S.name][0][:], 0.0)
                    fw.op(pool, [], [affTb[S.name][1]], "memset", affTb[S.name][0][:], 0.0)
                    slot_tok[S.name] = PH.sb("slottok" + S.name, [128, S.NC, 32], F32)
                    affb[S.name] = PH.sb("affb" + S.name, [128, S.NC, 2, 16], BF16)
                with Phase(fw) as ph:
                    xin = ph.buf("xt2", [128, 8, 512], F32, 2)
                    h2b = ph.buf("h2T", [128, 8, 512], F32, 2)
                    pools = (ph.buf("sq2", [128, 8, 512], BF16, 1), ph.buf("rs2", [128, 512], F32, 2), ph.buf("tmp2", [128, 512], F32, 8))
                    hto = ph.buf("hto", [128, 4, 1024], BF16, 2)
                    sm = ph.buf("sm", [128, 4], F32, 4)
                    eb = ph.buf("ex", [128, 16], F32, 4)
                    for S in streams:
                        at, ar = aff[S.name]
                        for tl in S.tiles:
                            t, r = xin.get()
                            fw.dma([], [r], t[:], S.xT.rearrange("(c p) t -> p c t", p=128)[:, :, tl[0]:tl[0] + 512])
                            col = mcol(tl)
                            h2, h2r = h2b.get()
                            norm_mod(ph, t, r, lambda c: A2[:, c, col:col + 1], lambda c: mod[:, 24 + c, col:col + 1],
                                     lambda c: h2[:, c, :], h2r, 512, pools)
                            ho, hor = hto.get()
                            st4 = []
                            for tcq in range(4):
                                tok0 = tl[0] + tcq * 128
                                b = tok0 // S.n
                                tc = (tok0 % S.n) // 128
                                tsl = slice(tcq * 128, (tcq + 1) * 128)
                                pt, pr = fw.ps()
                                fw.mm([h2r, rwr], [pr], pt[:, 0:16], [(h2[:, dc, tsl], wr_[:, dc, :]) for dc in range(8)])
                                s, sr = sm.get()
                                e, er = eb.get()
                                st4.append((b, tc, tsl, pt, pr, s, sr, e, er))
                            for (b, tc, tsl, pt, pr, s, sr, e, er) in st4:
                                fw.op(dve, [pr], [sr], "tensor_reduce", s[:, 0:1], pt[:, 0:16], AX.X, ALU.max, negate=True)
                            for (b, tc, tsl, pt, pr, s, sr, e, er) in st4:
                                fw.op(act, [pr, sr], [er, sr], "activation", e[:], pt[:, 0:16], AF.Exp, bias=s[:, 0:1], accum_out=s[:, 1:2])
                            for (b, tc, tsl, pt, pr, s, sr, e, er) in st4:
                                fw.op(dve, [sr], [sr], "reciprocal", s[:, 2:3], s[:, 1:2])
                            for (b, tc, tsl, pt, pr, s, sr, e, er) in st4:
                                fw.op(dve, [er, sr], [ar], "tensor_scalar", at[:, tc, b, :], e[:], s[:, 2:3], None, ALU.mult)
                            for tcq in range(4):
                                tsl = slice(tcq * 128, (tcq + 1) * 128)
                                for hh in range(2):
                                    p2, pr2 = fw.ps()
                                    for q in range(4):
                                        dc = hh * 4 + q
                                        fw.tr([h2r, r_c], [pr2], p2[:, q * 128:(q + 1) * 128], h2[:, dc, tsl], ident_f[:])
                                    evac(hh, [pr2], [hor], ho[:, tcq, hh * 512:(hh + 1) * 512], p2[:, 0:512])
                            fw.dma([hor], [], S.h2tok[tl[0]:tl[0] + 512, :].rearrange("(c p) d -> p c d", p=128), ho[:])
                with Phase(fw) as ph:
                    for S in streams:
                        n = S.n
                        at, ar = aff[S.name]
                        affT, rat = ph.sb("affT", [32, n], F32)
                        wk = ph.buf("wk", [32, n], F32, 2)
                        m8, rm8 = ph.sb("m8", [32, 8], F32)
                        msk, rmsk = ph.sb("msk", [32, n], F32)
                        rnk, rrnk = ph.sb("rnk", [32, n], F32)
                        slf, rslf = ph.sb("slf", [32, n], F32)
                        fw.op(dve, [ar], [affb[S.name][1]], "tensor_copy", affb[S.name][0][:], at[:])
                        for t4 in range(max(1, S.NC // 4)):
                            nq = min(4, S.NC)
                            pt, pr = fw.ps()
                            for q in range(nq):
                                tc = t4 * 4 + q
                                fw.tr([ar, r_c], [pr], pt[0:32, q * 128:(q + 1) * 128], at[:, tc, :, :].rearrange("p a b -> p (a b)"), ident_f[:])
                            evac(t4, [pr], [rat], affT[:, t4 * 512:t4 * 512 + nq * 128], pt[0:32, 0:nq * 128])
                        cur, curr = affT, rat
                        nit = S.cap // 8
                        for it in range(nit):
                            fw.op(dve, [curr], [rm8], "max", m8[:], cur[:])
                            if it < nit - 1:
                                nx, nxr = wk.get()
                                fw.op(dve, [curr, rm8], [nxr], "match_replace", nx[:], m8[:], cur[:], -1e30)
                                cur, curr = nx, nxr
                        fw.op(dve, [rat, rm8], [rmsk], "tensor_scalar", msk[:], affT[:], m8[:, 7:8], None, ALU.is_ge)
                        fw.op(dve, [rmsk, r_c], [rrnk], "tensor_tensor_scan", rnk[:], msk[:], zrow[:, 0:n], 0.0, ALU.add, ALU.add)
                        fw.op(dve, [rrnk], [rslf], "tensor_single_scalar", slf[:], rnk[:], float(S.cap) + 0.5, ALU.is_lt)
                        fw.op(dve, [rslf, rmsk], [rmsk], "tensor_tensor", msk[:], msk[:], slf[:], ALU.mult)
                        fw.op(dve, [rrnk, rmsk], [rslf], "tensor_tensor", slf[:], rnk[:], msk[:], ALU.mult)
                        fw.op(dve, [rslf], [rslf], "tensor_single_scalar", slf[:], slf[:], -1.0, ALU.add)
                        st_, rst = slotT[S.name]
                        fw.op(dve, [rslf], [rst], "tensor_copy", st_[0:32, :], slf[:])
                        fw.op(dve, [rat], [affTb[S.name][1]], "tensor_copy", affTb[S.name][0][0:32, :], affT[:])
                        sk, rsk = slot_tok[S.name]
                        for t4 in range(max(1, S.NC // 4)):
                            nq = min(4, S.NC)
                            pt, pr = fw.ps()
                            for q in range(nq):
                                tc = t4 * 4 + q
                                fw.tr([rslf, r_c], [pr], pt[:, q * 32:(q + 1) * 32], slf[:, tc * 128:(tc + 1) * 128], ident_f[0:32, 0:32])
                            evac(t4, [pr], [rsk], sk[:, t4 * 4:t4 * 4 + nq, :], pt[:, 0:nq * 32].rearrange("p (a b) -> p a b", a=nq))
                with Phase(fw) as ph:
                    stage = None
                    wgb = ph.buf("wg", [128, 8, 512], BF16, 3)
                    wub = ph.buf("wu", [128, 8, 512], BF16, 3)
                    wdb = ph.buf("wd", [128, 16, 512], BF16, 2)
                    sel = {}
                    selT = {}
                    xg = {}
                    actT = {}
                    wsl = {}
                    for S in streams:
                        sel[S.name] = ph.buf("sel" + S.name, [128, S.NC, S.cap], BF16, 1)
                        selT[S.name] = ph.buf("selT" + S.name, [S.SP, S.SC, S.n], BF16, 1)
                        xg[S.name] = ph.buf("xg" + S.name, [128, 8, 2 * S.cap], BF16, 1)
                        actT[S.name] = ph.buf("actT" + S.name, [128, 16, 2 * S.cap], BF16, 1)
                    h2t = ph.buf("h2t", [128, 16, 1024], BF16, 1)
                    abuf = ph.buf("abuf", [128, 512], F32, 2)
                    s0buf = ph.buf("s0buf", [128, 512], BF16, 3)
                    sa = ph.buf("sa", [128, 512], F32, 2)
                    ygb = ph.buf("ygb", [128, 512], BF16, 3)
                    cast_i = 0
                    ev_i = 0
                    for e in range(16):
                        XG = {}
                        AT = {}
                        WS = {}
                        for S in streams:
                            n, cap, SC, SP, NCn = S.n, S.cap, S.SC, S.SP, S.NC
                            xgt, xgr = xg[S.name].get()
                            XG[S.name] = (xgt, xgr)
                            sk, rsk = slot_tok[S.name]
                            st_, rst = slotT[S.name]
                            ab_, rab = affb[S.name]
                            for b in range(2):
                                q = b * 16 + e
                                se, ser = sel[S.name].get()
                                for tc in range(NCn):
                                    fw.op(dve, [rsk, r_c], [ser], "tensor_scalar", se[:, tc, :], iota_f[:, 0:cap], sk[:, tc, q:q + 1], None, ALU.is_equal)
                                sT, sTr = selT[S.name].get()
                                aTb, raT = affTb[S.name]
                                for tt in range(n // S.W):
                                    cs = slice(tt * S.W, (tt + 1) * S.W)
                                    pt, pr = fw.ps()
                                    fw.mm([rst, r_c], [pr], pt[:, 0:S.W], [(esel[:, q * 128:(q + 1) * 128], st_[:, cs])])
                                    pa, par = fw.ps()
                                    fw.mm([raT, r_c], [par], pa[:, 0:S.W], [(esel[:, q * 128:(q + 1) * 128], aTb[:, cs])])
                                    ab, abr = abuf.get()
                                    fw.op(act, [par], [abr], "activation", ab[0:SP, 0:S.W], pa[0:SP, 0:S.W], AF.Copy)
                                    for sc in range(SC):
                                        s0, s0r = s0buf.get()
                                        fw.op(dve, [pr, r_c], [s0r], "tensor_scalar", s0[0:SP, 0:S.W], pt[0:SP, 0:S.W],
                                              iota_p[0:SP, sc:sc + 1], None, ALU.is_equal)
                                        fw.op(pool, [s0r, abr], [sTr], "tensor_tensor", sT[:, sc, cs], s0[0:SP, 0:S.W], ab[0:SP, 0:S.W], ALU.mult)
                                fw.dma([sTr], [], S.selT[b, e].rearrange("s p t -> p s t"), sT[:])
                                pg = [fw.ps() for _ in range(4)] if cap == 256 else [fw.ps()]
                                ngrp = max(1, NCn // 4)
                                nq = min(4, NCn)
                                ht, hr = h2t.get()
                                for g in range(ngrp):
                                    fw.dma([], [hr], ht[:, g * nq:(g + 1) * nq, :], S.h2tok[b * n + g * 512:b * n + g * 512 + nq * 128, :].rearrange("(c p) d -> p c d", p=128))
                                for dc in range(8):
                                    if cap == 256:
                                        pt, pr = pg[dc // 2]
                                        oap = pt[:, (dc % 2) * 256:(dc % 2 + 1) * 256]
                                    else:
                                        pt, pr = pg[0]
                                        oap = pt[:, dc * cap:(dc + 1) * cap]
                                    fw.mm([hr, ser], [pr], oap, [(ht[:, tc, dc * 128:(dc + 1) * 128], se[:, tc, :]) for tc in range(NCn)])
                                for dc in range(8):
                                    if cap == 256:
                                        pt, pr = pg[dc // 2]
                                        oap = pt[:, (dc % 2) * 256:(dc % 2 + 1) * 256]
                                    else:
                                        pt, pr = pg[0]
                                        oap = pt[:, dc * cap:(dc + 1) * cap]
                                    evac(dc, [pr], [xgr], xgt[:, dc, b * cap:(b + 1) * cap], oap)
                        for S in streams:
                            AT[S.name] = actT[S.name].get()
                        wgs = W["w_gate"][l, e].rearrange("(c p) f -> p c f", p=128)
                        wus = W["w_up"][l, e].rearrange("(c p) f -> p c f", p=128)
                        for ft in range(4):
                            wg, wgr = wgb.get()
                            wu, wur = wub.get()
                            load_cast(stage, wg[:], wgr, wgs[:, :, ft * 512:(ft + 1) * 512], [128, 8, 512], cast_i); cast_i += 1
                            load_cast(stage, wu[:], wur, wus[:, :, ft * 512:(ft + 1) * 512], [128, 8, 512], cast_i); cast_i += 1
                            for S in streams:
                                N2 = 2 * S.cap
                                xgt, xgr = XG[S.name]
                                att, atr = AT[S.name]
                                for fq in range(4):
                                    fcx = ft * 4 + fq
                                    pa, par = fw.ps()
                                    pu, pur = fw.ps()
                                    fw.mm([wgr, xgr], [par], pa[:, 0:N2], [(wg[:, dc, fq * 128:(fq + 1) * 128], xgt[:, dc, :]) for dc in range(8)])
                                    fw.mm([wur, xgr], [pur], pu[:, 0:N2], [(wu[:, dc, fq * 128:(fq + 1) * 128], xgt[:, dc, :]) for dc in range(8)])
                                    s, sr = sa.get()
                                    fw.op(act, [par], [sr], "activation", s[:, 0:N2], pa[:, 0:N2], AF.Silu)
                                    fw.op(dve, [pur, sr], [atr], "tensor_tensor", att[:, fcx, :], pu[:, 0:N2], s[:, 0:N2], ALU.mult)
                        wds = W["w_down"][l, e].rearrange("(c p) d -> p c d", p=128)
                        for dh in range(2):
                            wd, wdr = wdb.get()
                            for hf in range(2):
                                load_cast(stage, wd[:, hf * 8:(hf + 1) * 8, :], wdr, wds[:, hf * 8:(hf + 1) * 8, dh * 512:(dh + 1) * 512], [128, 8, 512], cast_i); cast_i += 1
                            for S in streams:
                                att, atr = AT[S.name]
                                cap, SC, SP = S.cap, S.SC, S.SP
                                if S is SX:
                                    pt, pr = fw.ps()
                                    fw.mm([wdr, atr], [pr], pt[0:64, 0:512], [(att[:, fcx, 0:64], wd[:, fcx, :]) for fcx in range(16)])
                                    y, yr = ygb.get()
                                    evac(ev_i, [pr], [yr], y[0:64, :], pt[0:64, 0:512]); ev_i += 1
                                    for b in range(2):
                                        fw.dma([yr], [], S.yg[b, e, 0, :, dh * 512:(dh + 1) * 512], y[b * 32:(b + 1) * 32, :])
                                    continue
                                for b in range(2):
                                    for sc in range(SC):
                                        c0 = b * cap + sc * SP
                                        pt, pr = fw.ps()
                                        fw.mm([wdr, atr], [pr], pt[0:SP, 0:512], [(att[:, fcx, c0:c0 + SP], wd[:, fcx, :]) for fcx in range(16)])
                                        y, yr = ygb.get()
                                        evac(ev_i, [pr], [yr], y[0:SP, :], pt[0:SP, 0:512]); ev_i += 1
                                        fw.dma([yr], [], S.yg[b, e, sc, :, dh * 512:(dh + 1) * 512], y[0:SP, :])
                for S in streams:
                    n, SC, SP, Wn = S.n, S.SC, S.SP, S.W
                    with Phase(fw) as ph:
                        yga = ph.buf("yga", [SP, 16, SC, 1024], BF16, 1)
                        stl = ph.buf("stl", [SP, 16, SC, Wn], BF16, 1 if S is SL else 2)
                        xb = ph.buf("xc", [128, 8, Wn], F32, 2)
                        for b in range(2):
                            yt, yr = yga.get()
                            for e4 in range(4):
                                fw.dma([], [yr], yt[:, e4 * 4:(e4 + 1) * 4, :, :].rearrange("p e s d -> p (e s) d"), S.yg[b, e4 * 4:(e4 + 1) * 4].rearrange("e s p d -> p (e s) d"))
                            col = 2 if S is SX else b
                            for tt in range(n // Wn):
                                sl = slice(b * n + tt * Wn, b * n + (tt + 1) * Wn)
                                st_, sr = stl.get()
                                for e4 in range(4):
                                    fw.dma([], [sr], st_[:, e4 * 4:(e4 + 1) * 4, :, :].rearrange("p e s t -> p (e s) t"),
                                           S.selT[b, e4 * 4:(e4 + 1) * 4, :, :, tt * Wn:(tt + 1) * Wn].rearrange("e s p t -> p (e s) t"))
                                xt, xr = xb.get()
                                fw.dma([], [xr], xt[:], S.xT.rearrange("(c p) t -> p c t", p=128)[:, :, sl])
                                for dc in range(8):
                                    pt, pr = fw.ps()
                                    fw.mm([yr, sr], [pr], pt[:, 0:Wn],
                                          [(yt[:, e, sc, dc * 128:(dc + 1) * 128], st_[:, e, sc, :]) for e in range(16) for sc in range(SC)])
                                    fw.op(dve, [pr, xr, r_mod], [xr], "scalar_tensor_tensor", xt[:, dc, :], pt[:, 0:Wn], mod[:, 40 + dc, col:col + 1], xt[:, dc, :], ALU.mult, ALU.add)
                                fw.dma([xr], [], S.xT.rearrange("(c p) t -> p c t", p=128)[:, :, sl], xt[:])

        if not last:
            attention(SX)
        attention(SL)
        for S in streams:
            hy_spectrum(S)
            hyena(S)
            fnet(S)
        merge()
        moe()

    for l in range(nlayers):
        layer(l)

    with Phase(fw) as ph:
        xin = ph.buf("xf", [128, 8, 512], F32, 2)
        xnb = ph.buf("xn", [128, 8, 512], F32, 2)
        pools = (ph.buf("sqf", [128, 8, 512], BF16, 1), ph.buf("rsf", [128, 512], F32, 2), ph.buf("tmpf", [128, 512], F32, 8))
        ot = ph.buf("ot", [128, 4, 1024], F32, 2)
        oflat = out.rearrange("b t d -> (b t) d")
        for tl in SL.tiles:
            t, r = xin.get()
            fw.dma([], [r], t[:], SL.xT.rearrange("(c p) t -> p c t", p=128)[:, :, tl[0]:tl[0] + 512])
            xn, xnr = xnb.get()
            norm_mod(ph, t, r, lambda c: fing[:, c:c + 1], None, lambda c: xn[:, c, :], xnr, 512, pools)
            o, orr = ot.get()
            k = 0
            for tcq in range(4):
                for hh in range(2):
                    pt, pr = fw.ps()
                    for q in range(4):
                        dc = hh * 4 + q
                        fw.tr([xnr, r_c], [pr], pt[:, q * 128:(q + 1) * 128], xn[:, dc, tcq * 128:(tcq + 1) * 128], ident_f[:])
                    evac(k, [pr], [orr], o[:, tcq, hh * 512:(hh + 1) * 512], pt[:, 0:512])
                    k += 1
            fw.dma([orr], [], oflat[tl[0]:tl[0] + 512, :].rearrange("(c p) d -> p c d", p=128), o[:])
    fw.barrier()
    G.es.close()
    fw.close()
    print("built: n_ins", fw.n_ins, "n_wait", fw.n_wait)
    return nc

from concourse.bass_utils import run_bass_kernel_spmd


def kernel(**inputs):
    inp = {k: np.asarray(v) for k, v in inputs.items()}
    nc = build({"layers": DEPTH})
    shared = {}
    shared.update(shared_consts())
    shared.update(prep_weights(inp))
    in_maps = []
    for core in range(8):
        m = dict(shared)
        m.update(prep_core(inp, core))
        in_maps.append(m)
    res = run_bass_kernel_spmd(nc, in_maps, core_ids=list(range(8)))
    outs = [np.asarray(r["out"], dtype=np.float32) for r in res.results]
    return np.concatenate(outs, axis=0)
```

```python
import contextlib
import numpy as np
import concourse.bass as bass
import concourse.mybir as mybir

F32 = mybir.dt.float32
BF16 = mybir.dt.bfloat16
U32 = mybir.dt.uint32
I32 = mybir.dt.int32
ALU = mybir.AluOpType
AF = mybir.ActivationFunctionType
AX = mybir.AxisListType


class Res:
    __slots__ = ("w", "r")

    def __init__(self):
        self.w = {}
        self.r = {}


class Eng:
    def __init__(self, name, h, sem):
        self.name = name
        self.h = h
        self.sem = sem
        self.cnt = 0
        self.seen = {}


class DmaQ:
    def __init__(self, name, h, sems):
        self.name = name
        self.h = h
        self.sems = sems
        self.j = 0
        self.seen = {}


class FW:
    def __init__(self, nc, n_dma_sems=8):
        self.nc = nc
        self.es = contextlib.ExitStack()
        mk = lambda n: self.es.enter_context(nc.semaphore(n))
        self.pe = Eng("pe", nc.tensor, mk("s_pe"))
        self.act = Eng("act", nc.scalar, mk("s_act"))
        self.dve = Eng("dve", nc.vector, mk("s_dve"))
        self.pool = Eng("pool", nc.gpsimd, mk("s_pool"))
        self.engs = [self.pe, self.act, self.dve, self.pool]
        self.sp = DmaQ("sp", nc.sync, [mk("s_sp%d" % i) for i in range(n_dma_sems)])
        self.qs = [self.sp]
        self.semid = {}
        self.n_wait = 0
        self.n_ins = 0
        self._psum = []
        self._psum_i = 0
        self._ring = list(range(8))

    def sbuf(self, name, shape, dtype):
        return self.es.enter_context(self.nc.sbuf_tensor(name, list(shape), dtype))

    def psum_banks(self):
        for i in range(8):
            t = self.es.enter_context(self.nc.psum_tensor("ps%d" % i, [128, 512], F32))
            self._psum.append((t, Res()))

    def ps(self):
        ring = self._ring
        t, r = self._psum[ring[self._psum_i % len(ring)]]
        self._psum_i += 1
        return t, r

    def ring(self, banks):
        self._ring = list(banks)

    def bank(self, i):
        return self._psum[i]

    def tr(self, reads, writes, out, in_, ident):
        e = self.pe
        self._wait(e, self._deps(reads, writes))
        ins = e.h.transpose(out, in_, ident)
        e.cnt += 1
        ins.then_inc(e.sem, 1)
        self._mark(reads, writes, self._key(e.sem), e.cnt)
        self.n_ins += 1

    def close(self):
        self.es.close()

    def _key(self, sem):
        k = id(sem)
        self.semid[k] = sem
        return k

    def _deps(self, reads, writes):
        d = {}
        for r in reads:
            for k, v in r.w.items():
                if d.get(k, 0) < v:
                    d[k] = v
        for w in writes:
            for k, v in w.w.items():
                if d.get(k, 0) < v:
                    d[k] = v
            for k, v in w.r.items():
                if d.get(k, 0) < v:
                    d[k] = v
        return d

    def _wait(self, e, deps):
        for k, v in deps.items():
            if e is self.pe and self.semid[k] is e.sem:
                continue
            if e.seen.get(k, 0) < v:
                e.h.wait_ge(self.semid[k], v)
                e.seen[k] = v
                self.n_wait += 1

    def _mark(self, reads, writes, k, v):
        for r in reads:
            if r.r.get(k, 0) < v:
                r.r[k] = v
        for w in writes:
            w.w = {k: v}
            w.r = {}

    def op(self, e, reads, writes, name, *a, **kw):
        self._wait(e, self._deps(reads, writes))
        ins = getattr(e.h, name)(*a, **kw)
        e.cnt += 1
        ins.then_inc(e.sem, 1)
        k = self._key(e.sem)
        self._mark(reads, writes, k, e.cnt)
        self.n_ins += 1
        return ins

    def mm(self, reads, writes, out, pairs, start=True, stop=True):
        e = self.pe
        self._wait(e, self._deps(reads, writes))
        ins = None
        n = len(pairs)
        for i, (a, b) in enumerate(pairs):
            ins = e.h.matmul(out, a, b, start=(start and i == 0), stop=(stop and i == n - 1))
            self.n_ins += 1
        e.cnt += 1
        ins.then_inc(e.sem, 1)
        k = self._key(e.sem)
        self._mark(reads, writes, k, e.cnt)

    def dma(self, reads, writes, out, in_, q=None, **kw):
        q = q or self.sp
        deps = self._deps(reads, writes)
        n = len(q.sems)
        slot = q.j % n
        sem = q.sems[slot]
        k = self._key(sem)
        prev = 16 * (q.j // n)
        if prev > 0:
            deps[k] = max(deps.get(k, 0), prev)
        self._wait(q, deps)
        q.h.dma_start(out=out, in_=in_, **kw).then_inc(sem, 16)
        q.j += 1
        self._mark(reads, writes, k, prev + 16)
        self.n_ins += 1

    def barrier(self):
        d = {}
        for e in self.engs:
            if e.cnt:
                d[self._key(e.sem)] = e.cnt
        for q in self.qs:
            n = len(q.sems)
            for s in range(n):
                uses = (q.j - s + n - 1) // n
                if uses > 0:
                    d[self._key(q.sems[s])] = 16 * uses
        for e in self.engs + self.qs:
            self._wait(e, d)

import math
import numpy as np
import ml_dtypes

BF = ml_dtypes.bfloat16
D = 1024
DEPTH = 4
NL = 2048
NCX = 256
HD = 64


def col_order():
    cols = []
    types = []
    sw = lambda base, n: [base + (i ^ 1) for i in range(n)]
    for hp in range(4):
        b = hp * 128
        cols += list(range(b, b + 128)); types.append(("q", hp))
        cols += sw(b, 128); types.append(("qs", hp))
    cols += list(range(512, 640)); types.append(("k", 0))
    cols += sw(512, 128); types.append(("ks", 0))
    cols += list(range(640, 768)); types.append(("v", 0))
    for i in range(2):
        cols += list(range(1536 + i * 128, 1536 + (i + 1) * 128)); types.append(("fn", i))
    for i in range(6):
        cols += list(range(768 + i * 128, 768 + (i + 1) * 128)); types.append(("hy", i))
    for i in range(24):
        cols += list(range(1792 + i * 128, 1792 + (i + 1) * 128)); types.append(("g", i))
    return np.array(cols), types


def pvec(v, nch):
    return np.ascontiguousarray(v.reshape(nch, 128).T)


def tile_stationary(M):
    R, C = M.shape
    return np.ascontiguousarray(M.reshape(R // 128, 128, C // 128, 128).transpose(2, 1, 0, 3))


def tile_moving(M, w):
    R, C = M.shape
    return np.ascontiguousarray(M.reshape(R // 128, 128, C // w, w).transpose(2, 1, 0, 3))


def dft_consts(n, pre):
    a = np.arange(n, dtype=np.float64)
    ang = np.pi * np.outer(a, a) / n
    Cm = np.cos(ang)
    Fs = -np.sin(ang)
    Fs[:, 0] = (-1.0) ** a
    Gs = Fs.T.copy()
    ang2 = 2.0 * ang
    Ct = np.cos(ang2)
    Stn = -np.sin(ang2)
    w = min(512, n)
    c = {}
    c[pre + "C_st"] = tile_stationary(Cm).astype(BF)
    c[pre + "C_mv"] = tile_moving(Cm, w).astype(BF)
    c[pre + "Fs_st"] = tile_stationary(Fs).astype(BF)
    c[pre + "Gs_st"] = tile_stationary(Gs).astype(BF)
    c[pre + "Gs_mv"] = tile_moving(Gs, w).astype(BF)
    c[pre + "Ct_mv"] = tile_moving(Ct, w).astype(BF)
    c[pre + "St_mv"] = tile_moving(Stn, w).astype(BF)
    t = a / (n - 1)
    bands = np.linspace(1e-4, 15.0, 16)
    an = (2.0 * math.pi / n) * np.outer(a, bands)
    feats = np.concatenate([t[:, None], np.cos(an), -np.sin(an)], axis=-1)
    c[pre + "featsT"] = np.ascontiguousarray(feats.T).astype(np.float32)
    deltas = np.abs(np.linspace(math.log(1e-2) / 1.5, math.log(1e-2) / 0.3, 256))
    dec = np.exp(-t[:, None] * deltas[None, :])
    dec2 = np.concatenate([dec, dec], axis=1)
    c[pre + "dec2"] = np.ascontiguousarray(dec2.reshape(n // 128, 128, 512).transpose(1, 0, 2)).astype(np.float32)
    rs = np.zeros((n, 2))
    rs[:, 0] = 1.0 / n
    rs[0, 0] = 0.5 / n
    rs[:, 1] = 1.0 / n
    rs[0, 1] = 0.0
    c[pre + "rs"] = np.ascontiguousarray(rs.reshape(n // 128, 128, 2).transpose(1, 0, 2)).astype(np.float32)
    return c


def shared_consts():
    c = {}
    c.update(dft_consts(NL, "L_"))
    c.update(dft_consts(NCX, "X_"))
    tt = np.arange(NL)
    row = (tt // 64).astype(np.float64)
    col = (tt % 64).astype(np.float64)
    inv = 10000.0 ** (-np.arange(0, 32, 2, dtype=np.float64) / 32)
    ang = np.concatenate([row[:, None] * inv, col[:, None] * inv], axis=-1)
    p = np.arange(128)
    dim = p % 64
    i = dim // 2
    sgn = np.where(dim % 2 == 0, -1.0, 1.0)
    c["ropeC"] = np.cos(ang)[:, i].T.astype(np.float32).copy()
    c["ropeS"] = (np.sin(ang)[:, i] * sgn[None, :]).T.astype(np.float32).copy()
    cc = np.arange(64)
    th = 2 * np.pi * np.outer(cc, cc) / 64
    for pre, n in (("L_", NL), ("X_", NCX)):
        sc = 1.0 / math.sqrt(n * 64)
        Dc = np.zeros((128, 128)); Ds = np.zeros((128, 128))
        for g in range(2):
            Dc[g * 64:(g + 1) * 64, g * 64:(g + 1) * 64] = np.cos(th) * sc
            Ds[g * 64:(g + 1) * 64, g * 64:(g + 1) * 64] = np.sin(th) * sc
        c[pre + "DcDs"] = np.concatenate([Dc, Ds], axis=1).astype(BF)
    bo = np.zeros((128, 128)); bo[:64, :64] = 1; bo[64:, 64:] = 1
    c["blockones"] = bo.astype(BF)
    c["ones_bf"] = np.ones((128, 128)).astype(BF)
    alt = ((-1.0) ** np.arange(128))[:, None]
    c["altcol"] = alt.astype(BF)
    es = np.zeros((128, 32, 128))
    for q in range(32):
        es[q, q, :] = 1
    c["esel"] = es.reshape(128, 32 * 128).astype(BF)
    return c


def prep_weights(inp):
    w = {}
    cols, _ = col_order()
    w["w_mod"] = np.ascontiguousarray(inp["w_mod"])
    w["b_mod"] = np.stack([pvec(inp["b_mod"][l], 48) for l in range(DEPTH)])
    w["n1g"] = np.stack([pvec(inp["norm1_g"][l], 8) for l in range(DEPTH)])
    w["n2g"] = np.stack([pvec(inp["norm2_g"][l], 8) for l in range(DEPTH)])
    w["fing"] = pvec(inp["final_g"], 8)
    w["w_in"] = np.ascontiguousarray(inp["w_in"][:, :, cols])
    p = np.arange(128) % 64
    qg = inp["q_gain"]; kg = inp["k_gain"]
    w["qkg"] = np.ascontiguousarray(np.stack([qg[:, p], qg[:, p ^ 1], kg[:, p], kg[:, p ^ 1]], axis=-1))
    w["hy_sw"] = np.ascontiguousarray(inp["hy_short_w"].reshape(DEPTH, 3, 6, 128).transpose(0, 3, 2, 1))
    w["hy_sb"] = np.stack([pvec(inp["hy_short_b"][l], 6) for l in range(DEPTH)])
    w["hy_w1"] = np.ascontiguousarray(inp["hy_f_w1"])
    w["hy_w2"] = np.ascontiguousarray(inp["hy_f_w2"])
    w["hy_w3"] = np.ascontiguousarray(inp["hy_f_w3"])
    w["hy_b1"] = np.ascontiguousarray(inp["hy_f_b1"][:, :, None])
    w["hy_b2"] = np.ascontiguousarray(inp["hy_f_b2"][:, :, None])
    w["hy_fr"] = np.ascontiguousarray(inp["hy_f_freq"][:, :, None])
    w["hy_bias"] = np.ascontiguousarray(inp["hy_bias"].reshape(DEPTH, 1, 512))
    w["w_branch"] = np.ascontiguousarray(inp["w_branch"])
    w["w_out"] = np.ascontiguousarray(inp["w_out"])
    w["w_router"] = np.ascontiguousarray(inp["w_router"])
    w["w_gate"] = np.ascontiguousarray(inp["w_gate"])
    w["w_up"] = np.ascontiguousarray(inp["w_up"])
    w["w_down"] = np.ascontiguousarray(inp["w_down"])
    return w


def prep_core(inp, core):
    b0 = 2 * core
    m = {}
    m["x"] = np.ascontiguousarray(inp["x"][b0:b0 + 2])
    m["ctx"] = np.ascontiguousarray(inp["ctx"][b0:b0 + 2])
    c3 = np.stack([inp["c"][b0], inp["c"][b0 + 1], inp["c_ctx"]], axis=-1)
    m["cT"] = np.ascontiguousarray(c3.reshape(8, 128, 3).transpose(1, 0, 2))
    return m

import contextlib
import math
import numpy as np

EPS = 1e-6
PI = math.pi


class Buf:
    def __init__(self, ph, name, shape, dt, n=2):
        self.t = []
        self.r = []
        for i in range(n):
            t, r = ph.sb("%s_%d" % (name, i), shape, dt)
            self.t.append(t)
            self.r.append(r)
        self.i = 0

    def get(self):
        i = self.i % len(self.t)
        self.i += 1
        return self.t[i], self.r[i]


class Phase:
    cnt = 0

    def __init__(self, fw):
        self.fw = fw
        self.es = contextlib.ExitStack()

    def __enter__(self):
        return self

    def __exit__(self, *a):
        self.fw.barrier()
        self.es.close()
        return False

    def sb(self, name, shape, dt):
        Phase.cnt += 1
        t = self.es.enter_context(self.fw.nc.sbuf_tensor("%s_%d" % (name, Phase.cnt), list(shape), dt))
        return t, Res()

    def buf(self, name, shape, dt, n=2):
        return Buf(self, name, shape, dt, n)


class Stream:
    def __init__(self, nc, name, n, dumps=()):
        self.name = name
        self.n = n
        self.NT = 2 * n
        self.NC = n // 128
        self.W = min(512, n)
        self.cap = 2 * n // 16
        self.SC = (self.cap + 127) // 128
        self.SP = min(self.cap, 128)
        def d(nm, sh, dt):
            full = "%s_%s" % (name, nm)
            if full in dumps:
                return nc.dram_tensor(full, list(sh), dt, kind="ExternalOutput").ap()
            return nc.dram_tensor(full, list(sh), dt).ap()
        NT = self.NT
        self.xT = d("xT", [1024, NT], F32)
        self.qT = d("qT", [512, NT], BF16)
        self.kT = d("kT", [128, NT], BF16)
        self.v = d("v", [NT, 128], BF16)
        self.hyu = d("hyu", [768, NT], F32)
        self.ucs = d("ucs", [NT, 2, 256], BF16)
        self.sg = d("sg", [3072, NT], BF16)
        self.mixT = d("mixT", [1024, NT], BF16)
        self.specA = d("specA", [n, 512], F32)
        self.specB = d("specB", [n, 512], F32)
        self.specA2 = d("specA2", [128, 512], F32)
        self.h2tok = d("h2tok", [NT, 1024], BF16)
        self.selT = d("selT", [2, 16, self.SC, self.SP, n], BF16)
        self.yg = d("yg", [2, 16, self.SC, self.SP, 1024], BF16)
        if name == "L":
            self.tiles = [(i * 512, i // 4, (i % 4) * 512) for i in range(8)]
        else:
            self.tiles = [(0, None, 0)]


def build(cfg):
    nlayers = cfg.get("layers", 4)
    dumps = cfg.get("dump", [])
    nc = bass.Bass("TRN2", target_bir_lowering=False)
    fw = FW(nc, n_dma_sems=8)
    fw.psum_banks()
    pe, act, dve, pool = fw.pe, fw.act, fw.dve, fw.pool
    fw.gq = DmaQ("gq", nc.gpsimd, [fw.es.enter_context(nc.semaphore("s_gq%d" % i)) for i in range(3)])
    fw.qs.append(fw.gq)

    def ein(name, shape, dt):
        return nc.dram_tensor(name, list(shape), dt, kind="ExternalInput").ap()

    x_in = ein("x", [2, NL, D], F32)
    ctx_in = ein("ctx", [2, NCX, D], F32)
    cT_in = ein("cT", [128, 8, 3], F32)
    W = {}
    wshapes = dict(
        w_mod=([4, 1024, 6144], F32), b_mod=([4, 128, 48], F32), n1g=([4, 128, 8], F32), n2g=([4, 128, 8], F32),
        fing=([128, 8], F32), w_in=([4, 1024, 5504], F32), qkg=([4, 128, 4], F32), hy_sw=([4, 128, 6, 3], F32),
        hy_sb=([4, 128, 6], F32), hy_w1=([4, 33, 64], F32), hy_w2=([4, 64, 64], F32), hy_w3=([4, 64, 1024], F32),
        hy_b1=([4, 64, 1], F32), hy_b2=([4, 64, 1], F32), hy_fr=([4, 64, 1], F32), hy_bias=([4, 1, 512], F32),
        w_branch=([4, 1024, 1024], F32), w_out=([4, 1024, 1024], F32), w_router=([4, 1024, 16], F32),
        w_gate=([4, 16, 1024, 2048], F32), w_up=([4, 16, 1024, 2048], F32), w_down=([4, 16, 2048, 1024], F32))
    for k, (sh, dt) in wshapes.items():
        W[k] = ein(k, sh, dt)
    C = {}
    for pre, n in (("L_", NL), ("X_", NCX)):
        ncn = n // 128
        w = min(512, n)
        for nm in ("C_st", "Fs_st", "Gs_st"):
            C[pre + nm] = ein(pre + nm, [ncn, 128, ncn, 128], BF16)
        for nm in ("C_mv", "Gs_mv", "Ct_mv", "St_mv"):
            C[pre + nm] = ein(pre + nm, [n // w, 128, ncn, w], BF16)
        C[pre + "featsT"] = ein(pre + "featsT", [33, n], F32)
        C[pre + "dec2"] = ein(pre + "dec2", [128, ncn, 512], F32)
        C[pre + "rs"] = ein(pre + "rs", [128, ncn, 2], F32)
        C[pre + "DcDs"] = ein(pre + "DcDs", [128, 256], BF16)
    C["ropeC"] = ein("ropeC", [128, NL], F32)
    C["ropeS"] = ein("ropeS", [128, NL], F32)
    C["blockones"] = ein("blockones", [128, 128], BF16)
    C["ones_bf"] = ein("ones_bf", [128, 128], BF16)
    C["altcol"] = ein("altcol", [128, 1], BF16)
    C["esel"] = ein("esel", [128, 32 * 128], BF16)
    out = nc.dram_tensor("out", [2, NL, D], F32, kind="ExternalOutput").ap()

    SL = Stream(nc, "L", NL, dumps)
    SX = Stream(nc, "X", NCX, dumps)
    dump_aps = {}

    G = Phase(fw)
    ident_f, r_c = G.sb("ident_f", [128, 128], F32)
    ident_b, _ = G.sb("ident_b", [128, 128], BF16)
    ones_bf, _ = G.sb("ones_bf", [128, 128], BF16)
    blockones, _ = G.sb("blockones", [128, 128], BF16)
    altcol, _ = G.sb("altcol", [128, 1], BF16)
    esel, _ = G.sb("esel", [128, 32 * 128], BF16)
    iota_f, _ = G.sb("iota_f", [128, 256], F32)
    iota_p, _ = G.sb("iota_p", [128, 2], F32)
    mask0, _ = G.sb("mask0", [128, 1], F32)
    zrow, _ = G.sb("zrow", [32, NL], F32)
    bmodT, _ = G.sb("bmodT", [128, 4, 48], F32)
    n1g, _ = G.sb("n1g", [128, 4, 8], F32)
    n2g, _ = G.sb("n2g", [128, 4, 8], F32)
    fing, _ = G.sb("fing", [128, 8], F32)
    qkg, _ = G.sb("qkg", [128, 4, 4], F32)
    hysw, _ = G.sb("hysw", [128, 4, 6, 3], F32)
    hysb, _ = G.sb("hysb", [128, 4, 6], F32)
    scT, r_scT = G.sb("scT", [128, 8, 3], F32)
    mod, r_mod = G.sb("mod", [128, 48, 3], F32)
    A1, _ = G.sb("A1", [128, 8, 3], F32)
    A2, _ = G.sb("A2", [128, 8, 3], F32)
    one3, _ = G.sb("one3", [128, 8, 3], F32)

    def ld(t_ap, res, src):
        fw.dma([], [res], t_ap, src)

    ld(ones_bf[:], r_c, C["ones_bf"][:, :])
    ld(blockones[:], r_c, C["blockones"][:, :])
    ld(altcol[:], r_c, C["altcol"][:, :])
    ld(esel[:], r_c, C["esel"][:, :])
    ld(bmodT[:], r_c, W["b_mod"].rearrange("l p c -> p l c"))
    ld(n1g[:], r_c, W["n1g"].rearrange("l p c -> p l c"))
    ld(n2g[:], r_c, W["n2g"].rearrange("l p c -> p l c"))
    ld(fing[:], r_c, W["fing"][:, :])
    ld(qkg[:], r_c, W["qkg"].rearrange("l p c -> p l c"))
    ld(hysw[:].rearrange("p l c j -> p l (c j)"), r_c, W["hy_sw"].rearrange("l p c j -> p l (c j)"))
    ld(hysb[:], r_c, W["hy_sb"].rearrange("l p c -> p l c"))
    ld(scT[:], r_scT, cT_in[:, :, :])
    fw.op(pool, [], [r_c], "iota", iota_f[:], [[1, 256]], base=0, channel_multiplier=0, allow_small_or_imprecise_dtypes=True)
    fw.op(pool, [], [r_c], "iota", iota_p[:], [[128, 2]], base=0, channel_multiplier=1, allow_small_or_imprecise_dtypes=True)
    fw.op(pool, [], [r_c], "memset", zrow[:], 0.0)
    fw.op(pool, [], [r_c], "memset", one3[:], 1.0)
    fw.op(dve, [r_c], [r_c], "tensor_scalar", ident_f[:], iota_f[:, 0:128], iota_p[:, 0:1], None, ALU.is_equal)
    fw.op(dve, [r_c], [r_c], "tensor_copy", ident_b[:], ident_f[:])
    fw.op(dve, [r_c], [r_c], "tensor_single_scalar", mask0[:], iota_p[:, 0:1], 0.5, ALU.is_gt)
    fw.op(act, [r_scT], [r_scT], "activation", scT[:], scT[:], AF.Silu)
    fw.barrier()

    def evac(i, reads, writes, out_ap, in_ap):
        if i % 2 == 0:
            fw.op(act, reads, writes, "activation", out_ap, in_ap, AF.Copy)
        else:
            fw.op(dve, reads, writes, "tensor_copy", out_ap, in_ap)

    with Phase(fw) as ph:
        xin = ph.buf("xin", [128, 4, 1024], F32, 2)
        xo = ph.buf("xo", [128, 8, 512], F32, 2)
        for S, src in ((SL, x_in), (SX, ctx_in)):
            flat = src.rearrange("b t d -> (b t) d")
            for ti in range(S.NT // 512):
                t, r = xin.get()
                fw.dma([], [r], t[:], flat[ti * 512:(ti + 1) * 512, :].rearrange("(c p) d -> p c d", p=128))
                o, orr = xo.get()
                for dc in range(8):
                    pt, pr = fw.ps()
                    for tcq in range(4):
                        fw.tr([r, r_c], [pr], pt[:, tcq * 128:(tcq + 1) * 128], t[:, tcq, dc * 128:(dc + 1) * 128], ident_f[:])
                    evac(dc, [pr], [orr], o[:, dc, :], pt[:, 0:512])
                fw.dma([orr], [], S.xT.rearrange("(c p) t -> p c t", p=128)[:, :, ti * 512:(ti + 1) * 512], o[:])

    def norm_mod(ph, xt, xr, Asc, Bsc, outs, out_res, width, pools):
        sqp, rsp, tmpp = pools
        sq, sqr = sqp.get()
        fw.op(act, [xr], [sqr], "activation", sq[:, :, 0:width], xt[:, :, 0:width], AF.Square)
        pt, pr = fw.ps()
        fw.mm([sqr, r_c], [pr], pt[:, 0:width], [(ones_bf[:], sq[:, c, 0:width]) for c in range(8)])
        rs, rsr = rsp.get()
        fw.op(act, [pr], [rsr], "activation", rs[:, 0:width], pt[:, 0:width], AF.Sqrt, scale=1.0 / 1024, bias=EPS)
        fw.op(dve, [rsr], [rsr], "reciprocal", rs[:, 0:width], rs[:, 0:width])
        for c in range(8):
            tmp, tr_ = tmpp.get()
            fw.op(dve, [xr, rsr], [tr_], "tensor_tensor", tmp[:, 0:width], xt[:, c, 0:width], rs[:, 0:width], ALU.mult)
            if Bsc is None:
                fw.op(act, [tr_, r_c], [out_res], "activation", outs(c), tmp[:, 0:width], AF.Identity, scale=Asc(c))
            else:
                fw.op(act, [tr_, r_c, r_mod], [out_res], "activation", outs(c), tmp[:, 0:width], AF.Identity,
                      scale=Asc(c), bias=Bsc(c))

    def load_cast(stage, dst_ap, dst_res, src_ap, shape, idx):
        fw.dma([], [dst_res], dst_ap, src_ap, q=fw.gq)

    _, ctypes = col_order()

    def layer(l):
        last = (l == nlayers - 1) and (l == DEPTH - 1)
        streams = [SL] if last else [SX, SL]

        with Phase(fw) as ph:
            wst = ph.buf("wm", [128, 8, 512], F32, 2)
            mrow = ph.buf("mrow", [128, 512], F32, 2)
            scTp, r_sp = ph.sb("scTp", [128, 8, 128], F32)
            fw.op(pool, [], [r_sp], "memset", scTp[:], 0.0)
            fw.op(dve, [r_scT, r_sp], [r_sp], "tensor_copy", scTp[:, :, 0:3], scT[:])
            wm = W["w_mod"][l].rearrange("(c p) f -> p c f", p=128)
            for blk in range(12):
                t, r = wst.get()
                fw.dma([], [r], t[:], wm[:, :, blk * 512:(blk + 1) * 512])
                pt, pr = fw.ps()
                fw.mm([r, r_sp], [pr], pt[:, 0:512], [(scTp[:, dc, :], t[:, dc, :]) for dc in range(8)])
                mr_, mrr = mrow.get()
                evac(blk, [pr], [mrr], mr_[:], pt[:, 0:512])
                p2, pr2 = fw.ps()
                for j in range(4):
                    fw.tr([mrr, r_c], [pr2], p2[:, j * 128:(j + 1) * 128], mr_[:, j * 128:(j + 1) * 128], ident_f[:])
                for col in range(3):
                    fw.op(dve, [pr2, r_c], [r_mod], "tensor_tensor", mod[:, blk * 4:(blk + 1) * 4, col],
                          p2[:, 0:512].rearrange("p (a b) -> p a b", a=4)[:, :, col], bmodT[:, l, blk * 4:(blk + 1) * 4], ALU.add)
            for (Ax, ng, base) in ((A1, n1g, 8), (A2, n2g, 32)):
                fw.op(dve, [r_mod], [r_mod], "tensor_tensor", Ax[:], mod[:, base:base + 8, :], one3[:], ALU.add)
                for col in range(3):
                    fw.op(dve, [r_mod, r_c], [r_mod], "tensor_tensor", Ax[:, :, col], Ax[:, :, col], ng[:, l, :], ALU.mult)

        def mcol(tile):
            return 2 if tile[1] is None else tile[1]

        with Phase(fw) as ph:
            hT, r_hT = ph.sb("hT", [128, 8, 4608], BF16)
            ropeC, _ = ph.sb("ropeC", [128, NL], F32)
            ropeS, _ = ph.sb("ropeS", [128, NL], F32)
            dcds = {}
            for S in (SL, SX):
                dcds[S.name], _ = ph.sb("dcds" + S.name, [128, 256], BF16)
                ld(dcds[S.name][:], r_c, C[S.name + "_DcDs"][:, :])
            ld(ropeC[:], r_c, C["ropeC"][:, :])
            ld(ropeS[:], r_c, C["ropeS"][:, :])
            alltiles = [(SL, tl) for tl in SL.tiles] + [(SX, tl) for tl in SX.tiles]
            hoff = {"L": 0, "X": 4096}
            with Phase(fw) as p1:
                xin = p1.buf("xt", [128, 8, 512], F32, 2)
                pools = (p1.buf("sq", [128, 8, 512], BF16, 1), p1.buf("rs", [128, 512], F32, 2), p1.buf("tmp", [128, 512], F32, 8))
                for S, tl in alltiles:
                    t, r = xin.get()
                    fw.dma([], [r], t[:], S.xT.rearrange("(c p) t -> p c t", p=128)[:, :, tl[0]:tl[0] + 512])
                    col = mcol(tl)
                    c0 = hoff[S.name] + tl[0]
                    norm_mod(p1, t, r, lambda c: A1[:, c, col:col + 1], lambda c: mod[:, c, col:col + 1],
                             lambda c: hT[:, c, c0:c0 + 512], r_hT, 512, pools)
            with Phase(fw) as p2:
                stage = None
                wb = p2.buf("wbf", [128, 8, 512], BF16, 3)
                sqb = p2.buf("sqb", [128, 512], BF16, 2)
                rb = p2.buf("rb", [128, 512], F32, 2)
                t1b = p2.buf("t1b", [128, 512], F32, 2)
                t2b = p2.buf("t2b", [128, 512], F32, 2)
                ob = p2.buf("ob", [128, 512], BF16, 3)
                of = p2.buf("of", [128, 512], F32, 2)
                vb = p2.buf("vb", [128, 4, 128], BF16, 2)
                ub = p2.buf("ub", [128, 4, 256], BF16, 2)
                win = W["w_in"][l].rearrange("(c p) f -> p c f", p=128)
                nchunks = len(ctypes)
                for blk in range((nchunks + 3) // 4):
                    nb = min(4, nchunks - blk * 4)
                    wt, wr = wb.get()
                    load_cast(stage, wt[:, :, 0:nb * 128], wr, win[:, :, blk * 512:blk * 512 + nb * 128], [128, 8, nb * 128], blk)
                    for S, tl in alltiles:
                        c0 = hoff[S.name] + tl[0]
                        isx = S is SX
                        pos0 = tl[2]
                        held = None
                        for j in range(nb):
                            ty, idx = ctypes[blk * 4 + j]
                            if isx and ty in ("qs", "ks"):
                                continue
                            if isx and last and ty not in ("k", "v"):
                                continue
                            wj = lambda dc: wt[:, dc, j * 128:(j + 1) * 128]
                            if ty == "v":
                                vt, vr = vb.get()
                                pt, pr = fw.ps()
                                for tcq in range(4):
                                    fw.mm([wr, r_hT], [pr], pt[:, tcq * 128:(tcq + 1) * 128],
                                          [(hT[:, dc, c0 + tcq * 128:c0 + (tcq + 1) * 128], wj(dc)) for dc in range(8)])
                                evac(0, [pr], [vr], vt[:], pt[:, 0:512].rearrange("p (a b) -> p a b", a=4))
                                fw.dma([vr], [], S.v[tl[0]:tl[0] + 512, :].rearrange("(c p) d -> p c d", p=128), vt[:])
                                continue
                            pt, pr = fw.ps()
                            fw.mm([wr, r_hT], [pr], pt[:, 0:512], [(wj(dc), hT[:, dc, c0:c0 + 512]) for dc in range(8)])
                            if ty in ("q", "k"):
                                gi = 0 if ty == "q" else 2
                                dst = S.qT[idx * 128:(idx + 1) * 128, tl[0]:tl[0] + 512] if ty == "q" else S.kT[:, tl[0]:tl[0] + 512]
                                sq, sqr = sqb.get()
                                fw.op(act, [pr], [sqr], "activation", sq[:], pt[:, 0:512], AF.Square)
                                p2, pr2 = fw.ps()
                                fw.mm([sqr, r_c], [pr2], p2[:, 0:512], [(blockones[:], sq[:])])
                                rt, rr = rb.get()
                                fw.op(act, [pr2], [rr], "activation", rt[:], p2[:, 0:512], AF.Sqrt, scale=1.0 / 64, bias=EPS)
                                fw.op(dve, [rr], [rr], "reciprocal", rt[:], rt[:])
                                if isx:
                                    o, orr = ob.get()
                                    fw.op(dve, [pr, rr, r_c], [orr], "scalar_tensor_tensor", o[:], pt[:, 0:512], qkg[:, l, gi:gi + 1], rt[:], ALU.mult, ALU.mult)
                                    fw.dma([orr], [], dst, o[:])
                                else:
                                    t1, t1r = t1b.get()
                                    fw.op(dve, [pr, r_c], [t1r], "scalar_tensor_tensor", t1[:], pt[:, 0:512], qkg[:, l, gi:gi + 1],
                                          ropeC[:, pos0:pos0 + 512], ALU.mult, ALU.mult)
                                    held = (t1, t1r, rt, rr, dst)
                            elif ty in ("qs", "ks"):
                                gi = 1 if ty == "qs" else 3
                                t1, t1r, rt, rr, dst = held
                                t2, t2r = t2b.get()
                                fw.op(dve, [pr, r_c], [t2r], "scalar_tensor_tensor", t2[:], pt[:, 0:512], qkg[:, l, gi:gi + 1],
                                      ropeS[:, pos0:pos0 + 512], ALU.mult, ALU.mult)
                                fw.op(pool, [t1r, t2r], [t2r], "tensor_tensor", t2[:], t1[:], t2[:], ALU.add)
                                o, orr = ob.get()
                                fw.op(pool, [t2r, rr], [orr], "tensor_tensor", o[:], t2[:], rt[:], ALU.mult)
                                fw.dma([orr], [], dst, o[:])
                            elif ty == "hy":
                                o, orr = of.get()
                                evac(j, [pr], [orr], o[:], pt[:, 0:512])
                                fw.dma([orr], [], S.hyu[idx * 128:(idx + 1) * 128, tl[0]:tl[0] + 512], o[:])
                            elif ty == "g":
                                o, orr = ob.get()
                                fw.op(act, [pr], [orr], "activation", o[:], pt[:, 0:512], AF.Sigmoid)
                                fw.dma([orr], [], S.sg[idx * 128:(idx + 1) * 128, tl[0]:tl[0] + 512], o[:])
                            elif ty == "fn":
                                o, orr = ob.get()
                                evac(j, [pr], [orr], o[:], pt[:, 0:512])
                                ut, ur = ub.get()
                                for half in range(2):
                                    p2, pr2 = fw.ps()
                                    for q2 in range(2):
                                        tcq = half * 2 + q2
                                        fw.mm([orr, r_c], [pr2], p2[:, q2 * 256:(q2 + 1) * 256],
                                              [(o[:, tcq * 128:(tcq + 1) * 128], dcds[S.name][:])])
                                    evac(half, [pr2], [ur], ut[:, half * 2:half * 2 + 2, :], p2[:, 0:512].rearrange("p (a b) -> p a b", a=2))
                                fw.dma([ur], [], S.ucs[tl[0]:tl[0] + 512, idx, :].rearrange("(c p) d -> p c d", p=128), ut[:])

        def attention(S):
            n = S.n
            NK = n + (NCX if S is SL else 0)
            NKC = NK // 128
            QW = S.W
            with Phase(fw) as ph:
                kTs = ph.buf("kTs", [128, 2, NK], BF16, 2)
                for i_ in range(2):
                    fw.op(pool, [], [kTs.r[i_]], "memset", kTs.t[i_][:], 0.0)
                vaug = ph.buf("vaug", [128, NKC, 2, 128], BF16, 2)
                for t_ in vaug.t:
                    fw.op(pool, [], [vaug.r[vaug.t.index(t_)]], "memset", t_[:], 1.0)
                qh = ph.buf("qh", [128, n], BF16, 3)
                for i_ in range(3):
                    fw.op(pool, [], [qh.r[i_]], "memset", qh.t[i_][:], 0.0)
                eb = ph.buf("eb", [128, 512], BF16, 6)
                rcp = ph.buf("rcp", [64, 512], F32, 2)
                ao = ph.buf("ao", [64, 512], BF16, 2)
                fw.ring([0, 1, 2, 3, 6, 7])
                obank = 0
                for b in range(2):
                    kt, kr = kTs.get()
                    vt, vr = vaug.get()
                    fw.dma([], [kr], kt[0:64, :, 0:n], S.kT[:, b * n:(b + 1) * n].rearrange("(k d) t -> d k t", k=2))
                    for kk in range(2):
                        fw.dma([], [vr], vt[:, 0:n // 128, kk, 0:64],
                               S.v[b * n:(b + 1) * n, kk * 64:(kk + 1) * 64].rearrange("(c p) d -> p c d", p=128))
                    if S is SL:
                        fw.dma([], [kr], kt[0:64, :, n:NK], SX.kT[:, b * NCX:(b + 1) * NCX].rearrange("(k d) t -> d k t", k=2))
                        for kk in range(2):
                            fw.dma([], [vr], vt[:, n // 128:NKC, kk, 0:64],
                                   SX.v[b * NCX:(b + 1) * NCX, kk * 64:(kk + 1) * 64].rearrange("(c p) d -> p c d", p=128))
                    steps = []
                    for h in range(8):
                        for qi in range(n // QW):
                            for sc in range(NKC):
                                steps.append((h, qi, sc))
                    qcur = {}

                    def get_q(h):
                        if h not in qcur:
                            qt_, qr = qh.get()
                            fw.dma([], [qr], qt_[0:64, :], S.qT[h * 64:(h + 1) * 64, b * n:(b + 1) * n])
                            qcur[h] = (qt_, qr)
                        return qcur[h]

                    def issue_s(st):
                        h, qi, sc = st
                        qt_, qr = get_q(h)
                        pt, pr = fw.ps()
                        fw.mm([kr, qr], [pr], pt[:, 0:QW], [(kt[:, h // 4, sc * 128:(sc + 1) * 128], qt_[:, qi * QW:(qi + 1) * QW])])
                        return pt, pr

                    PRE = 3
                    pend = [issue_s(st) for st in steps[:PRE]]
                    po = por = None
                    for i, (h, qi, sc) in enumerate(steps):
                        kvh = h // 4
                        pt, pr = pend.pop(0)
                        if i + PRE < len(steps):
                            pend.append(issue_s(steps[i + PRE]))
                        if sc == 0:
                            po, por = fw.bank(4 + obank % 2)
                            obank += 1
                        e, er = eb.get()
                        fw.op(act, [pr], [er], "activation", e[:, 0:QW], pt[:, 0:QW], AF.Exp, scale=0.125)
                        fw.mm([vr, er], [por], po[:, 0:QW], [(vt[:, sc, kvh, :], e[:, 0:QW])], start=(sc == 0), stop=(sc == NKC - 1))
                        if sc == NKC - 1:
                            rc, rcr = rcp.get()
                            fw.op(dve, [por], [rcr], "reciprocal", rc[:, 0:QW], po[64:128, 0:QW])
                            a, ar = ao.get()
                            fw.op(dve, [por, rcr], [ar], "tensor_tensor", a[:, 0:QW], po[0:64, 0:QW], rc[:, 0:QW], ALU.mult)
                            fw.dma([ar], [], S.mixT[h * 64:(h + 1) * 64, b * n + qi * QW:b * n + (qi + 1) * QW], a[:, 0:QW])
                fw.ring(range(8))

        def hy_spectrum(S):
            n = S.n
            NCn = S.NC
            Wn = S.W
            pre = S.name + "_"
            with Phase(fw) as ph:
                feats, rf = ph.sb("feats", [33, n], F32)
                w1, rw = ph.sb("w1", [33, 64], F32)
                w2, _ = ph.sb("w2", [64, 64], F32)
                w3, _ = ph.sb("w3", [128, 1024], F32)
                b1, _ = ph.sb("b1", [64, 1], F32)
                b2, _ = ph.sb("b2", [64, 1], F32)
                fr, _ = ph.sb("fr", [64, 1], F32)
                dec2, _ = ph.sb("dec2", [128, NCn, 512], F32)
                rs_, _ = ph.sb("rs", [128, NCn, 2], F32)
                dB, _ = ph.sb("dB", [128, 512], F32)
                ld(feats[:], rw, C[pre + "featsT"][:, :])
                ld(w1[:], rw, W["hy_w1"][l])
                ld(w2[:], rw, W["hy_w2"][l])
                fw.op(pool, [], [rw], "memset", w3[:], 0.0)
                ld(w3[0:64, :], rw, W["hy_w3"][l])
                ld(b1[:], rw, W["hy_b1"][l])
                ld(b2[:], rw, W["hy_b2"][l])
                ld(fr[:], rw, W["hy_fr"][l])
                ld(dec2[:], rw, C[pre + "dec2"][:, :, :])
                ld(rs_[:], rw, C[pre + "rs"][:, :, :])
                ld(dB[:], rw, W["hy_bias"][l].partition_broadcast(128))
                fw.op(dve, [rw], [rw], "tensor_tensor", b1[:], b1[:], fr[:], ALU.mult)
                fw.op(dve, [rw], [rw], "tensor_tensor", b2[:], b2[:], fr[:], ALU.mult)
                h1, rh1 = ph.sb("h1", [64, n], F32)
                h2, rh2 = ph.sb("h2", [128, n], F32)
                fw.op(pool, [], [rh2], "memset", h2[:], 0.0)
                zb = ph.buf("zb", [64, 512], F32, 2)
                mb = ph.buf("mb", [64, 512], F32, 2)

                def sin_layer(wt, K, src, rsrc, bt, dst, rdst):
                    for ti in range(n // Wn):
                        sl = slice(ti * Wn, (ti + 1) * Wn)
                        pt, pr = fw.ps()
                        fw.mm([rw, rsrc], [pr], pt[0:64, 0:Wn], [(wt[0:K, :], src[0:K, sl])])
                        z, zr = zb.get()
                        fw.op(dve, [pr, rw], [zr], "tensor_scalar", z[:, 0:Wn], pt[0:64, 0:Wn], fr[:, 0:1], bt[:, 0:1], ALU.mult, ALU.add)
                        for rep in range(2):
                            m, mr = mb.get()
                            fw.op(dve, [zr], [mr], "tensor_scalar", m[:, 0:Wn], z[:, 0:Wn], PI, -2 * PI, ALU.is_gt, ALU.mult)
                            fw.op(dve, [zr, mr], [zr], "tensor_tensor", z[:, 0:Wn], z[:, 0:Wn], m[:, 0:Wn], ALU.add)
                            m, mr = mb.get()
                            fw.op(dve, [zr], [mr], "tensor_scalar", m[:, 0:Wn], z[:, 0:Wn], -PI, 2 * PI, ALU.is_lt, ALU.mult)
                            fw.op(dve, [zr, mr], [zr], "tensor_tensor", z[:, 0:Wn], z[:, 0:Wn], m[:, 0:Wn], ALU.add)
                        fw.op(act, [zr], [rdst], "activation", dst[0:64, sl], z[:, 0:Wn], AF.Sin)

                sin_layer(w1, 33, feats, rw, b1, h1, rh1)
                sin_layer(w2, 64, h1, rh1, b2, h2, rh2)
                gsum, rgs = ph.sb("gsum", [128, NCn, 512], BF16)
                gdif, rgd = ph.sb("gdif", [128, NCn, 512], BF16)
                hfb = ph.buf("hf", [128, 512], F32, 2)
                hbb = ph.buf("hb", [128, 512], F32, 2)
                a1b = ph.buf("a1", [128, 512], F32, 2)
                a2b = ph.buf("a2", [128, 512], BF16, 2)
                a3b = ph.buf("a3", [128, 512], F32, 2)
                fw.ring([0, 1, 2, 3, 4, 5])
                pl1, pl1r = fw.bank(7)
                for mc in range(NCn):
                    pa, par = fw.ps()
                    pb, pbr = fw.ps()
                    fw.mm([rh2, rw], [par], pa[:, 0:512], [(h2[:, mc * 128:(mc + 1) * 128], w3[:, 0:512])])
                    fw.mm([rh2, rw], [pbr], pb[:, 0:512], [(h2[:, mc * 128:(mc + 1) * 128], w3[:, 512:1024])])
                    hf, hfr = hfb.get()
                    hb, hbr = hbb.get()
                    fw.op(dve, [par, rw], [hfr], "tensor_tensor", hf[:], pa[:, 0:512], dec2[:, mc, :], ALU.mult)
                    if mc == 0:
                        fw.op(dve, [pbr, rw, r_c], [hbr], "scalar_tensor_tensor", hb[:], pb[:, 0:512], mask0[:, 0:1], dec2[:, mc, :], ALU.mult, ALU.mult)
                    else:
                        fw.op(dve, [pbr, rw], [hbr], "tensor_tensor", hb[:], pb[:, 0:512], dec2[:, mc, :], ALU.mult)
                    fw.op(pool, [hfr, hbr], [rgs], "tensor_tensor", gsum[:, mc, :], hf[:], hb[:], ALU.add)
                    fw.op(pool, [hfr, hbr], [rgd], "tensor_tensor", gdif[:, mc, :], hf[:], hb[:], ALU.subtract)
                    a1, a1r = a1b.get()
                    a2, a2r = a2b.get()
                    a3, a3r = a3b.get()
                    fw.op(act, [hfr], [a1r], "activation", a1[:], hf[:], AF.Abs)
                    fw.op(act, [hbr], [a3r], "activation", a3[:], hb[:], AF.Abs)
                    fw.op(pool, [a1r, a3r], [a2r], "tensor_tensor", a2[:], a1[:], a3[:], ALU.add)
                    fw.mm([a2r, r_c], [pl1r], pl1[:, 0:512], [(ones_bf[:], a2[:])], start=(mc == 0), stop=(mc == NCn - 1))
                il1, ril = ph.sb("il1", [128, 512], F32)
                fw.op(dve, [pl1r], [ril], "reciprocal", il1[:], pl1[:, 0:512])
                cst = ph.buf("cst", [128, NCn, 128], BF16, 2)
                fst = ph.buf("fst", [128, NCn, 128], BF16, 2)
                ab = ph.buf("ab", [128, 512], F32, 2)
                bb = ph.buf("bb", [128, 512], F32, 2)
                for fc in range(NCn):
                    ct, cr = cst.get()
                    ft, fr_ = fst.get()
                    fw.dma([], [cr], ct[:], C[pre + "C_st"][fc])
                    fw.dma([], [fr_], ft[:], C[pre + "Fs_st"][fc])
                    pa, par = fw.ps()
                    pb, pbr = fw.ps()
                    fw.mm([cr, rgs], [par], pa[:, 0:512], [(ct[:, mc, :], gsum[:, mc, :]) for mc in range(NCn)])
                    fw.mm([fr_, rgd], [pbr], pb[:, 0:512], [(ft[:, mc, :], gdif[:, mc, :]) for mc in range(NCn)])
                    a, ar = ab.get()
                    fw.op(dve, [par, ril], [ar], "tensor_tensor", a[:], pa[:, 0:512], il1[:], ALU.mult)
                    fw.op(pool, [ar, rw], [ar], "tensor_tensor", a[:], a[:], dB[:], ALU.add)
                    fw.op(act, [ar, rw], [ar], "activation", a[:], a[:], AF.Identity, scale=rs_[:, fc, 0:1])
                    bt_, btr = bb.get()
                    fw.op(dve, [pbr, ril, rw], [btr], "scalar_tensor_tensor", bt_[:], pb[:, 0:512], rs_[:, fc, 1:2], il1[:], ALU.mult, ALU.mult)
                    fw.dma([ar], [], S.specA[fc * 128:(fc + 1) * 128, :], a[:])
                    fw.dma([btr], [], S.specB[fc * 128:(fc + 1) * 128, :], bt_[:])
                    if fc == 0:
                        pn, pnr = fw.ps()
                        fw.mm([rgs, r_c], [pnr], pn[0:1, 0:512], [(altcol[:, 0:1], gsum[:, mc, :]) for mc in range(NCn)])
                        a2_, a2r_ = ph.sb("a2c0", [128, 512], F32)
                        nq, nqr = ph.sb("nq", [1, 512], F32)
                        fw.op(dve, [pnr, ril], [nqr], "tensor_tensor", nq[:], pn[0:1, 0:512], il1[0:1, :], ALU.mult)
                        fw.op(dve, [nqr, rw], [nqr], "tensor_tensor", nq[:], nq[:], dB[0:1, :], ALU.add)
                        fw.op(dve, [ar], [a2r_], "tensor_copy", a2_[:], a[:])
                        fw.op(dve, [nqr, a2r_], [a2r_], "tensor_single_scalar", a2_[0:1, :], nq[:], 0.5 / n, ALU.mult)
                        fw.dma([a2r_], [], S.specA2[:, :], a2_[:])
                fw.ring(range(8))

        def hyena(S):
            n = S.n
            NCn = S.NC
            Wn = S.W
            pre = S.name + "_"
            with Phase(fw) as ph:
                vtok, rv = ph.sb("vtok", [128, NCn, 512], BF16)
                g1tok, rg1 = ph.sb("g1tok", [128, NCn, 512], BF16)
                g2T, rg2 = ph.sb("g2T", [128, 4, n], F32)
                ztok, rz = ph.sb("ztok", [128, NCn, 512], BF16)
                Y, rY = ph.sb("Y", [128, NCn, 2, 512], BF16)
                with Phase(fw) as p1:
                    ub = p1.buf("u", [128, n], F32, 2)
                    cb = p1.buf("cv", [128, n], F32, 2)
                    for b in range(2):
                        for hc in range(6):
                            u, ur = ub.get()
                            fw.dma([], [ur], u[:], S.hyu[hc * 128:(hc + 1) * 128, b * n:(b + 1) * n])
                            if hc >= 4:
                                orr = rg2
                                ct_ = None
                            else:
                                ct_, orr = cb.get()

                            def osl(a, b_):
                                return g2T[:, b * 2 + (hc - 4), a:b_] if hc >= 4 else ct_[:, a:b_]
                            fw.op(dve, [ur, r_c], [orr], "tensor_scalar", osl(0, n), u[:], hysw[:, l, hc, 1:2], hysb[:, l, hc:hc + 1], ALU.mult, ALU.add)
                            fw.op(dve, [ur, r_c, orr], [orr], "scalar_tensor_tensor", osl(1, n), u[:, 0:n - 1], hysw[:, l, hc, 0:1], osl(1, n), ALU.mult, ALU.add)
                            fw.op(dve, [ur, r_c, orr], [orr], "scalar_tensor_tensor", osl(0, n - 1), u[:, 1:n], hysw[:, l, hc, 2:3], osl(0, n - 1), ALU.mult, ALU.add)
                            if hc < 4:
                                dst, rdst = (vtok, rv) if hc < 2 else (g1tok, rg1)
                                cbase = b * 256 + (hc % 2) * 128
                                nq = min(4, NCn)
                                for t4 in range(NCn // nq):
                                    pt, pr = fw.ps()
                                    for q in range(nq):
                                        tc = t4 * nq + q
                                        fw.tr([orr, r_c], [pr], pt[:, q * 128:(q + 1) * 128], ct_[:, tc * 128:(tc + 1) * 128], ident_f[:])
                                    evac(t4, [pr], [rdst], dst[:, t4 * nq:t4 * nq + nq, cbase:cbase + 128],
                                         pt[:, 0:nq * 128].rearrange("p (a b) -> p a b", a=nq))
                with Phase(fw) as p2:
                    stA = p2.buf("stA", [128, NCn, 128], BF16, 2)
                    stB = p2.buf("stB", [128, NCn, 128], BF16, 2)
                    spA = p2.buf("spA", [128, 2, 256], F32, 2)
                    spB = p2.buf("spB", [128, 2, 256], F32, 2)
                    spA2 = p2.buf("spA2", [128, 2, 256], F32, 1)
                    t1b = p2.buf("t1", [128, 512], F32, 2)
                    t2b = p2.buf("t2", [128, 512], F32, 2)
                    t3b = p2.buf("t3", [128, 512], F32, 2)
                    t4b = p2.buf("t4", [128, 512], F32, 2)

                    def fwd(src, rsrc, o):
                        for fc in range(NCn):
                            ct, cr = stA.get()
                            ft, fr_ = stB.get()
                            fw.dma([], [cr], ct[:], C[pre + "C_st"][fc])
                            fw.dma([], [fr_], ft[:], C[pre + "Fs_st"][fc])
                            at, ar = spA.get()
                            bt_, br = spB.get()
                            for b in range(2):
                                fw.dma([], [ar], at[:, b, :], S.specA[fc * 128:(fc + 1) * 128, o * 256:(o + 1) * 256])
                                fw.dma([], [br], bt_[:, b, :], S.specB[fc * 128:(fc + 1) * 128, o * 256:(o + 1) * 256])
                            if fc == 0:
                                a2t, a2r = spA2.get()
                                for b in range(2):
                                    fw.dma([], [a2r], a2t[:, b, :], S.specA2[:, o * 256:(o + 1) * 256])
                            else:
                                a2t, a2r = at, ar
                            pzr, pzrr = fw.ps()
                            pzi, pzir = fw.ps()
                            fw.mm([cr, rsrc], [pzrr], pzr[:, 0:512], [(ct[:, tc, :], src[:, tc, :]) for tc in range(NCn)])
                            fw.mm([fr_, rsrc], [pzir], pzi[:, 0:512], [(ft[:, tc, :], src[:, tc, :]) for tc in range(NCn)])
                            af = at[:].rearrange("p a b -> p (a b)")
                            bf = bt_[:].rearrange("p a b -> p (a b)")
                            a2f = a2t[:].rearrange("p a b -> p (a b)")
                            t1, r1 = t1b.get()
                            t2, r2 = t2b.get()
                            t3, r3 = t3b.get()
                            t4, r4 = t4b.get()
                            fw.op(dve, [pzrr, ar], [r1], "tensor_tensor", t1[:], pzr[:, 0:512], af, ALU.mult)
                            fw.op(dve, [pzir, br], [r2], "tensor_tensor", t2[:], pzi[:, 0:512], bf, ALU.mult)
                            fw.op(dve, [pzrr, br], [r3], "tensor_tensor", t3[:], pzr[:, 0:512], bf, ALU.mult)
                            fw.op(dve, [pzir, a2r], [r4], "tensor_tensor", t4[:], pzi[:, 0:512], a2f, ALU.mult)
                            fw.op(pool, [r1, r2], [rY], "tensor_tensor", Y[:, fc, 0, :], t1[:], t2[:], ALU.subtract)
                            fw.op(pool, [r3, r4], [rY], "tensor_tensor", Y[:, fc, 1, :], t3[:], t4[:], ALU.add)

                    fwd(vtok, rv, 0)
                    for tc in range(NCn):
                        ct, cr = stA.get()
                        gt, gr = stB.get()
                        fw.dma([], [cr], ct[:], C[pre + "C_st"][tc])
                        fw.dma([], [gr], gt[:], C[pre + "Gs_st"][tc])
                        pz, pzr_ = fw.ps()
                        fw.mm([cr, gr, rY], [pzr_], pz[:, 0:512],
                              [(ct[:, fc, :], Y[:, fc, 0, :]) for fc in range(NCn)] + [(gt[:, fc, :], Y[:, fc, 1, :]) for fc in range(NCn)])
                        fw.op(dve, [pzr_, rg1], [rz], "tensor_tensor", ztok[:, tc, :], pz[:, 0:512], g1tok[:, tc, :], ALU.mult)
                    fwd(ztok, rz, 1)
                with Phase(fw) as p3:
                    mvA = p3.buf("mvA", [128, NCn, Wn], BF16, 1)
                    mvB = p3.buf("mvB", [128, NCn, Wn], BF16, 1)
                    yo = p3.buf("yo", [128, Wn], BF16, 2)
                    for tt in range(n // Wn):
                        ct, cr = mvA.get()
                        gt, gr = mvB.get()
                        fw.dma([], [cr], ct[:], C[pre + "C_mv"][tt])
                        fw.dma([], [gr], gt[:], C[pre + "Gs_mv"][tt])
                        for bcc in range(4):
                            py, pyr = fw.ps()
                            fw.mm([cr, gr, rY], [pyr], py[:, 0:Wn],
                                  [(Y[:, fc, 0, bcc * 128:(bcc + 1) * 128], ct[:, fc, :]) for fc in range(NCn)] +
                                  [(Y[:, fc, 1, bcc * 128:(bcc + 1) * 128], gt[:, fc, :]) for fc in range(NCn)])
                            o, orr = yo.get()
                            fw.op(dve, [pyr, rg2], [orr], "tensor_tensor", o[:], py[:, 0:Wn], g2T[:, bcc, tt * Wn:(tt + 1) * Wn], ALU.mult)
                            b, cc = bcc // 2, bcc % 2
                            fw.dma([orr], [], S.mixT[512 + cc * 128:512 + (cc + 1) * 128, b * n + tt * Wn:b * n + (tt + 1) * Wn], o[:])

        def fnet(S):
            n = S.n
            NCn = S.NC
            Wn = S.W
            pre = S.name + "_"
            with Phase(fw) as ph:
                us, rus = ph.sb("ucs", [128, 2, NCn, 2, 256], BF16)
                for b in range(2):
                    fw.dma([], [rus], us[:, b, :, :, :].rearrange("p c a d -> p c (a d)"), S.ucs[b * n:(b + 1) * n, :, :].rearrange("(c p) a d -> p c (a d)", p=128))
                mvA = ph.buf("fA", [128, NCn, Wn], BF16, 2)
                mvB = ph.buf("fB", [128, NCn, Wn], BF16, 2)
                yo = ph.buf("fyo", [128, Wn], BF16, 2)
                k = 0
                for kt in range(n // Wn):
                    ct, cr = mvA.get()
                    st_, sr = mvB.get()
                    fw.dma([], [cr], ct[:], C[pre + "Ct_mv"][kt])
                    fw.dma([], [sr], st_[:], C[pre + "St_mv"][kt])
                    for b in range(2):
                        for a in range(2):
                            py, pyr = fw.ps()
                            fw.mm([cr, sr, rus], [pyr], py[:, 0:Wn],
                                  [(us[:, b, tc, a, 0:128], ct[:, tc, :]) for tc in range(NCn)] +
                                  [(us[:, b, tc, a, 128:256], st_[:, tc, :]) for tc in range(NCn)])
                            o, orr = yo.get()
                            evac(k, [pyr], [orr], o[:], py[:, 0:Wn])
                            k += 1
                            fw.dma([orr], [], S.mixT[768 + a * 128:768 + (a + 1) * 128, b * n + kt * Wn:b * n + (kt + 1) * Wn], o[:])

        def merge():
            with Phase(fw) as ph:
                stage = None
                wbr, rwb = ph.sb("wbr", [128, 8, 1024], BF16)
                wo, rwo = ph.sb("wo", [128, 8, 1024], BF16)
                for i, (dst, rd, src) in enumerate(((wbr, rwb, W["w_branch"][l]), (wo, rwo, W["w_out"][l]))):
                    sv = src.rearrange("(c p) f -> p c f", p=128)
                    for hlf in range(2):
                        load_cast(stage, dst[:, :, hlf * 512:(hlf + 1) * 512], rd, sv[:, :, hlf * 512:(hlf + 1) * 512], [128, 8, 512], i * 2 + hlf)
                mixb = ph.buf("mixb", [128, 8, 512], BF16, 2)
                sgb = ph.buf("sgb", [128, 24, 512], BF16, 1)
                xb = ph.buf("xb", [128, 8, 512], F32, 2)
                mT = ph.buf("mT", [128, 8, 512], BF16, 1)
                tb = [ph.buf("mt%d" % i, [128, 512], F32, 2) for i in range(3)]
                for S in streams:
                    for tl in S.tiles:
                        sl = slice(tl[0], tl[0] + 512)
                        col = mcol(tl)
                        mx, mxr = mixb.get()
                        sg, sgr = sgb.get()
                        xt, xr = xb.get()
                        m, mr = mT.get()
                        fw.dma([], [mxr], mx[:], S.mixT.rearrange("(c p) t -> p c t", p=128)[:, :, sl])
                        fw.dma([], [sgr], sg[:], S.sg.rearrange("(c p) t -> p c t", p=128)[:, :, sl])
                        fw.dma([], [xr], xt[:], S.xT.rearrange("(c p) t -> p c t", p=128)[:, :, sl])
                        for dc in range(8):
                            ts = []
                            for bi, (k0, k1) in enumerate(((0, 4), (4, 6), (6, 8))):
                                pt, pr = fw.ps()
                                fw.mm([rwb, mxr], [pr], pt[:, 0:512], [(wbr[:, kc, dc * 128:(dc + 1) * 128], mx[:, kc, :]) for kc in range(k0, k1)])
                                t, tr_ = tb[bi].get()
                                fw.op(dve, [pr, sgr], [tr_], "tensor_tensor", t[:], pt[:, 0:512], sg[:, bi * 8 + dc, :], ALU.mult)
                                ts.append((t, tr_))
                            fw.op(pool, [ts[0][1], ts[1][1]], [ts[1][1]], "tensor_tensor", ts[1][0][:], ts[0][0][:], ts[1][0][:], ALU.add)
                            fw.op(pool, [ts[1][1], ts[2][1]], [mr], "tensor_tensor", m[:, dc, :], ts[1][0][:], ts[2][0][:], ALU.add)
                        for dc in range(8):
                            pt, pr = fw.ps()
                            fw.mm([rwo, mr], [pr], pt[:, 0:512], [(wo[:, kc, dc * 128:(dc + 1) * 128], m[:, kc, :]) for kc in range(8)])
                            fw.op(dve, [pr, xr, r_mod], [xr], "scalar_tensor_tensor", xt[:, dc, :], pt[:, 0:512], mod[:, 16 + dc, col:col + 1], xt[:, dc, :], ALU.mult, ALU.add)
                        fw.dma([xr], [], S.xT.rearrange("(c p) t -> p c t", p=128)[:, :, sl], xt[:])

        def moe():
            with Phase(fw) as PH:
                aff = {}
                for S in streams:
                    aff[S.name] = PH.sb("aff" + S.name, [128, S.NC, 2, 16], F32)
                wr_, rwr = PH.sb("wr", [128, 8, 16], F32)
                ld(wr_[:], rwr, W["w_router"][l].rearrange("(c p) e -> p c e", p=128))
                slotT = {}
                slot_tok = {}
                affb = {}
                affTb = {}
                for S in streams:
                    slotT[S.name] = PH.sb("slotT" + S.name, [128, S.n], BF16)
                    affTb[S.name] = PH.sb("affTb" + S.name, [128, S.n], BF16)
                    fw.op(pool, [], [slotT[S.name][1]], "memset", slotT[S.name][0][:], 0.0)
                    fw.op(pool, [], [affTb[S.name][1]], "memset", affTb[S.name][0][:], 0.0)
                    slot_tok[S.name] = PH.sb("slottok" + S.name, [128, S.NC, 32], F32)
                    affb[S.name] = PH.sb("affb" + S.name, [128, S.NC, 2, 16], BF16)
                with Phase(fw) as ph:
                    xin = ph.buf("xt2", [128, 8, 512], F32, 2)
                    h2b = ph.buf("h2T", [128, 8, 512], F32, 2)
                    pools = (ph.buf("sq2", [128, 8, 512], BF16, 1), ph.buf("rs2", [128, 512], F32, 2), ph.buf("tmp2", [128, 512], F32, 8))
                    hto = ph.buf("hto", [128, 4, 1024], BF16, 2)
                    sm = ph.buf("sm", [128, 4], F32, 4)
                    eb = ph.buf("ex", [128, 16], F32, 4)
                    for S in streams:
                        at, ar = aff[S.name]
                        for tl in S.tiles:
                            t, r = xin.get()
                            fw.dma([], [r], t[:], S.xT.rearrange("(c p) t -> p c t", p=128)[:, :, tl[0]:tl[0] + 512])
                            col = mcol(tl)
                            h2, h2r = h2b.get()
                            norm_mod(ph, t, r, lambda c: A2[:, c, col:col + 1], lambda c: mod[:, 24 + c, col:col + 1],
                                     lambda c: h2[:, c, :], h2r, 512, pools)
                            ho, hor = hto.get()
                            st4 = []
                            for tcq in range(4):
                                tok0 = tl[0] + tcq * 128
                                b = tok0 // S.n
                                tc = (tok0 % S.n) // 128
                                tsl = slice(tcq * 128, (tcq + 1) * 128)
                                pt, pr = fw.ps()
                                fw.mm([h2r, rwr], [pr], pt[:, 0:16], [(h2[:, dc, tsl], wr_[:, dc, :]) for dc in range(8)])
                                s, sr = sm.get()
                                e, er = eb.get()
                                st4.append((b, tc, tsl, pt, pr, s, sr, e, er))
                            for (b, tc, tsl, pt, pr, s, sr, e, er) in st4:
                                fw.op(dve, [pr], [sr], "tensor_reduce", s[:, 0:1], pt[:, 0:16], AX.X, ALU.max, negate=True)
                            for (b, tc, tsl, pt, pr, s, sr, e, er) in st4:
                                fw.op(act, [pr, sr], [er, sr], "activation", e[:], pt[:, 0:16], AF.Exp, bias=s[:, 0:1], accum_out=s[:, 1:2])
                            for (b, tc, tsl, pt, pr, s, sr, e, er) in st4:
                                fw.op(dve, [sr], [sr], "reciprocal", s[:, 2:3], s[:, 1:2])
                            for (b, tc, tsl, pt, pr, s, sr, e, er) in st4:
                                fw.op(dve, [er, sr], [ar], "tensor_scalar", at[:, tc, b, :], e[:], s[:, 2:3], None, ALU.mult)
                            for tcq in range(4):
                                tsl = slice(tcq * 128, (tcq + 1) * 128)
                                for hh in range(2):
                                    p2, pr2 = fw.ps()
                                    for q in range(4):
                                        dc = hh * 4 + q
                                        fw.tr([h2r, r_c], [pr2], p2[:, q * 128:(q + 1) * 128], h2[:, dc, tsl], ident_f[:])
                                    evac(hh, [pr2], [hor], ho[:, tcq, hh * 512:(hh + 1) * 512], p2[:, 0:512])
                            fw.dma([hor], [], S.h2tok[tl[0]:tl[0] + 512, :].rearrange("(c p) d -> p c d", p=128), ho[:])
                with Phase(fw) as ph:
                    for S in streams:
                        n = S.n
                        at, ar = aff[S.name]
                        affT, rat = ph.sb("affT", [32, n], F32)
                        wk = ph.buf("wk", [32, n], F32, 2)
                        m8, rm8 = ph.sb("m8", [32, 8], F32)
                        msk, rmsk = ph.sb("msk", [32, n], F32)
                        rnk, rrnk = ph.sb("rnk", [32, n], F32)
                        slf, rslf = ph.sb("slf", [32, n], F32)
                        fw.op(dve, [ar], [affb[S.name][1]], "tensor_copy", affb[S.name][0][:], at[:])
                        apad, rap = ph.sb("apad", [128, 4, 128], F32)
                        fw.op(pool, [], [rap], "memset", apad[:], 0.0)
                        for t4 in range(max(1, S.NC // 4)):
                            nq = min(4, S.NC)
                            fw.op(dve, [ar, rap], [rap], "tensor_copy", apad[:, 0:nq, 0:32],
                                  at[:, t4 * 4:t4 * 4 + nq, :, :].rearrange("p c a b -> p c (a b)"))
                            pt, pr = fw.ps()
                            for q in range(nq):
                                fw.tr([rap, r_c], [pr], pt[:, q * 128:(q + 1) * 128], apad[:, q, :], ident_f[:])
                            evac(t4, [pr], [rat], affT[:, t4 * 512:t4 * 512 + nq * 128], pt[0:32, 0:nq * 128])
                        cur, curr = affT, rat
                        nit = S.cap // 8
                        for it in range(nit):
                            fw.op(dve, [curr], [rm8], "max", m8[:], cur[:])
                            if it < nit - 1:
                                nx, nxr = wk.get()
                                fw.op(dve, [curr, rm8], [nxr], "match_replace", nx[:], m8[:], cur[:], -1e30)
                                cur, curr = nx, nxr
                        fw.op(dve, [rat, rm8], [rmsk], "tensor_scalar", msk[:], affT[:], m8[:, 7:8], None, ALU.is_ge)
                        fw.op(dve, [rmsk, r_c], [rrnk], "tensor_tensor_scan", rnk[:], msk[:], zrow[:, 0:n], 0.0, ALU.add, ALU.add)
                        fw.op(dve, [rrnk], [rslf], "tensor_single_scalar", slf[:], rnk[:], float(S.cap) + 0.5, ALU.is_lt)
                        fw.op(dve, [rslf, rmsk], [rmsk], "tensor_tensor", msk[:], msk[:], slf[:], ALU.mult)
                        fw.op(dve, [rrnk, rmsk], [rslf], "tensor_tensor", slf[:], rnk[:], msk[:], ALU.mult)
                        fw.op(dve, [rslf], [rslf], "tensor_single_scalar", slf[:], slf[:], -1.0, ALU.add)
                        st_, rst = slotT[S.name]
                        fw.op(dve, [rslf], [rst], "tensor_copy", st_[0:32, :], slf[:])
                        fw.op(dve, [rat], [affTb[S.name][1]], "tensor_copy", affTb[S.name][0][0:32, :], affT[:])
                        sk, rsk = slot_tok[S.name]
                        for t4 in range(max(1, S.NC // 4)):
                            nq = min(4, S.NC)
                            pt, pr = fw.ps()
                            for q in range(nq):
                                tc = t4 * 4 + q
                                fw.tr([rslf, r_c], [pr], pt[:, q * 32:(q + 1) * 32], slf[:, tc * 128:(tc + 1) * 128], ident_f[0:32, 0:32])
                            evac(t4, [pr], [rsk], sk[:, t4 * 4:t4 * 4 + nq, :], pt[:, 0:nq * 32].rearrange("p (a b) -> p a b", a=nq))
                with Phase(fw) as ph:
                    stage = None
                    wgb = ph.buf("wg", [128, 8, 512], BF16, 3)
                    wub = ph.buf("wu", [128, 8, 512], BF16, 3)
                    wdb = ph.buf("wd", [128, 16, 512], BF16, 2)
                    sel = {}
                    selT = {}
                    xg = {}
                    actT = {}
                    wsl = {}
                    for S in streams:
                        sel[S.name] = ph.buf("sel" + S.name, [128, S.NC, S.cap], BF16, 1)
                        selT[S.name] = ph.buf("selT" + S.name, [S.SP, S.SC, S.n], BF16, 1)
                        xg[S.name] = ph.buf("xg" + S.name, [128, 8, 2 * S.cap], BF16, 1)
                        actT[S.name] = ph.buf("actT" + S.name, [128, 16, 2 * S.cap], BF16, 1)
                    h2t = ph.buf("h2t", [128, 16, 1024], BF16, 1)
                    abuf = ph.buf("abuf", [128, 512], F32, 2)
                    s0buf = ph.buf("s0buf", [128, 512], BF16, 3)
                    sa = ph.buf("sa", [128, 512], F32, 2)
                    ygb = ph.buf("ygb", [128, 512], BF16, 3)
                    cast_i = 0
                    ev_i = 0
                    for e in range(16):
                        XG = {}
                        AT = {}
                        WS = {}
                        for S in streams:
                            n, cap, SC, SP, NCn = S.n, S.cap, S.SC, S.SP, S.NC
                            xgt, xgr = xg[S.name].get()
                            XG[S.name] = (xgt, xgr)
                            sk, rsk = slot_tok[S.name]
                            st_, rst = slotT[S.name]
                            ab_, rab = affb[S.name]
                            for b in range(2):
                                q = b * 16 + e
                                se, ser = sel[S.name].get()
                                for tc in range(NCn):
                                    fw.op(dve, [rsk, r_c], [ser], "tensor_scalar", se[:, tc, :], iota_f[:, 0:cap], sk[:, tc, q:q + 1], None, ALU.is_equal)
                                sT, sTr = selT[S.name].get()
                                aTb, raT = affTb[S.name]
                                for tt in range(n // S.W):
                                    cs = slice(tt * S.W, (tt + 1) * S.W)
                                    pt, pr = fw.ps()
                                    fw.mm([rst, r_c], [pr], pt[:, 0:S.W], [(esel[:, q * 128:(q + 1) * 128], st_[:, cs])])
                                    pa, par = fw.ps()
                                    fw.mm([raT, r_c], [par], pa[:, 0:S.W], [(esel[:, q * 128:(q + 1) * 128], aTb[:, cs])])
                                    ab, abr = abuf.get()
                                    fw.op(act, [par], [abr], "activation", ab[0:SP, 0:S.W], pa[0:SP, 0:S.W], AF.Copy)
                                    for sc in range(SC):
                                        s0, s0r = s0buf.get()
                                        fw.op(dve, [pr, r_c], [s0r], "tensor_scalar", s0[0:SP, 0:S.W], pt[0:SP, 0:S.W],
                                              iota_p[0:SP, sc:sc + 1], None, ALU.is_equal)
                                        fw.op(pool, [s0r, abr], [sTr], "tensor_tensor", sT[:, sc, cs], s0[0:SP, 0:S.W], ab[0:SP, 0:S.W], ALU.mult)
                                fw.dma([sTr], [], S.selT[b, e].rearrange("s p t -> p s t"), sT[:])
                                pg = [fw.ps() for _ in range(4)] if cap == 256 else [fw.ps()]
                                ngrp = max(1, NCn // 4)
                                nq = min(4, NCn)
                                ht, hr = h2t.get()
                                for g in range(ngrp):
                                    fw.dma([], [hr], ht[:, g * nq:(g + 1) * nq, :], S.h2tok[b * n + g * 512:b * n + g * 512 + nq * 128, :].rearrange("(c p) d -> p c d", p=128))
                                for dc in range(8):
                                    if cap == 256:
                                        pt, pr = pg[dc // 2]
                                        oap = pt[:, (dc % 2) * 256:(dc % 2 + 1) * 256]
                                    else:
                                        pt, pr = pg[0]
                                        oap = pt[:, dc * cap:(dc + 1) * cap]
                                    fw.mm([hr, ser], [pr], oap, [(ht[:, tc, dc * 128:(dc + 1) * 128], se[:, tc, :]) for tc in range(NCn)])
                                for dc in range(8):
                                    if cap == 256:
                                        pt, pr = pg[dc // 2]
                                        oap = pt[:, (dc % 2) * 256:(dc % 2 + 1) * 256]
                                    else:
                                        pt, pr = pg[0]
                                        oap = pt[:, dc * cap:(dc + 1) * cap]
                                    evac(dc, [pr], [xgr], xgt[:, dc, b * cap:(b + 1) * cap], oap)
                        for S in streams:
                            AT[S.name] = actT[S.name].get()
                        wgs = W["w_gate"][l, e].rearrange("(c p) f -> p c f", p=128)
                        wus = W["w_up"][l, e].rearrange("(c p) f -> p c f", p=128)
                        for ft in range(4):
                            wg, wgr = wgb.get()
                            wu, wur = wub.get()
                            load_cast(stage, wg[:], wgr, wgs[:, :, ft * 512:(ft + 1) * 512], [128, 8, 512], cast_i); cast_i += 1
                            load_cast(stage, wu[:], wur, wus[:, :, ft * 512:(ft + 1) * 512], [128, 8, 512], cast_i); cast_i += 1
                            for S in streams:
                                N2 = 2 * S.cap
                                xgt, xgr = XG[S.name]
                                att, atr = AT[S.name]
                                for fq in range(4):
                                    fcx = ft * 4 + fq
                                    pa, par = fw.ps()
                                    pu, pur = fw.ps()
                                    fw.mm([wgr, xgr], [par], pa[:, 0:N2], [(wg[:, dc, fq * 128:(fq + 1) * 128], xgt[:, dc, :]) for dc in range(8)])
                                    fw.mm([wur, xgr], [pur], pu[:, 0:N2], [(wu[:, dc, fq * 128:(fq + 1) * 128], xgt[:, dc, :]) for dc in range(8)])
                                    s, sr = sa.get()
                                    fw.op(act, [par], [sr], "activation", s[:, 0:N2], pa[:, 0:N2], AF.Silu)
                                    fw.op(dve, [pur, sr], [atr], "tensor_tensor", att[:, fcx, :], pu[:, 0:N2], s[:, 0:N2], ALU.mult)
                        wds = W["w_down"][l, e].rearrange("(c p) d -> p c d", p=128)
                        for dh in range(2):
                            wd, wdr = wdb.get()
                            for hf in range(2):
                                load_cast(stage, wd[:, hf * 8:(hf + 1) * 8, :], wdr, wds[:, hf * 8:(hf + 1) * 8, dh * 512:(dh + 1) * 512], [128, 8, 512], cast_i); cast_i += 1
                            for S in streams:
                                att, atr = AT[S.name]
                                cap, SC, SP = S.cap, S.SC, S.SP
                                if S is SX:
                                    pt, pr = fw.ps()
                                    fw.mm([wdr, atr], [pr], pt[0:64, 0:512], [(att[:, fcx, 0:64], wd[:, fcx, :]) for fcx in range(16)])
                                    y, yr = ygb.get()
                                    evac(ev_i, [pr], [yr], y[0:64, :], pt[0:64, 0:512]); ev_i += 1
                                    for b in range(2):
                                        fw.dma([yr], [], S.yg[b, e, 0, :, dh * 512:(dh + 1) * 512], y[b * 32:(b + 1) * 32, :])
                                    continue
                                for b in range(2):
                                    for sc in range(SC):
                                        c0 = b * cap + sc * SP
                                        pt, pr = fw.ps()
                                        fw.mm([wdr, atr], [pr], pt[0:SP, 0:512], [(att[:, fcx, c0:c0 + SP], wd[:, fcx, :]) for fcx in range(16)])
                                        y, yr = ygb.get()
                                        evac(ev_i, [pr], [yr], y[0:SP, :], pt[0:SP, 0:512]); ev_i += 1
                                        fw.dma([yr], [], S.yg[b, e, sc, :, dh * 512:(dh + 1) * 512], y[0:SP, :])
                for S in streams:
                    n, SC, SP, Wn = S.n, S.SC, S.SP, S.W
                    with Phase(fw) as ph:
                        yga = ph.buf("yga", [SP, 16, SC, 1024], BF16, 1)
                        stl = ph.buf("stl", [SP, 16, SC, Wn], BF16, 1 if S is SL else 2)
                        xb = ph.buf("xc", [128, 8, Wn], F32, 2)
                        for b in range(2):
                            yt, yr = yga.get()
                            for e4 in range(4):
                                fw.dma([], [yr], yt[:, e4 * 4:(e4 + 1) * 4, :, :].rearrange("p e s d -> p (e s) d"), S.yg[b, e4 * 4:(e4 + 1) * 4].rearrange("e s p d -> p (e s) d"))
                            col = 2 if S is SX else b
                            for tt in range(n // Wn):
                                sl = slice(b * n + tt * Wn, b * n + (tt + 1) * Wn)
                                st_, sr = stl.get()
                                for e4 in range(4):
                                    fw.dma([], [sr], st_[:, e4 * 4:(e4 + 1) * 4, :, :].rearrange("p e s t -> p (e s) t"),
                                           S.selT[b, e4 * 4:(e4 + 1) * 4, :, :, tt * Wn:(tt + 1) * Wn].rearrange("e s p t -> p (e s) t"))
                                xt, xr = xb.get()
                                fw.dma([], [xr], xt[:], S.xT.rearrange("(c p) t -> p c t", p=128)[:, :, sl])
                                for dc in range(8):
                                    pt, pr = fw.ps()
                                    fw.mm([yr, sr], [pr], pt[:, 0:Wn],
                                          [(yt[:, e, sc, dc * 128:(dc + 1) * 128], st_[:, e, sc, :]) for e in range(16) for sc in range(SC)])
                                    fw.op(dve, [pr, xr, r_mod], [xr], "scalar_tensor_tensor", xt[:, dc, :], pt[:, 0:Wn], mod[:, 40 + dc, col:col + 1], xt[:, dc, :], ALU.mult, ALU.add)
                                fw.dma([xr], [], S.xT.rearrange("(c p) t -> p c t", p=128)[:, :, sl], xt[:])

        if not last:
            attention(SX)
        attention(SL)
        for S in streams:
            hy_spectrum(S)
            hyena(S)
            fnet(S)
        merge()
        moe()

    for l in range(nlayers):
        layer(l)

    with Phase(fw) as ph:
        xin = ph.buf("xf", [128, 8, 512], F32, 2)
        xnb = ph.buf("xn", [128, 8, 512], F32, 2)
        pools = (ph.buf("sqf", [128, 8, 512], BF16, 1), ph.buf("rsf", [128, 512], F32, 2), ph.buf("tmpf", [128, 512], F32, 8))
        ot = ph.buf("ot", [128, 4, 1024], F32, 2)
        oflat = out.rearrange("b t d -> (b t) d")
        for tl in SL.tiles:
            t, r = xin.get()
            fw.dma([], [r], t[:], SL.xT.rearrange("(c p) t -> p c t", p=128)[:, :, tl[0]:tl[0] + 512])
            xn, xnr = xnb.get()
            norm_mod(ph, t, r, lambda c: fing[:, c:c + 1], None, lambda c: xn[:, c, :], xnr, 512, pools)
            o, orr = ot.get()
            k = 0
            for tcq in range(4):
                for hh in range(2):
                    pt, pr = fw.ps()
                    for q in range(4):
                        dc = hh * 4 + q
                        fw.tr([xnr, r_c], [pr], pt[:, q * 128:(q + 1) * 128], xn[:, dc, tcq * 128:(tcq + 1) * 128], ident_f[:])
                    evac(k, [pr], [orr], o[:, tcq, hh * 512:(hh + 1) * 512], pt[:, 0:512])
                    k += 1
            fw.dma([orr], [], oflat[tl[0]:tl[0] + 512, :].rearrange("(c p) d -> p c d", p=128), o[:])
    fw.barrier()
    G.es.close()
    fw.close()
    print("built: n_ins", fw.n_ins, "n_wait", fw.n_wait)
    return nc

from concourse.bass_utils import run_bass_kernel_spmd


def kernel(**inputs):
    inp = {k: np.asarray(v) for k, v in inputs.items()}
    nc = build({"layers": DEPTH})
    shared = {}
    shared.update(shared_consts())
    shared.update(prep_weights(inp))
    in_maps = []
    for core in range(8):
        m = dict(shared)
        m.update(prep_core(inp, core))
        in_maps.append(m)
    res = run_bass_kernel_spmd(nc, in_maps, core_ids=list(range(8)))
    outs = [np.asarray(r["out"], dtype=np.float32) for r in res.results]
    return np.concatenate(outs, axis=0)
```
